# Optimizing a Trainium2 kernel written in Bass

```python
import math
import jax
import jax.numpy as jnp
from jax import lax
import numpy as np

D_MODEL = 2048
BATCH = 4
SEQ = 4096
DEPTH = 2

GRID_W = 64
CTX_LEN = 256
N_EVEN = (DEPTH + 1) // 2
N_ODD = DEPTH // 2
N_MOD = 6
EPS = 1e-6
NEG_INF = -1e30

A_WIDTH = D_MODEL // 2
S5_GROUP = 16
S5_GROUPS = A_WIDTH // S5_GROUP
S5_STATE = 64
B_WIDTH = D_MODEL // 2
GLA_HEADS = 4
GLA_DV = B_WIDTH // GLA_HEADS
GLA_DK = GLA_DV // 2
GLA_QK = GLA_HEADS * GLA_DK
GLA_RANK = 16
GLA_TAU = 16.0
GLA_CHUNK = 64
AB_IN = A_WIDTH + 2 * GLA_QK + 2 * B_WIDTH + 2 * GLA_RANK
AB_OUT = A_WIDTH + B_WIDTH
NA_HEAD_DIM = 128
NA_HEADS = D_MODEL // NA_HEAD_DIM
NA_KH_MAX = 8
NA_KW = 16
ROPE_THETA = 10000.0
MOE_GROUPS = 4
MOE_PER_GROUP = 8
MOE_EXPERTS = MOE_GROUPS * MOE_PER_GROUP
MOE_HIDDEN = D_MODEL // 2
MOE_TOP_K = 2
MOE_BLOCK = 128

F32 = jnp.float32

kernel_name = 'hybrid_s5_gla_natten_hmoe_block'


def rmsnorm(x, g):
    x32 = x.astype(F32)
    y = x32 * lax.rsqrt(jnp.mean(x32 * x32, axis=-1, keepdims=True) + EPS)
    return (y * g.astype(F32)).astype(x.dtype)


def modulate(x, g, shift, scale):
    return rmsnorm(x, g) * (1 + scale) + shift


def axial_rope(t):
    L, d = t.shape[1], t.shape[-1]
    half = d // 2
    nf = half // 2
    inv = ROPE_THETA ** (-jnp.arange(nf, dtype=F32) / nf)
    pos = jnp.arange(L)

    def rot(z, p):
        ang = p.astype(F32)[:, None] * inv[None, :]
        cos = jnp.cos(ang)[None, :, None, :]
        sin = jnp.sin(ang)[None, :, None, :]
        z1, z2 = z[..., :nf], z[..., nf:]
        return jnp.concatenate([z1 * cos - z2 * sin, z1 * sin + z2 * cos], axis=-1)

    t32 = t.astype(F32)
    out = jnp.concatenate([rot(t32[..., :half], pos // GRID_W), rot(t32[..., half:], pos % GRID_W)], axis=-1)
    return out.astype(t.dtype)


def s5_scan(u, h0_re, h0_im, lam_re, lam_im, log_dt, b_re, b_im, c_re, c_im):
    lr = jnp.minimum(lam_re.astype(F32), -1e-4)
    li = lam_im.astype(F32)
    dt = jnp.exp(log_dt.astype(F32))[:, None]
    mag = jnp.exp(lr * dt)
    abar_re = mag * jnp.cos(li * dt)
    abar_im = mag * jnp.sin(li * dt)
    num_re = abar_re - 1.0
    den = lr * lr + li * li
    f_re = (num_re * lr + abar_im * li) / den
    f_im = (abar_im * lr - num_re * li) / den
    bu_re = jnp.einsum('blgi,gpi->blgp', u, b_re.astype(F32))
    bu_im = jnp.einsum('blgi,gpi->blgp', u, b_im.astype(F32))
    x_re = f_re * bu_re - f_im * bu_im
    x_im = f_re * bu_im + f_im * bu_re
    L = u.shape[1]
    a_re = jnp.broadcast_to(abar_re, (1, L) + abar_re.shape)
    a_im = jnp.broadcast_to(abar_im, (1, L) + abar_im.shape)

    def combine(e1, e2):
        a1r, a1i, b1r, b1i = e1
        a2r, a2i, b2r, b2i = e2
        return (a2r * a1r - a2i * a1i, a2r * a1i + a2i * a1r,
                a2r * b1r - a2i * b1i + b2r, a2r * b1i + a2i * b1r + b2i)

    pr, pim, hr, hi = lax.associative_scan(combine, (a_re, a_im, x_re, x_im), axis=1)
    h0r, h0i = h0_re[:, None], h0_im[:, None]
    hr = hr + pr * h0r - pim * h0i
    hi = hi + pr * h0i + pim * h0r
    y = jnp.einsum('blgp,gip->blgi', hr, c_re.astype(F32)) - jnp.einsum('blgp,gip->blgi', hi, c_im.astype(F32))
    return y, hr[:, -1], hi[:, -1]


def s5_mixer(u_x, u_c, lam_re, lam_im, log_dt, b_re, b_im, c_re, c_im, d_skip, w_glu):
    def groups(u):
        return u.astype(F32).reshape(u.shape[0], u.shape[1], S5_GROUPS, S5_GROUP)

    ux, uc = groups(u_x), groups(u_c)
    zero = jnp.zeros((u_x.shape[0], S5_GROUPS, S5_STATE), F32)

    def prm(d):
        return (lam_re[d], lam_im[d], log_dt[d], b_re[d], b_im[d], c_re[d], c_im[d])

    def flip(t):
        return jnp.flip(t, axis=1)

    yc_f, hf_re, hf_im = s5_scan(uc, zero, zero, *prm(0))
    yx_f, _, _ = s5_scan(ux, hf_re, hf_im, *prm(0))
    yc_b, hb_re, hb_im = s5_scan(flip(uc), zero, zero, *prm(1))
    yx_b, _, _ = s5_scan(flip(ux), hb_re, hb_im, *prm(1))
    dsk = d_skip.astype(F32).reshape(S5_GROUPS, S5_GROUP)
    wg = w_glu.astype(F32)

    def out(u, yf, yb, like):
        Bn, L = u.shape[:2]
        y = (yf + flip(yb) + dsk * u).reshape(Bn, L, A_WIDTH)
        g = jax.nn.gelu(y)
        return (g * jax.nn.sigmoid(g @ wg)).astype(like.dtype)

    return out(ux, yx_f, yx_b, u_x), out(uc, yc_f, yc_b, u_c)


def gla_chunked(q, k, v, log_a, s0):
    Bn, H, L, dk = q.shape
    dv = v.shape[-1]
    n, C = L // GLA_CHUNK, GLA_CHUNK
    q = q.reshape(Bn, H, n, C, dk)
    k = k.reshape(Bn, H, n, C, dk)
    v = v.reshape(Bn, H, n, C, dv)
    b = jnp.cumsum(log_a.reshape(Bn, H, n, C, dk), axis=3)
    b_last = b[:, :, :, -1:, :]
    q_in = q * jnp.exp(b)
    k_in = k * jnp.exp(-b)
    k_out = k * jnp.exp(b_last - b)
    mask = jnp.tril(jnp.ones((C, C), dtype=bool))
    att = jnp.where(mask, jnp.einsum('bhncd,bhnsd->bhncs', q_in, k_in), 0.0)
    o = jnp.einsum('bhncs,bhnse->bhnce', att, v)
    u_chunk = jnp.einsum('bhncd,bhnce->bhnde', k_out, v)
    decay = jnp.exp(b_last[:, :, :, 0, :])

    def step(s, inp):
        dec, uc = inp
        return dec[..., None] * s + uc, s

    s_fin, s_before = lax.scan(step, s0, (jnp.moveaxis(decay, 2, 0), jnp.moveaxis(u_chunk, 2, 0)))
    s_before = jnp.moveaxis(s_before, 0, 2)
    o = o + jnp.einsum('bhncd,bhnde->bhnce', q_in, s_before)
    return o.reshape(Bn, H, L, dv), s_fin


def gla_mixer(p_x, p_c, w_gate2, b_gate, norm_g):
    o1 = GLA_QK
    o2 = 2 * GLA_QK
    o3 = o2 + B_WIDTH
    o4 = o3 + B_WIDTH

    def prep(p, rope):
        Bn, L, _ = p.shape
        q = p[..., :o1].reshape(Bn, L, GLA_HEADS, GLA_DK)
        k = p[..., o1:o2].reshape(Bn, L, GLA_HEADS, GLA_DK)
        v = p[..., o2:o3].reshape(Bn, L, GLA_HEADS, GLA_DV)
        r = p[..., o3:o4]
        lowrank = p[..., o4:]
        if rope:
            q, k = axial_rope(q), axial_rope(k)
        q = q * GLA_DK ** -0.5

        def gate(d):
            z = lowrank[..., d * GLA_RANK:(d + 1) * GLA_RANK] @ w_gate2[d] + b_gate[d]
            return (jax.nn.log_sigmoid(z.astype(F32)) / GLA_TAU).reshape(Bn, L, GLA_HEADS, GLA_DK)

        def bhld(t):
            return jnp.swapaxes(t, 1, 2).astype(F32)

        return bhld(q), bhld(k), bhld(v), r, bhld(gate(0)), bhld(gate(1))

    qx, kx, vx, rx, ax_f, ax_b = prep(p_x, True)
    qc, kc, vc, rc, ac_f, ac_b = prep(p_c, False)
    s0 = jnp.zeros((qx.shape[0], GLA_HEADS, GLA_DK, GLA_DV), F32)

    def fl(t):
        return jnp.flip(t, axis=2)

    oc_f, sc_f = gla_chunked(qc, kc, vc, ac_f, s0)
    ox_f, _ = gla_chunked(qx, kx, vx, ax_f, sc_f)
    oc_b, sc_b = gla_chunked(fl(qc), fl(kc), fl(vc), fl(ac_b), s0)
    ox_b, _ = gla_chunked(fl(qx), fl(kx), fl(vx), fl(ax_b), sc_b)
    g = norm_g.astype(F32).reshape(GLA_HEADS, GLA_DV)

    def finish(o, r):
        o = jnp.swapaxes(o, 1, 2)
        o = o * lax.rsqrt(jnp.mean(o * o, axis=-1, keepdims=True) + EPS) * g
        Bn, L = o.shape[:2]
        return (o.reshape(Bn, L, B_WIDTH) * jax.nn.silu(r.astype(F32))).astype(r.dtype)

    return finish(ox_f + fl(ox_b), rx), finish(oc_f + fl(oc_b), rc)


def ab_mixer(hx, hc, w_in, w_out, s5_lam_re, s5_lam_im, s5_log_dt, s5_b_re, s5_b_im, s5_c_re, s5_c_im,
             s5_d, s5_w_glu, gla_w_gate2, gla_b_gate, gla_norm_g):
    px, pc = hx @ w_in, hc @ w_in
    ax, ac = s5_mixer(px[..., :A_WIDTH], pc[..., :A_WIDTH], s5_lam_re, s5_lam_im, s5_log_dt,
                      s5_b_re, s5_b_im, s5_c_re, s5_c_im, s5_d, s5_w_glu)
    bx, bc = gla_mixer(px[..., A_WIDTH:], pc[..., A_WIDTH:], gla_w_gate2, gla_b_gate, gla_norm_g)
    return (jnp.concatenate([ax, bx], axis=-1) @ w_out, jnp.concatenate([ac, bc], axis=-1) @ w_out)


def na_mixer(hx, hc, w_qkv, w_out, q_norm, k_norm, rpb, ctx_out):
    Bn, L, D = hx.shape
    rows = L // GRID_W
    kh = min(NA_KH_MAX, rows)
    scale = NA_HEAD_DIM ** -0.5

    def heads(h):
        qkv = (h @ w_qkv).reshape(h.shape[0], h.shape[1], 3, NA_HEADS, NA_HEAD_DIM)
        return rmsnorm(qkv[:, :, 0], q_norm), rmsnorm(qkv[:, :, 1], k_norm), qkv[:, :, 2]

    qx, kx, vx = heads(hx)
    qc, kc, vc = heads(hc)
    qg = qx.reshape(Bn, rows, GRID_W, NA_HEADS, NA_HEAD_DIM)
    kg = kx.reshape(Bn, rows, GRID_W, NA_HEADS, NA_HEAD_DIM)
    vg = vx.reshape(Bn, rows, GRID_W, NA_HEADS, NA_HEAD_DIM)
    col = jnp.arange(GRID_W)
    cs = jnp.clip(col - NA_KW // 2, 0, GRID_W - NA_KW)
    col_ok = (col[None, :] >= cs[:, None]) & (col[None, :] < cs[:, None] + NA_KW)
    dc_idx = jnp.clip(col[None, :] - col[:, None] + NA_KW - 1, 0, 2 * NA_KW - 2)
    rpb_c = rpb.astype(F32)[:, :, dc_idx]

    def row_block(r):
        start = jnp.clip(r - kh // 2, 0, rows - kh)
        q_r = lax.dynamic_index_in_dim(qg, r, axis=1, keepdims=False)
        k_b = lax.dynamic_slice_in_dim(kg, start, kh, axis=1).reshape(Bn, kh * GRID_W, NA_HEADS, NA_HEAD_DIM)
        v_b = lax.dynamic_slice_in_dim(vg, start, kh, axis=1).reshape(Bn, kh * GRID_W, NA_HEADS, NA_HEAD_DIM)
        dr_idx = start + jnp.arange(kh) - r + NA_KH_MAX - 1
        bias = jnp.take(rpb_c, dr_idx, axis=1).transpose(0, 2, 1, 3)
        bias = jnp.where(col_ok[None, :, None, :], bias, NEG_INF).reshape(NA_HEADS, GRID_W, kh * GRID_W)
        s_lat = jnp.einsum('bqhd,bkhd->bhqk', q_r, k_b).astype(F32) * scale + bias
        s_ctx = jnp.einsum('bqhd,bkhd->bhqk', q_r, kc).astype(F32) * scale
        p = jax.nn.softmax(jnp.concatenate([s_lat, s_ctx], axis=-1), axis=-1).astype(vx.dtype)
        nl = kh * GRID_W
        return (jnp.einsum('bhqk,bkhd->bqhd', p[..., :nl], v_b)
                + jnp.einsum('bhqk,bkhd->bqhd', p[..., nl:], vc))

    o = lax.map(row_block, jnp.arange(rows))
    o_x = jnp.moveaxis(o, 0, 1).reshape(Bn, L, D) @ w_out
    if not ctx_out:
        return o_x, None
    s = jnp.einsum('bqhd,bkhd->bhqk', qc, kc).astype(F32) * scale
    p = jax.nn.softmax(s, axis=-1).astype(vc.dtype)
    o_c = jnp.einsum('bhqk,bkhd->bqhd', p, vc).reshape(hc.shape) @ w_out
    return o_x, o_c


def hier_moe(h, w_route_group, w_route_expert, w_gate, w_up, w_down):
    N, D = h.shape
    g_prob = jax.nn.softmax((h @ w_route_group).astype(F32), axis=-1)
    p_top, g_idx = lax.top_k(g_prob, 1)
    e_logits = (h @ w_route_expert).astype(F32).reshape(N, MOE_GROUPS, MOE_PER_GROUP)
    e_sel = jnp.take_along_axis(e_logits, g_idx[:, :, None], axis=1)[:, 0]
    e_val, e_idx = lax.top_k(e_sel, MOE_TOP_K)
    w = jax.nn.softmax(e_val, axis=-1) * p_top
    eid = (g_idx * MOE_PER_GROUP + e_idx).reshape(-1)
    tok = jnp.repeat(jnp.arange(N, dtype=jnp.int32), MOE_TOP_K)
    wt = w.reshape(-1)
    M = N * MOE_TOP_K
    order = jnp.argsort(eid)
    s_eid, s_tok, s_w = eid[order], tok[order], wt[order]
    counts = jnp.bincount(eid, length=MOE_EXPERTS)
    padded = (counts + MOE_BLOCK - 1) // MOE_BLOCK * MOE_BLOCK
    start = jnp.cumsum(counts) - counts
    pend = jnp.cumsum(padded)
    pstart = pend - padded
    dest = pstart[s_eid] + jnp.arange(M) - start[s_eid]
    P = (M + MOE_EXPERTS * (MOE_BLOCK - 1) + MOE_BLOCK - 1) // MOE_BLOCK * MOE_BLOCK
    nb = P // MOE_BLOCK
    buf_tok = jnp.full((P,), N, jnp.int32).at[dest].set(s_tok)
    buf_w = jnp.zeros((P,), F32).at[dest].set(s_w)
    blk_e = jnp.minimum(jnp.searchsorted(pend, jnp.arange(nb) * MOE_BLOCK, side='right'), MOE_EXPERTS - 1)
    h_pad = jnp.concatenate([h, jnp.zeros((1, D), h.dtype)], axis=0)
    xb = h_pad[buf_tok].reshape(nb, MOE_BLOCK, D)

    def expert_block(args):
        xblk, e = args
        return (jax.nn.silu(xblk @ w_gate[e]) * (xblk @ w_up[e])) @ w_down[e]

    yb = lax.map(expert_block, (xb, blk_e)).reshape(P, D)
    y = jax.ops.segment_sum(yb * buf_w.astype(yb.dtype)[:, None], buf_tok, num_segments=N + 1)
    return y[:N]


def setup_inputs(seed: int = 0) -> dict:
    key = jax.random.key(seed)
    ks = jax.random.split(key, 40)

    def nrm(i, shape, s):
        return jax.random.normal(ks[i], shape, F32) * s

    D = D_MODEL
    G, P, I = S5_GROUPS, S5_STATE, S5_GROUP
    E, F = MOE_EXPERTS, MOE_HIDDEN
    lam_im = jnp.broadcast_to(math.pi * jnp.arange(P, dtype=F32), (N_EVEN, 2, G, P))
    return {
        'x': nrm(0, (BATCH, SEQ, D), 1.0),
        'c': nrm(1, (BATCH, D), 1.0),
        'ctx': nrm(2, (BATCH, CTX_LEN, D), 1.0),
        'c_ctx': nrm(3, (D,), 1.0),
        'ada_w': nrm(4, (DEPTH, D, N_MOD * D), 0.5 * D ** -0.5),
        'ada_b': nrm(5, (DEPTH, N_MOD * D), 0.02),
        'norm1_g': 1.0 + nrm(6, (DEPTH, D), 0.02),
        'norm2_g': 1.0 + nrm(7, (DEPTH, D), 0.02),
        'ab_w_in': nrm(8, (N_EVEN, D, AB_IN), D ** -0.5),
        'ab_w_out': nrm(9, (N_EVEN, AB_OUT, D), AB_OUT ** -0.5),
        's5_lam_re': -0.5 + nrm(10, (N_EVEN, 2, G, P), 0.01),
        's5_lam_im': lam_im + nrm(11, (N_EVEN, 2, G, P), 0.01),
        's5_log_dt': jax.random.uniform(ks[12], (N_EVEN, 2, G), F32, minval=math.log(1e-3), maxval=math.log(1e-1)),
        's5_b_re': nrm(13, (N_EVEN, 2, G, P, I), (2 * I) ** -0.5),
        's5_b_im': nrm(14, (N_EVEN, 2, G, P, I), (2 * I) ** -0.5),
        's5_c_re': nrm(15, (N_EVEN, 2, G, I, P), P ** -0.5),
        's5_c_im': nrm(16, (N_EVEN, 2, G, I, P), P ** -0.5),
        's5_d': nrm(17, (N_EVEN, A_WIDTH), 1.0),
        's5_w_glu': nrm(18, (N_EVEN, A_WIDTH, A_WIDTH), A_WIDTH ** -0.5),
        'gla_w_gate2': nrm(19, (N_EVEN, 2, GLA_RANK, GLA_QK), GLA_RANK ** -0.5),
        'gla_b_gate': nrm(20, (N_EVEN, 2, GLA_QK), 0.1),
        'gla_norm_g': 1.0 + nrm(21, (N_EVEN, B_WIDTH), 0.02),
        'na_w_qkv': nrm(22, (N_ODD, D, 3 * D), D ** -0.5),
        'na_w_out': nrm(23, (N_ODD, D, D), D ** -0.5),
        'na_q_norm': 1.0 + nrm(24, (N_ODD, NA_HEAD_DIM), 0.02),
        'na_k_norm': 1.0 + nrm(25, (N_ODD, NA_HEAD_DIM), 0.02),
        'na_rpb': nrm(26, (N_ODD, NA_HEADS, 2 * NA_KH_MAX - 1, 2 * NA_KW - 1), 0.05),
        'moe_w_route_group': nrm(27, (DEPTH, D, MOE_GROUPS), D ** -0.5),
        'moe_w_route_expert': nrm(28, (DEPTH, D, E), D ** -0.5),
        'moe_w_gate': nrm(29, (DEPTH, E, D, F), D ** -0.5),
        'moe_w_up': nrm(30, (DEPTH, E, D, F), D ** -0.5),
        'moe_w_down': nrm(31, (DEPTH, E, F, D), F ** -0.5),
    }


def reference(x, c, ctx, c_ctx, ada_w, ada_b, norm1_g, norm2_g, ab_w_in, ab_w_out,
              s5_lam_re, s5_lam_im, s5_log_dt, s5_b_re, s5_b_im, s5_c_re, s5_c_im, s5_d, s5_w_glu,
              gla_w_gate2, gla_b_gate, gla_norm_g, na_w_qkv, na_w_out, na_q_norm, na_k_norm, na_rpb,
              moe_w_route_group, moe_w_route_expert, moe_w_gate, moe_w_up, moe_w_down):
    D = x.shape[-1]
    sc = jax.nn.silu(c)
    scc = jax.nn.silu(c_ctx)
    for i in range(DEPTH):
        j = i // 2
        last = i == DEPTH - 1
        mod_x = (sc @ ada_w[i] + ada_b[i])[:, None, :]
        mod_c = (scc @ ada_w[i] + ada_b[i])[None, None, :]
        sh1, sc1, g1, sh2, sc2, g2 = jnp.split(mod_x, N_MOD, axis=-1)
        csh1, csc1, cg1, csh2, csc2, cg2 = jnp.split(mod_c, N_MOD, axis=-1)
        hx = modulate(x, norm1_g[i], sh1, sc1)
        hc = modulate(ctx, norm1_g[i], csh1, csc1)
        if i % 2 == 0:
            ox, oc = ab_mixer(hx, hc, ab_w_in[j], ab_w_out[j], s5_lam_re[j], s5_lam_im[j], s5_log_dt[j],
                              s5_b_re[j], s5_b_im[j], s5_c_re[j], s5_c_im[j], s5_d[j], s5_w_glu[j],
                              gla_w_gate2[j], gla_b_gate[j], gla_norm_g[j])
        else:
            ox, oc = na_mixer(hx, hc, na_w_qkv[j], na_w_out[j], na_q_norm[j], na_k_norm[j], na_rpb[j],
                              not last)
        x = x + g1 * ox
        hx = modulate(x, norm2_g[i], sh2, sc2)
        moe_w = (moe_w_route_group[i], moe_w_route_expert[i], moe_w_gate[i], moe_w_up[i], moe_w_down[i])
        if last:
            x = x + g2 * hier_moe(hx.reshape(-1, D), *moe_w).reshape(x.shape)
        else:
            ctx = ctx + cg1 * oc
            hc = modulate(ctx, norm2_g[i], csh2, csc2)
            nx = hx.shape[0] * hx.shape[1]
            y = hier_moe(jnp.concatenate([hx.reshape(-1, D), hc.reshape(-1, D)], axis=0), *moe_w)
            x = x + g2 * y[:nx].reshape(x.shape)
            ctx = ctx + cg2 * y[nx:].reshape(ctx.shape)
    return x
```

```python
import math, time, sys


import sys
import numpy as np
import concourse.bass as bass
import concourse.mybir as mybir
from concourse.bass_utils import run_bass_kernel_spmd

F32 = mybir.dt.float32
BF16 = mybir.dt.bfloat16
I32 = mybir.dt.int32
AF = mybir.ActivationFunctionType
ALU = mybir.AluOpType
AX = mybir.AxisListType


class V:
    def __init__(self, tile, ap):
        self.tile = tile
        self.ap = ap

    def __getitem__(self, idx):
        return V(self.tile, self.ap[idx])


class T:
    def __init__(self, kb, tensor, name):
        self.kb = kb
        self.t = tensor
        self.name = name
        self.writes = {}
        self.reads = {}

    def __getitem__(self, idx):
        return V(self, self.t[idx])

    @property
    def all(self):
        return V(self, self.t[:])


def _ap(x):
    return x.ap if isinstance(x, V) else x


class KB:
    NDMA = 28

    def __init__(self):
        self.nc = bass.Bass("TRN2", target_bir_lowering=False)
        nc = self.nc
        self.eng = {'pe': nc.tensor, 'act': nc.scalar, 'dve': nc.vector, 'pool': nc.gpsimd, 'sp': nc.sync}
        self.sems = {}
        self.cnt = {}
        for e in ['pe', 'act', 'dve', 'pool']:
            self.sems[e] = nc.alloc_semaphore(name=f"sem_{e}")
            self.cnt[e] = 0
        for i in range(self.NDMA):
            self.sems[('d', i)] = nc.alloc_semaphore(name=f"sem_d{i}")
            self.cnt[('d', i)] = 0
        self.rr = 0
        self.waited = {e: {} for e in self.eng}
        self.out_tokens = []
        self.ntiles = 0
        self.ninstr = 0
        self.cms = []

    def sb(self, shape, dtype=F32, name=None):
        self.ntiles += 1
        name = "s_" + (name or f"t{self.ntiles}")
        cm = self.nc.sbuf_tensor(name, list(shape), dtype)
        t = cm.__enter__()
        self.cms.append(cm)
        return T(self, t, name)

    def mark(self):
        return len(self.cms)

    def barrier(self):
        for e in self.eng:
            for sk, val in self.cnt.items():
                if val > 0:
                    self._wait(e, sk, val)

    def release(self, mark):
        self.barrier()
        while len(self.cms) > mark:
            cm = self.cms.pop()
            cm.__exit__(None, None, None)

    def ps(self, shape, dtype=F32, name=None):
        self.ntiles += 1
        name = "ps_" + (name or f"p{self.ntiles}")
        cm = self.nc.psum_tensor(name, list(shape), dtype)
        t = cm.__enter__()
        self.cms.append(cm)
        return T(self, t, name)

    def dram_in(self, name, shape, dtype=F32):
        return self.nc.dram_tensor(name, list(shape), dtype, kind="ExternalInput").ap()

    def dram_out(self, name, shape, dtype=F32):
        return self.nc.dram_tensor(name, list(shape), dtype, kind="ExternalOutput").ap()

    def _wait(self, e, sk, val):
        w = self.waited[e]
        if w.get(sk, 0) >= val:
            return
        self.eng[e].wait_ge(self.sems[sk], val)
        w[sk] = val

    def issue(self, e, fn, outs, ins, dma=False):
        deps = []
        for v in ins:
            if isinstance(v, V):
                deps.extend(v.tile.writes.items())
        for v in outs:
            if isinstance(v, V):
                deps.extend(v.tile.writes.items())
                deps.extend(v.tile.reads.items())
        for sk, val in deps:
            if sk == e and e == 'pe':
                continue
            self._wait(e, sk, val)
        if dma:
            i = self.rr
            self.rr = (self.rr + 1) % self.NDMA
            sk = ('d', i)
            self._wait(e, sk, self.cnt[sk])
            inst = fn()
            self.cnt[sk] += 16
            inst.then_inc(self.sems[sk], 16)
        else:
            sk = e
            inst = fn()
            self.cnt[sk] += 1
            inst.then_inc(self.sems[sk], 1)
        tok = (sk, self.cnt[sk])
        self.ninstr += 1
        for v in ins:
            if isinstance(v, V):
                r = v.tile.reads
                if r.get(sk, 0) < tok[1]:
                    r[sk] = tok[1]
        for v in outs:
            if isinstance(v, V):
                w = v.tile.writes
                if w.get(sk, 0) < tok[1]:
                    w[sk] = tok[1]
        return tok

    def dma(self, out, in_, e='sp', is_output=False, **kw):
        tok = self.issue(e, lambda: self.eng[e].dma_start(out=_ap(out), in_=_ap(in_), **kw), [out], [in_], dma=True)
        if is_output:
            self.out_tokens.append(tok)
        return tok

    def mm(self, out, lhsT, rhs, start=True, stop=True, **kw):
        return self.issue('pe', lambda: self.nc.tensor.matmul(_ap(out), _ap(lhsT), _ap(rhs), start=start, stop=stop, **kw),
                          [out], [lhsT, rhs])

    def transpose(self, out, in_, ident):
        return self.issue('pe', lambda: self.nc.tensor.transpose(_ap(out), _ap(in_), _ap(ident)), [out], [in_, ident])

    def act(self, out, in_, func, bias=None, scale=None, accum_out=None, e='act'):
        kw = {}
        ins = [in_]
        outs = [out]
        if bias is not None:
            kw['bias'] = _ap(bias)
            if isinstance(bias, V):
                ins.append(bias)
        if scale is not None:
            kw['scale'] = _ap(scale)
            if isinstance(scale, V):
                ins.append(scale)
        if accum_out is not None:
            kw['accum_out'] = _ap(accum_out)
            outs.append(accum_out)
        return self.issue('act', lambda: self.nc.scalar.activation(out=_ap(out), in_=_ap(in_), func=func, **kw), outs, ins)

    def ts(self, out, in0, s1, op0, s2=None, op1=None, e='dve', accum_out=None):
        ins = [in0] + [s for s in (s1, s2) if isinstance(s, V)]
        kw = {}
        outs = [out]
        if op1 is not None:
            kw['op1'] = op1
        if accum_out is not None:
            kw['accum_out'] = _ap(accum_out)
            outs.append(accum_out)
        return self.issue(e, lambda: self.eng[e].tensor_scalar(out=_ap(out), in0=_ap(in0), scalar1=_ap(s1), scalar2=_ap(s2),
                                                               op0=op0, **kw), outs, ins)

    def tt(self, out, in0, in1, op, e='dve'):
        return self.issue(e, lambda: self.eng[e].tensor_tensor(out=_ap(out), in0=_ap(in0), in1=_ap(in1), op=op), [out], [in0, in1])

    def stt(self, out, in0, scalar, in1, op0, op1, e='dve'):
        ins = [in0, in1] + ([scalar] if isinstance(scalar, V) else [])
        return self.issue(e, lambda: self.eng[e].scalar_tensor_tensor(out=_ap(out), in0=_ap(in0), scalar=_ap(scalar), in1=_ap(in1),
                                                                      op0=op0, op1=op1), [out], ins)

    def copy(self, out, in_, e='dve'):
        if e == 'act':
            return self.act(out, in_, AF.Copy)
        return self.issue(e, lambda: self.eng[e].tensor_copy(out=_ap(out), in_=_ap(in_)), [out], [in_])

    def memset(self, out, val, e='dve'):
        return self.issue(e, lambda: self.eng[e].memset(_ap(out), val), [out], [])

    def recip(self, out, in_):
        return self.issue('dve', lambda: self.nc.vector.reciprocal(out=_ap(out), in_=_ap(in_)), [out], [in_])

    def scan(self, out, d0, d1, initial, op0=ALU.mult, op1=ALU.add):
        ins = [d0, d1] + ([initial] if isinstance(initial, V) else [])
        return self.issue('dve', lambda: self.nc.vector.tensor_tensor_scan(out=_ap(out), data0=_ap(d0), data1=_ap(d1),
                                                                           initial=_ap(initial), op0=op0, op1=op1), [out], ins)

    def reduce(self, out, in_, op, axis=AX.X, e='dve'):
        return self.issue(e, lambda: self.eng[e].tensor_reduce(out=_ap(out), in_=_ap(in_), axis=axis, op=op), [out], [in_])

    def iota(self, out, pattern, base=0, channel_multiplier=0, **kw):
        return self.issue('pool', lambda: self.nc.gpsimd.iota(_ap(out), pattern, base=base, channel_multiplier=channel_multiplier, **kw),
                          [out], [])

    def affine_select(self, out, in_, pattern, compare_op, fill, base=0, channel_multiplier=0):
        return self.issue('pool', lambda: self.nc.gpsimd.affine_select(out=_ap(out), in_=_ap(in_), pattern=pattern,
                                                                       compare_op=compare_op, fill=fill, base=base,
                                                                       channel_multiplier=channel_multiplier), [out], [in_])

    def finish(self):
        last = {}
        for sk, val in self.out_tokens:
            last[sk] = max(last.get(sk, 0), val)
        for sk, val in last.items():
            self._wait('sp', sk, val)
        return self.nc


def run(kb_or_nc, in_maps, n=8):
    nc = kb_or_nc.nc if isinstance(kb_or_nc, KB) else kb_or_nc
    import time as _time
    _t = _time.time()
    res = run_bass_kernel_spmd(nc, in_maps, core_ids=list(range(n)))
    nb = sum(a.nbytes for m in in_maps for a in m.values())
    print(f"[launch] {_time.time() - _t:.1f}s in_bytes={nb/1e6:.0f}MB", file=sys.stderr, flush=True)
    return res.results


def chunks(NT, sz=512):
    return [(s, min(sz, NT - s)) for s in range(0, NT, sz)]

def load_vecs(kb, vecs_d, nv):
    vt = kb.sb([128, 16, nv], name="vecs")
    kb.dma(vt.all, vecs_d.rearrange("(kt p) v -> p kt v", p=128))
    return vt

def consts(kb):
    c = {}
    c['ones'] = kb.sb([128, 128], name="ones"); kb.memset(c['ones'].all, 1.0)
    c['eps'] = kb.sb([128, 1], name="eps"); kb.memset(c['eps'].all, 1e-6)
    return c

def norm_mod(kb, c, xT, hT, vt, NT, seg_cols, gcol, D=2048, xdt=F32):
    KT = D // 128
    gsc = {}
    for (s0, sz, shc, scc) in seg_cols:
        if scc not in gsc:
            g = kb.sb([128, KT], name=f"gsc{scc}")
            kb.stt(g.all, vt[:, :, scc], 1.0, vt[:, :, gcol], ALU.add, ALU.mult)
            gsc[scc] = g
    sq = [kb.sb([128, 512], name=f"sq{i}") for i in range(2)]
    tmp = [kb.sb([128, 512], name=f"nm_tmp{i}") for i in range(2)]
    rstd = kb.sb([128, 512], name="rstd")
    ps = kb.ps([128, 512], name="ps_norm")
    n = 0
    for (s0, sz, shc, scc) in seg_cols:
        for (c0, cs) in chunks(sz):
            a = s0 + c0
            for kt in range(KT):
                q = sq[kt % 2]
                kb.act(q[:, :cs], xT[:, kt, a:a+cs], AF.Square)
                kb.mm(ps[:, :cs], c['ones'].all, q[:, :cs], start=(kt == 0), stop=(kt == KT-1))
            kb.act(rstd[:, :cs], ps[:, :cs], AF.Sqrt, bias=c['eps'].all, scale=1.0 / D)
            kb.recip(rstd[:, :cs], rstd[:, :cs])
            for kt in range(KT):
                t = tmp[kt % 2]
                kb.stt(t[:, :cs], xT[:, kt, a:a+cs], gsc[scc][:, kt:kt+1], rstd[:, :cs], ALU.mult, ALU.mult)
                kb.act(hT[:, kt, a:a+cs], t[:, :cs], AF.Identity, bias=vt[:, kt, shc:shc+1])

def proj(kb, hT, W_d, NT, n_out, KT, evac, wname="w", tag="", pss=None, wbufs=None):
    if wbufs is None:
        wst = [kb.sb([128, KT, 128], F32, name=f"{tag}wst{i}") for i in range(2)]
        wbf = [kb.sb([128, KT, 128], BF16, name=f"{tag}wbf{i}") for i in range(2)]
    else:
        wst, wbf = wbufs
    if pss is None:
        pss = [kb.ps([128, 512], name=f"{tag}pp{i}") for i in range(3)]
    pi = 0
    Wv = W_d.rearrange("(kt p) n -> p kt n", p=128)
    nj = (n_out + 127) // 128
    for j in range(nj):
        nsz = min(128, n_out - j * 128)
        ws, wb = wst[j % 2], wbf[j % 2]
        kb.dma(ws[:, :KT, :nsz], Wv[:, :, j*128:j*128+nsz])
        kb.copy(wb[:, :KT, :nsz], ws[:, :KT, :nsz], e='pool')
        for (c0, cs) in chunks(NT):
            p = pss[pi % 3]; pi += 1
            for kt in range(KT):
                kb.mm(p[:nsz, :cs], wb[:, kt, :nsz], hT[:, kt, c0:c0+cs], start=(kt == 0), stop=(kt == KT-1))
            evac(j, nsz, c0, cs, p[:nsz, :cs])

def build_front(NT=2176, NLAT=2048, n_out=4128):
    kb = KB()
    xT_d = kb.dram_in("xT", [2048, NT])
    vecs_d = kb.dram_in("vecs", [2048, 5])
    W_d = kb.dram_in("W", [2048, n_out])
    pT_d = kb.dram_out("pT", [n_out, NT])
    c = consts(kb)
    vt = load_vecs(kb, vecs_d, 5)
    xv = xT_d.rearrange("(kt p) t -> p kt t", p=128)
    hT = kb.sb([128, 16, NT], BF16, name="hT")
    def x_src(kt, a, cs, dst):
        kb.dma(dst, xv[:, kt, a:a+cs])
    def sink(a, cs, h32):
        kb.copy(hT[:, :, a:a+cs], h32[:, :, :cs], e='pool')
    m = kb.mark()
    norm_mod2(kb, c, x_src, sink, vt, [(0, NLAT, 0, 1), (NLAT, NT - NLAT, 2, 3)], 4, tag="n1")
    kb.release(m)
    ost = [kb.sb([128, NT], F32, name=f"ost{i}") for i in range(2)]
    state = {'n': 0}
    def evac(j, nsz, c0, cs, pv):
        o = ost[j % 2]
        if state['n'] % 2 == 0:
            kb.copy(o[:nsz, c0:c0+cs], pv, e='act')
        else:
            kb.copy(o[:nsz, c0:c0+cs], pv, e='dve')
        state['n'] += 1
        if c0 + cs == NT:
            kb.dma(pT_d[j*128:j*128+nsz, :], o[:nsz, :], is_output=True)
    proj(kb, hT, W_d, NT, n_out, 16, evac)
    kb.finish()
    return kb

def ref_front(x, vecs, W):
    x = x.astype(np.float64)
    g = vecs[:, 4]
    y = x / np.sqrt((x * x).mean(-1, keepdims=True) + 1e-6) * g
    NT = x.shape[0]
    return y


GC = 2 * math.sqrt(2 / math.pi)

def dtrack(kb, ap, name):
    return V(T(kb, None, name), ap)

def norm_mod2(kb, c, x_src, hT_sink, vt, seg_cols, gcol, D=2048, tag="nm"):
    KT = D // 128
    gsc = {}
    for (s0, sz, shc, scc) in seg_cols:
        if scc not in gsc:
            g = kb.sb([128, KT], name=f"{tag}gsc{scc}")
            kb.stt(g.all, vt[:, :, scc], 1.0, vt[:, :, gcol], ALU.add, ALU.mult)
            gsc[scc] = g
    xc = kb.sb([128, KT, 512], name=f"{tag}_xc")
    h32 = kb.sb([128, KT, 512], name=f"{tag}_h32")
    sq = [kb.sb([128, 512], name=f"{tag}_sq{i}") for i in range(2)]
    rstd = kb.sb([128, 512], name=f"{tag}_rstd")
    ps = kb.ps([128, 512], name=f"{tag}_ps")
    for (s0, sz, shc, scc) in seg_cols:
        for (c0, cs) in chunks(sz):
            a = s0 + c0
            for kt in range(KT):
                x_src(kt, a, cs, xc[:, kt, :cs])
                q = sq[kt % 2]
                kb.act(q[:, :cs], xc[:, kt, :cs], AF.Square)
                kb.mm(ps[:, :cs], c['ones'].all, q[:, :cs], start=(kt == 0), stop=(kt == KT-1))
            kb.act(rstd[:, :cs], ps[:, :cs], AF.Sqrt, bias=c['eps'].all, scale=1.0 / D)
            kb.recip(rstd[:, :cs], rstd[:, :cs])
            for kt in range(KT):
                kb.stt(xc[:, kt, :cs], xc[:, kt, :cs], gsc[scc][:, kt:kt+1], rstd[:, :cs], ALU.mult, ALU.mult)
                kb.act(h32[:, kt, :cs], xc[:, kt, :cs], AF.Identity, bias=vt[:, kt, shc:shc+1])
            hT_sink(a, cs, h32)

def gelu_tanh(kb, out, x, t1, t2):
    kb.tt(t1, x, x, ALU.mult, e='pool')
    kb.ts(t1, t1, 0.044715, ALU.mult, 1.0, ALU.add)
    kb.tt(t1, t1, x, ALU.mult, e='pool')
    kb.act(t2, t1, AF.Sigmoid, scale=GC)
    kb.tt(out, t2, x, ALU.mult)

def routing(kb, lg, cw_out, wk):
    L = wk['L']; kb.copy(L[:, :], lg, e='act')
    gmax, ngmax, gm, eg, se, pen = wk['gmax'], wk['ngmax'], wk['gm'], wk['eg'], wk['se'], wk['pen']
    kb.reduce(gmax.all, L[:, 0:4], ALU.max)
    kb.ts(ngmax.all, gmax.all, -1.0, ALU.mult)
    kb.ts(gm.all, L[:, 0:4], gmax[:, 0:1], ALU.is_equal)
    kb.act(eg.all, L[:, 0:4], AF.Exp, bias=ngmax[:, 0:1], accum_out=se.all)
    kb.recip(se.all, se.all)
    kb.ts(pen.all, gm.all, 1e30, ALU.mult, -1e30, ALU.add)
    elm = wk['elm']
    for g in range(4):
        kb.ts(elm[:, g*8:(g+1)*8], L[:, 4+g*8:4+(g+1)*8], pen[:, g:g+1], ALU.add)
    top8 = wk['top8']
    kb.issue('dve', lambda: kb.nc.vector.max(out=top8.all.ap, in_=elm.all.ap), [top8.all], [elm.all])
    d, w1, w2, m1, m2 = wk['d'], wk['w1'], wk['w2'], wk['m1'], wk['m2']
    kb.tt(d.all, top8[:, 1:2], top8[:, 0:1], ALU.subtract)
    kb.act(d.all, d.all, AF.Exp)
    kb.ts(w1.all, d.all, 1.0, ALU.add)
    kb.recip(w1.all, w1.all)
    kb.tt(w2.all, d.all, w1.all, ALU.mult)
    kb.tt(w1.all, w1.all, se.all, ALU.mult)
    kb.tt(w2.all, w2.all, se.all, ALU.mult)
    kb.ts(m1.all, elm.all, top8[:, 0:1], ALU.is_equal, w1[:, 0:1], ALU.mult)
    kb.ts(m2.all, elm.all, top8[:, 1:2], ALU.is_equal, w2[:, 0:1], ALU.mult)
    kb.tt(cw_out, m1.all, m2.all, ALU.add)

def routing_wk(kb):
    def s(n, w): return kb.sb([128, w], name="rt_" + n)
    return {'L': s('L', 36), 'gmax': s('gmax', 1), 'ngmax': s('ngmax', 1), 'gm': s('gm', 4), 'eg': s('eg', 4), 'se': s('se', 1),
            'pen': s('pen', 4), 'elm': s('elm', 32), 'top8': s('top8', 8), 'd': s('d', 1), 'w1': s('w1', 1), 'w2': s('w2', 1),
            'm1': s('m1', 32), 'm2': s('m2', 32)}

def back_tail(kb, c, aT, KTa, wout_d, xT_d, x1T_d, h2T_d, cw_d, vt, wr_d, NT, segs, g1cols, gcol, pss=None, wbufs=None):
    x1tr = dtrack(kb, x1T_d, "x1T_dram")
    x1v = x1T_d.rearrange("(kt p) t -> p kt t", p=128)
    xv = xT_d.rearrange("(kt p) t -> p kt t", p=128)
    xin = [kb.sb([128, 512], name=f"bt_xin{i}") for i in range(3)]
    xo = [kb.sb([128, 512], name=f"bt_xo{i}") for i in range(3)]
    st = {'n': 0}
    def seg_of(tok):
        for i, (s0, sz, _, _) in enumerate(segs):
            if s0 <= tok < s0 + sz: return i
    def evac(j, nsz, c0, cs, pv):
        i = st['n'] % 3; st['n'] += 1
        kb.dma(xin[i][:, :cs], xv[:, j, c0:c0+cs])
        sg = seg_of(c0)
        kb.stt(xo[i][:, :cs], pv, vt[:, j, g1cols[sg]:g1cols[sg]+1], xin[i][:, :cs], ALU.mult, ALU.add)
        kb.dma(V(x1tr.tile, x1v[:, j, c0:c0+cs]), xo[i][:, :cs], is_output=True)
    proj(kb, aT, wout_d, NT, 2048, KTa, evac, tag="wo", pss=pss, wbufs=wbufs)
    wr = kb.sb([128, 16, 36], name="bt_wr"); kb.dma(wr.all, wr_d.rearrange("(kt p) n -> p kt n", p=128))
    hb = kb.sb([128, 16, 512], BF16, name="bt_hb")
    cwsb = kb.sb([128, 32], name="bt_cw")
    psr = kb.ps([128, 36], name="bt_psr")
    wk = routing_wk(kb)
    h2v = h2T_d.rearrange("(kt p) t -> p kt t", p=128)
    def x_src(kt, a, cs, dst):
        kb.dma(dst, V(x1tr.tile, x1v[:, kt, a:a+cs]))
    def sink(a, cs, h32):
        kb.copy(hb[:, :, :cs], h32[:, :, :cs], e='pool')
        kb.dma(h2v[:, :, a:a+cs], hb[:, :, :cs], is_output=True)
        for tt_ in range(cs // 128):
            for kt in range(16):
                kb.mm(psr.all, h32[:, kt, tt_*128:(tt_+1)*128], wr[:, kt, :], start=(kt == 0), stop=(kt == 15))
            routing(kb, psr.all, cwsb.all, wk)
            kb.dma(cw_d[a+tt_*128:a+(tt_+1)*128, :], cwsb.all, is_output=True)
    norm_mod2(kb, c, x_src, sink, vt, segs, gcol, tag="n2")

def build_back0(NT=2176, NLAT=2048):
    kb = KB()
    D = {}
    for n in ["yfT", "ybT", "uT", "ofT", "obT", "rT"]:
        D[n] = kb.dram_in(n, [1024, NT])
    xT_d = kb.dram_in("xT", [2048, NT])
    vecs_d = kb.dram_in("vecs", [2048, 7])
    v8_d = kb.dram_in("v8", [1024, 2])
    wglu_d = kb.dram_in("wglu", [1024, 1024]); wout_d = kb.dram_in("wout", [2048, 2048]); wr_d = kb.dram_in("wr", [2048, 36])
    x1T_d = kb.dram_out("x1T", [2048, NT]); h2T_d = kb.dram_out("h2T", [2048, NT], BF16); cw_d = kb.dram_out("cw", [NT, 32])
    c = consts(kb)
    vt = kb.sb([128, 16, 7], name="vecs_sb"); kb.dma(vt.all, vecs_d.rearrange("(kt p) v -> p kt v", p=128))
    v8 = kb.sb([128, 8, 2], name="v8_sb"); kb.dma(v8.all, v8_d.rearrange("(kt p) v -> p kt v", p=128))
    aT = kb.sb([128, 16, NT], BF16, name="aT")
    mk = kb.mark()
    gT = kb.sb([128, 8, NT], BF16, name="gT")
    def ld(n): return [kb.sb([128, 512], name=f"ld_{n}{i}") for i in range(2)]
    A, Bt, Ct = ld("a"), ld("b"), ld("c")
    t1 = kb.sb([128, 512], name="b0_t1"); t2 = kb.sb([128, 512], name="b0_t2")
    views = {n: D[n].rearrange("(kt p) t -> p kt t", p=128) for n in D}
    n = 0
    for (c0, cs) in chunks(NT):
        for kt in range(8):
            a, b, u = A[n % 2], Bt[n % 2], Ct[n % 2]; n += 1
            kb.dma(a[:, :cs], views["yfT"][:, kt, c0:c0+cs]); kb.dma(b[:, :cs], views["ybT"][:, kt, c0:c0+cs])
            kb.dma(u[:, :cs], views["uT"][:, kt, c0:c0+cs])
            kb.tt(a[:, :cs], a[:, :cs], b[:, :cs], ALU.add)
            kb.stt(a[:, :cs], u[:, :cs], v8[:, kt, 0:1], a[:, :cs], ALU.mult, ALU.add)
            gelu_tanh(kb, gT[:, kt, c0:c0+cs], a[:, :cs], t1[:, :cs], t2[:, :cs])
    def evac_glu(j, nsz, c0, cs, pv):
        kb.act(t2[:, :cs], pv, AF.Sigmoid)
        kb.tt(aT[:, j, c0:c0+cs], t2[:, :cs], gT[:, j, c0:c0+cs], ALU.mult)
    pss = [kb.ps([128, 512], name=f"shp{i}") for i in range(3)]
    wbufs = ([kb.sb([128, 16, 128], F32, name=f"shwst{i}") for i in range(2)], [kb.sb([128, 16, 128], BF16, name=f"shwbf{i}") for i in range(2)])
    proj(kb, gT, wglu_d, NT, 1024, 8, evac_glu, tag="glu", pss=pss, wbufs=wbufs)
    o2 = [kb.sb([128, 512], name=f"b0_o{i}") for i in range(2)]
    r2 = [kb.sb([128, 512], name=f"b0_r{i}") for i in range(2)]
    sq = kb.sb([128, 512], name="b0_sq"); rstd = kb.sb([128, 512], name="b0_rstd")
    psn = kb.ps([128, 512], name="b0_psn")
    for (c0, cs) in chunks(NT):
        for h in range(4):
            for i in range(2):
                kt = 2 * h + i
                a, b = A[i], Bt[i]
                kb.dma(a[:, :cs], views["ofT"][:, kt, c0:c0+cs]); kb.dma(b[:, :cs], views["obT"][:, kt, c0:c0+cs])
                kb.dma(r2[i][:, :cs], views["rT"][:, kt, c0:c0+cs])
                kb.tt(o2[i][:, :cs], a[:, :cs], b[:, :cs], ALU.add)
                kb.act(sq[:, :cs], o2[i][:, :cs], AF.Square)
                kb.mm(psn[:, :cs], c['ones'].all, sq[:, :cs], start=(i == 0), stop=(i == 1))
            kb.act(rstd[:, :cs], psn[:, :cs], AF.Sqrt, bias=c['eps'].all, scale=1.0 / 256)
            kb.recip(rstd[:, :cs], rstd[:, :cs])
            for i in range(2):
                kt = 2 * h + i
                kb.stt(o2[i][:, :cs], o2[i][:, :cs], v8[:, kt, 1:2], rstd[:, :cs], ALU.mult, ALU.mult)
                kb.act(t2[:, :cs], r2[i][:, :cs], AF.Sigmoid)
                kb.tt(t1[:, :cs], r2[i][:, :cs], t2[:, :cs], ALU.mult, e='pool')
                kb.tt(aT[:, 8 + kt, c0:c0+cs], o2[i][:, :cs], t1[:, :cs], ALU.mult)
    kb.release(mk)
    segs = [(0, NLAT, 2, 3), (NLAT, NT - NLAT, 4, 5)]
    back_tail(kb, c, aT, 16, wout_d, xT_d, x1T_d, h2T_d, cw_d, vt, wr_d, NT, segs, [0, 1], 6)
    kb.finish()
    return kb

def np_gelu(x): return 0.5 * x * (1 + np.tanh(math.sqrt(2 / math.pi) * (x + 0.044715 * x ** 3)))
def np_sig(x): return 1 / (1 + np.exp(-x))

def ref_routing(h, wr):
    lg = h @ wr
    gl = lg[:, :4]; gp = np.exp(gl - gl.max(-1, keepdims=True)); gp /= gp.sum(-1, keepdims=True)
    gi = gp.argmax(-1); ptop = gp.max(-1)
    el = lg[:, 4:].reshape(-1, 4, 8)[np.arange(len(h)), gi]
    order = np.argsort(-el, axis=-1)[:, :2]
    ev = np.take_along_axis(el, order, -1)
    w = np.exp(ev - ev.max(-1, keepdims=True)); w /= w.sum(-1, keepdims=True); w *= ptop[:, None]
    cw = np.zeros((len(h), 32))
    for k in range(2):
        cw[np.arange(len(h)), gi * 8 + order[:, k]] = w[:, k]
    return cw

def ref_back0(I, NLAT):
    f = lambda n: I[n].astype(np.float64).T
    vecs = I["vecs"].astype(np.float64); v8 = I["v8"].astype(np.float64)
    y = f("yfT") + f("ybT") + v8[:, 0] * f("uT")
    g = np_gelu(y); aS = g * np_sig(g @ I["wglu"].astype(np.float64))
    o = (f("ofT") + f("obT")).reshape(-1, 4, 256)
    o = o / np.sqrt((o * o).mean(-1, keepdims=True) + 1e-6)
    r = f("rT")
    aG = o.reshape(-1, 1024) * v8[:, 1] * (r * np_sig(r))
    a = np.concatenate([aS, aG], -1)
    ox = a @ I["wout"].astype(np.float64)
    x = f("xT"); NT = x.shape[0]
    g1 = np.where(np.arange(NT)[:, None] < NLAT, vecs[:, 0], vecs[:, 1])
    x1 = x + g1 * ox
    yn = x1 / np.sqrt((x1 * x1).mean(-1, keepdims=True) + 1e-6) * vecs[:, 6]
    sh = np.where(np.arange(NT)[:, None] < NLAT, vecs[:, 2], vecs[:, 4]); sc = np.where(np.arange(NT)[:, None] < NLAT, vecs[:, 3], vecs[:, 5])
    h2 = yn * (1 + sc) + sh
    return x1, h2, ref_routing(h2, I["wr"].astype(np.float64))


TWO_PI = 2 * math.pi

def sin_rr(kb, out, ang, shift, shape, wk):
    a, n, m = wk['a'], wk['n'], wk['m']
    sl = tuple(slice(0, s) for s in shape)
    A, N, M = a[sl], n[sl], m[sl]
    kb.ts(A, ang, 1.0 / TWO_PI, ALU.mult, shift / TWO_PI, ALU.add)
    kb.copy(N, A)
    kb.copy(M, N)
    kb.tt(A, A, M, ALU.subtract)
    kb.ts(M, A, 0.5, ALU.is_gt)
    kb.tt(A, A, M, ALU.subtract)
    kb.ts(M, A, -0.5, ALU.is_lt)
    kb.tt(A, A, M, ALU.add)
    kb.ts(A, A, 0.4999999, ALU.min, -0.4999999, ALU.max)
    kb.act(out, A, AF.Sin, scale=TWO_PI)

def s5_part(kb, uT_d, prm_d, bT_d, cT_d, yT_d, L, NGP=32, T=512):
    prm = kb.sb([128, NGP, 3], name="s5prm"); kb.dma(prm.all, prm_d)
    bT = kb.sb([32, NGP, 2, 128], name="s5bT"); kb.dma(bT.all, bT_d)
    cT = kb.sb([128, NGP, 2, 32], name="s5cT"); kb.dma(cT.all, cT_d)
    kb.ts(cT[:, :, 1, :], cT[:, :, 1, :], -1.0, ALU.mult)
    def sm(name): return kb.sb([128, NGP], name="s5_" + name)
    lr, dt, mag, th, sn, cs = sm("lr"), sm("dt"), sm("mag"), sm("th"), sm("sn"), sm("cs")
    are, aim, den, fre, fim, nfre, t1, t2 = sm("are"), sm("aim"), sm("den"), sm("fre"), sm("fim"), sm("nfre"), sm("t1"), sm("t2")
    wk_s = {'a': kb.sb([128, NGP], name="wka"), 'n': kb.sb([128, NGP], I32, name="wkn"), 'm': kb.sb([128, NGP], name="wkm")}
    kb.ts(lr.all, prm[:, :, 0], -1e-4, ALU.min)
    kb.act(dt.all, prm[:, :, 2], AF.Exp)
    kb.tt(t1.all, lr.all, dt.all, ALU.mult)
    kb.act(mag.all, t1.all, AF.Exp)
    kb.tt(th.all, prm[:, :, 1], dt.all, ALU.mult)
    sin_rr(kb, sn.all, th.all, 0.0, (128, NGP), wk_s)
    sin_rr(kb, cs.all, th.all, math.pi / 2, (128, NGP), wk_s)
    kb.tt(are.all, mag.all, cs.all, ALU.mult)
    kb.tt(aim.all, mag.all, sn.all, ALU.mult)
    kb.ts(are.all, are.all, -1.0, ALU.add)
    li = prm[:, :, 1]
    kb.tt(den.all, lr.all, lr.all, ALU.mult)
    kb.tt(t1.all, li, li, ALU.mult)
    kb.tt(den.all, den.all, t1.all, ALU.add)
    kb.recip(den.all, den.all)
    kb.tt(t1.all, are.all, lr.all, ALU.mult)
    kb.tt(t2.all, aim.all, li, ALU.mult)
    kb.tt(t1.all, t1.all, t2.all, ALU.add)
    kb.tt(fre.all, t1.all, den.all, ALU.mult)
    kb.tt(t1.all, aim.all, lr.all, ALU.mult)
    kb.tt(t2.all, are.all, li, ALU.mult)
    kb.tt(t1.all, t1.all, t2.all, ALU.subtract)
    kb.tt(fim.all, t1.all, den.all, ALU.mult)
    kb.ts(nfre.all, fre.all, -1.0, ALU.mult)
    taui = kb.sb([128, T], I32, name="taui")
    kb.iota(taui.all, [[1, T]], base=1, channel_multiplier=0)
    tau = kb.sb([128, T], name="tau"); kb.copy(tau.all, taui.all)
    ones = kb.sb([128, T], name="onesT"); kb.memset(ones.all, 1.0)
    def big(name, dt_=F32): return kb.sb([128, T], dt_, name="s5_" + name)
    ang, c, s, wr, wi, rt = big("ang"), big("c"), big("s"), big("wr"), big("wi"), big("rt")
    wk = {'a': big("wa"), 'n': big("wn", I32), 'm': big("wm")}
    bre, bim = big("bre"), big("bim")
    p1, p2, p3, p4 = big("p1"), big("p2"), big("p3"), big("p4")
    xre, xim = big("xre"), big("xim")
    kre, kim = big("kre"), big("kim")
    hre = [big("hre0"), big("hre1")]; him = [big("him0"), big("him1")]
    psA = [kb.ps([128, 512], name=f"s5A{i}") for i in range(2)]
    psB = [kb.ps([128, 512], name=f"s5B{i}") for i in range(2)]
    psY = [kb.ps([32, 512], name=f"s5Y{i}") for i in range(2)]
    ut = [kb.sb([32, L], name=f"s5u{i}") for i in range(2)]
    ysb = [kb.sb([32, L], name=f"s5y{i}") for i in range(2)]
    tl = chunks(L, T)
    unit = 0
    for gp in range(NGP):
        u = ut[gp % 2]; yo = ysb[gp % 2]
        kb.dma(u.all, uT_d[gp*32:(gp+1)*32, :])
        kb.ts(ang.all, tau.all, th[:, gp:gp+1], ALU.mult)
        sin_rr(kb, s.all, ang.all, 0.0, (128, T), wk)
        sin_rr(kb, c.all, ang.all, math.pi / 2, (128, T), wk)
        kb.ts(wk['a'].all, c.all, fre[:, gp:gp+1], ALU.mult)
        kb.stt(wr.all, s.all, fim[:, gp:gp+1], wk['a'].all, ALU.mult, ALU.add)
        kb.ts(wk['m'].all, c.all, fim[:, gp:gp+1], ALU.mult)
        kb.stt(wi.all, s.all, nfre[:, gp:gp+1], wk['m'].all, ALU.mult, ALU.add)
        kb.ts(rt.all, ones.all, mag[:, gp:gp+1], ALU.mult)
        for ti, (t0, ts_) in enumerate(tl):
            A = psA[unit % 2]; B = psB[unit % 2]; Y = psY[unit % 2]; unit += 1
            kb.mm(A[:, :ts_], bT[:, gp, 0, :], u[:, t0:t0+ts_])
            kb.mm(B[:, :ts_], bT[:, gp, 1, :], u[:, t0:t0+ts_])
            kb.copy(bre[:, :ts_], A[:, :ts_], e='act')
            kb.copy(bim[:, :ts_], B[:, :ts_], e='act')
            kb.tt(p1[:, :ts_], bre[:, :ts_], wr[:, :ts_], ALU.mult, e='pool')
            kb.tt(p2[:, :ts_], bim[:, :ts_], wi[:, :ts_], ALU.mult, e='pool')
            kb.tt(p3[:, :ts_], bre[:, :ts_], wi[:, :ts_], ALU.mult, e='pool')
            kb.tt(p4[:, :ts_], bim[:, :ts_], wr[:, :ts_], ALU.mult, e='pool')
            kb.tt(xre[:, :ts_], p1[:, :ts_], p2[:, :ts_], ALU.subtract)
            kb.tt(xim[:, :ts_], p3[:, :ts_], p4[:, :ts_], ALU.add)
            if ti == 0:
                ir, ii = 0.0, 0.0
            else:
                pt = tl[ti-1][1]
                ir = hre[(ti-1) % 2][:, pt-1:pt]; ii = him[(ti-1) % 2][:, pt-1:pt]
            kb.scan(kre[:, :ts_], rt[:, :ts_], xre[:, :ts_], ir)
            kb.scan(kim[:, :ts_], rt[:, :ts_], xim[:, :ts_], ii)
            kb.tt(p1[:, :ts_], kre[:, :ts_], c[:, :ts_], ALU.mult, e='pool')
            kb.tt(p2[:, :ts_], kim[:, :ts_], s[:, :ts_], ALU.mult, e='pool')
            kb.tt(p3[:, :ts_], kre[:, :ts_], s[:, :ts_], ALU.mult, e='pool')
            kb.tt(p4[:, :ts_], kim[:, :ts_], c[:, :ts_], ALU.mult, e='pool')
            hr = hre[ti % 2]; hi = him[ti % 2]
            kb.tt(hr[:, :ts_], p1[:, :ts_], p2[:, :ts_], ALU.subtract)
            kb.tt(hi[:, :ts_], p3[:, :ts_], p4[:, :ts_], ALU.add)
            kb.mm(Y[:, :ts_], cT[:, gp, 0, :], hr[:, :ts_], start=True, stop=False)
            kb.mm(Y[:, :ts_], cT[:, gp, 1, :], hi[:, :ts_], start=False, stop=True)
            kb.copy(yo[:, t0:t0+ts_], Y[:, :ts_], e='act')
        kb.dma(yT_d[gp*32:(gp+1)*32, :], yo.all, is_output=True)

def s5_host_params(lam_re, lam_im, log_dt, b_re, b_im, c_re, c_im, NGP=32):
    G = NGP * 2
    P, I = 64, 16
    def gpl(a):
        return np.ascontiguousarray(a.reshape(NGP, 2, P).transpose(1, 2, 0).reshape(128, NGP))
    prm = np.stack([gpl(lam_re), gpl(lam_im), gpl(np.repeat(log_dt[:, None], P, axis=1))], axis=-1).astype(np.float32)
    bT = np.zeros((32, NGP, 2, 128), np.float32)
    cT = np.zeros((128, NGP, 2, 32), np.float32)
    for k, (br, cr) in enumerate([(b_re, c_re), (b_im, c_im)]):
        brr = br.reshape(NGP, 2, P, I); crr = cr.reshape(NGP, 2, I, P)
        for g2 in range(2):
            bT[g2*16:(g2+1)*16, :, k, g2*64:(g2+1)*64] = brr[:, g2].transpose(2, 0, 1)
            cT[g2*64:(g2+1)*64, :, k, g2*16:(g2+1)*16] = crr[:, g2].transpose(2, 0, 1)
    return prm, bT, cT

def s5_ref(u, lam_re, lam_im, log_dt, b_re, b_im, c_re, c_im):
    lr = np.minimum(lam_re.astype(np.float64), -1e-4); li = lam_im.astype(np.float64)
    dt = np.exp(log_dt.astype(np.float64))[:, None]
    lam = lr + 1j * li
    abar = np.exp(lam * dt)
    f = (abar - 1) / lam
    B = b_re.astype(np.float64) + 1j * b_im
    C = c_re.astype(np.float64) + 1j * c_im
    L = u.shape[0]
    x = f[None] * np.einsum('lgi,gpi->lgp', u, B)
    h = np.zeros_like(x[0]); ys = []
    for t in range(L):
        h = abar * h + x[t]
        ys.append(np.einsum('gp,gip->gi', h, C).real)
    return np.stack(ys)


def gla_part(kb, qT_d, kT_d, v_d, lrT_d, wg_d, bg_d, rc_d, rs_d, cmask_d, ident_d, o_d, L, T=512):
    H = 4
    cmask = kb.sb([64, 64], name="cmask"); kb.dma(cmask.all, cmask_d)
    identf = kb.sb([128, 128], name="identf"); kb.dma(identf.all, ident_d)
    ident = kb.sb([128, 128], BF16, name="ident"); kb.copy(ident.all, identf.all)
    wg = kb.sb([16, 512], name="wg"); kb.dma(wg.all, wg_d)
    bg = kb.sb([128, 4], name="bg"); kb.dma(bg.all, bg_d)
    rm = kb.sb([128, T], name="rm"); kb.memset(rm.all, 1.0)
    kb.memset(rm[:, 0:T:64], 0.0)
    def big(name, dt_=F32, n=4): return kb.sb([128, n, T], dt_, name="g_" + name)
    q32, qsw, k32, ksw = big("q32"), big("qsw"), big("k32"), big("ksw")
    rc = kb.sb([128, T], name="g_rc"); rs = kb.sb([128, T], name="g_rs")
    lr = kb.sb([16, T], name="g_lr")
    t1, t2 = big("t1"), big("t2")
    la, b16, eb, enb = big("la"), big("b16"), big("eb"), big("enb")
    qinb, kinb = big("qinb", BF16), big("kinb", BF16)
    kin32 = big("kin32")
    S32 = kb.sb([128, 4, 256], name="g_S32"); kb.memset(S32.all, 0.0)
    Sb = kb.sb([128, 4, 256], BF16, name="g_Sb"); kb.memset(Sb.all, 0.0)
    vst = [kb.sb([64, 1024], name=f"g_vst{i}") for i in range(2)]
    vb = [kb.sb([64, 1024], BF16, name=f"g_vb{i}") for i in range(2)]
    osb = [kb.sb([64, 1024], name=f"g_osb{i}") for i in range(2)]
    attb = [kb.sb([64, 64], BF16, name=f"g_attb{i}") for i in range(2)]
    kout = [kb.sb([128, 64], BF16, name=f"g_kout{i}") for i in range(2)]
    koT = [kb.sb([64, 128], BF16, name=f"g_koT{i}") for i in range(2)]
    pAtt = [kb.ps([64, 64], name=f"g_pAtt{i}") for i in range(2)]
    pKo = [kb.ps([64, 128], BF16, name=f"g_pKo{i}") for i in range(2)]
    pO = kb.ps([64, 1024], name="g_pO")
    pU = kb.ps([128, 256], name="g_pU")
    pZ = kb.ps([128, T], name="g_pZ")
    qv = qT_d.rearrange("(h p) t -> p h t", p=128)
    kv = kT_d.rearrange("(h p) t -> p h t", p=128)
    scale = 128 ** -0.5
    cn = 0
    for (t0, ts_) in chunks(L, T):
        kb.dma(q32[:, :, :ts_], qv[:, :, t0:t0+ts_])
        kb.dma(k32[:, :, :ts_], kv[:, :, t0:t0+ts_])
        for blk in range(4):
            src = blk ^ 1
            kb.dma(qsw[blk*32:(blk+1)*32, :, :ts_], qv[src*32:(src+1)*32, :, t0:t0+ts_])
            kb.dma(ksw[blk*32:(blk+1)*32, :, :ts_], kv[src*32:(src+1)*32, :, t0:t0+ts_])
        kb.dma(rc[:, :ts_], rc_d[:, t0:t0+ts_]); kb.dma(rs[:, :ts_], rs_d[:, t0:t0+ts_])
        kb.dma(lr[:, :ts_], lrT_d[:, t0:t0+ts_])
        for h in range(H):
            kb.mm(pZ[:, :ts_], wg[:, h*128:(h+1)*128], lr[:, :ts_])
            kb.act(la[:, h, :ts_], pZ[:, :ts_], AF.Sigmoid, bias=bg[:, h:h+1])
        for h in range(H):
            kb.act(la[:, h, :ts_], la[:, h, :ts_], AF.Ln)
            kb.scan(b16[:, h, :ts_], rm[:, :ts_], la[:, h, :ts_], 0.0)
        for h in range(H):
            kb.act(eb[:, h, :ts_], b16[:, h, :ts_], AF.Exp, scale=1.0 / 16)
            kb.act(enb[:, h, :ts_], b16[:, h, :ts_], AF.Exp, scale=-1.0 / 16)
        for h in range(H):
            kb.tt(t1[:, h, :ts_], q32[:, h, :ts_], rc[:, :ts_], ALU.mult)
            kb.tt(t2[:, h, :ts_], qsw[:, h, :ts_], rs[:, :ts_], ALU.mult, e='pool')
            kb.tt(t1[:, h, :ts_], t1[:, h, :ts_], t2[:, h, :ts_], ALU.add)
            kb.stt(qinb[:, h, :ts_], t1[:, h, :ts_], scale, eb[:, h, :ts_], ALU.mult, ALU.mult)
        for h in range(H):
            kb.tt(t1[:, h, :ts_], k32[:, h, :ts_], rc[:, :ts_], ALU.mult)
            kb.tt(t2[:, h, :ts_], ksw[:, h, :ts_], rs[:, :ts_], ALU.mult, e='pool')
            kb.tt(t1[:, h, :ts_], t1[:, h, :ts_], t2[:, h, :ts_], ALU.add)
            kb.tt(kin32[:, h, :ts_], t1[:, h, :ts_], enb[:, h, :ts_], ALU.mult)
            kb.copy(kinb[:, h, :ts_], kin32[:, h, :ts_], e='pool')
        for c in range(ts_ // 64):
            c0 = c * 64
            vs, vbb, ob = vst[cn % 2], vb[cn % 2], osb[cn % 2]
            kb.dma(vs.all, v_d[t0+c0:t0+c0+64, :])
            kb.copy(vbb.all, vs.all, e='pool')
            for h in range(H):
                i2 = (cn * 4 + h) % 2
                dec = eb[:, h, c0+63:c0+64]
                kb.mm(pAtt[i2].all, kinb[:, h, c0:c0+64], qinb[:, h, c0:c0+64])
                kb.tt(attb[i2].all, pAtt[i2].all, cmask.all, ALU.mult)
                kb.ts(kout[i2].all, kin32[:, h, c0:c0+64], dec, ALU.mult, e='pool')
                kb.transpose(pKo[i2].all, kout[i2].all, ident.all)
                kb.copy(koT[i2].all, pKo[i2].all, e='act')
                kb.mm(pO[:, h*256:(h+1)*256], attb[i2].all, vbb[:, h*256:(h+1)*256], start=True, stop=False)
                kb.mm(pO[:, h*256:(h+1)*256], qinb[:, h, c0:c0+64], Sb[:, h, :], start=False, stop=True)
                kb.mm(pU.all, koT[i2].all, vbb[:, h*256:(h+1)*256])
                kb.stt(S32[:, h, :], S32[:, h, :], dec, pU.all, ALU.mult, ALU.add)
                kb.copy(Sb[:, h, :], S32[:, h, :], e='act')
            kb.copy(ob[:, 0:512], pO[:, 0:512], e='act')
            kb.copy(ob[:, 512:1024], pO[:, 512:1024], e='dve')
            kb.dma(o_d[t0+c0:t0+c0+64, :], ob.all, is_output=True)
            cn += 1

def rope_tables(pos_row, pos_col, is_ctx):
    nf = 32
    inv = (10000.0 ** (-np.arange(nf, dtype=np.float32) / nf)).astype(np.float32)
    L = len(pos_row)
    C = np.ones((128, L), np.float32); S = np.zeros((128, L), np.float32)
    for half, pos in enumerate([pos_row, pos_col]):
        ang = pos.astype(np.float32)[None, :] * inv[:, None]
        c = np.cos(ang).astype(np.float32); s = np.sin(ang).astype(np.float32)
        C[half*64:half*64+32] = c; C[half*64+32:half*64+64] = c
        S[half*64:half*64+32] = -s; S[half*64+32:half*64+64] = s
    C[:, is_ctx] = 1.0; S[:, is_ctx] = 0.0
    return C, S

def gla_ref(q, k, v, lowrank, wg, bg, C, S):
    L = q.shape[0]
    def rope(z):
        sw = z.reshape(L, 4, 2, 2, 32)[:, :, :, ::-1, :].reshape(L, 4, 128)
        return z * C.T[:, None, :] + sw * S.T[:, None, :]
    q = rope(q) * 128 ** -0.5; k = rope(k)
    z = lowrank @ wg + bg
    la = -np.logaddexp(0, -z) / 16.0
    a = np.exp(la).reshape(L, 4, 128)
    St = np.zeros((4, 128, 256)); o = np.zeros((L, 4, 256))
    for t in range(L):
        St = a[t][:, :, None] * St + k[t][:, :, None] * v[t][:, None, :]
        o[t] = np.einsum('hd,hde->he', q[t], St)
    return o


def build_moe(NTOK=8704, NE=8, F=1024, D=2048):
    kb = KB()
    KT = D // 128; FT = F // 128
    hT_d = kb.dram_in("hT", [D, NTOK], BF16)
    cwT_d = kb.dram_in("cwT", [NE, NTOK])
    wg_d = kb.dram_in("wg", [NE, D, F]); wu_d = kb.dram_in("wu", [NE, D, F]); wd_d = kb.dram_in("wd", [NE, F, D])
    yT_d = kb.dram_out("yT", [D, NTOK])
    hv = hT_d.rearrange("(kt p) t -> p kt t", p=128)
    yv = yT_d.rearrange("(kt p) t -> p kt t", p=128)
    hT = [kb.sb([128, KT, 512], BF16, name=f"m_hT{i}") for i in range(2)]
    aT = kb.sb([128, NE * FT, 512], BF16, name="m_aT")
    cwb = [kb.sb([128, 512], name=f"m_cwb{i}") for i in range(2)]
    wgs = [kb.sb([128, KT, 128], name=f"m_wgs{i}") for i in range(2)]
    wus = [kb.sb([128, KT, 128], name=f"m_wus{i}") for i in range(2)]
    wgb = [kb.sb([128, KT, 128], BF16, name=f"m_wgb{i}") for i in range(2)]
    wub = [kb.sb([128, KT, 128], BF16, name=f"m_wub{i}") for i in range(2)]
    wds = [kb.sb([128, 512], name=f"m_wds{i}") for i in range(3)]
    wdb = [kb.sb([128, 512], BF16, name=f"m_wdb{i}") for i in range(3)]
    sg = [kb.sb([128, 512], name=f"m_sg{i}") for i in range(2)]
    t1 = [kb.sb([128, 512], name=f"m_t1{i}") for i in range(2)]
    ysb = [kb.sb([128, 512], name=f"m_ysb{i}") for i in range(2)]
    pG = [kb.ps([128, 512], name=f"m_pG{i}") for i in range(2)]
    pU = [kb.ps([128, 512], name=f"m_pU{i}") for i in range(2)]
    pY = [kb.ps([128, 512], name=f"m_pY{i}") for i in range(4)]
    n = 0; nd = 0; ny = 0
    for ci, (c0, cs) in enumerate(chunks(NTOK)):
        h = hT[ci % 2]
        kb.dma(h[:, :, :cs], hv[:, :, c0:c0+cs])
        for e in range(NE):
            cw = cwb[e % 2]
            kb.dma(cw[:, :cs], cwT_d[e:e+1, c0:c0+cs].broadcast_to([128, cs]))
            for f in range(FT):
                i = n % 2; n += 1
                kb.dma(wgs[i].all, wg_d[e, :, f*128:(f+1)*128].rearrange("(kt p) n -> p kt n", p=128))
                kb.dma(wus[i].all, wu_d[e, :, f*128:(f+1)*128].rearrange("(kt p) n -> p kt n", p=128))
                kb.copy(wgb[i].all, wgs[i].all, e='pool')
                kb.copy(wub[i].all, wus[i].all, e='pool')
                for kt in range(KT):
                    kb.mm(pG[i][:, :cs], wgb[i][:, kt, :], h[:, kt, :cs], start=(kt == 0), stop=(kt == KT-1))
                for kt in range(KT):
                    kb.mm(pU[i][:, :cs], wub[i][:, kt, :], h[:, kt, :cs], start=(kt == 0), stop=(kt == KT-1))
                kb.act(sg[i][:, :cs], pG[i][:, :cs], AF.Silu)
                kb.tt(t1[i][:, :cs], sg[i][:, :cs], pU[i][:, :cs], ALU.mult)
                kb.tt(aT[:, e*FT+f, :cs], t1[i][:, :cs], cw[:, :cs], ALU.mult)
        for dq in range(D // 512):
            for ef in range(NE * FT):
                e, f = ef // FT, ef % FT
                j = nd % 3; nd += 1
                kb.dma(wds[j].all, wd_d[e, f*128:(f+1)*128, dq*512:(dq+1)*512])
                kb.copy(wdb[j].all, wds[j].all, e='act' if nd % 2 else 'pool')
                for dd in range(4):
                    kb.mm(pY[dd][:, :cs], wdb[j][:, dd*128:(dd+1)*128], aT[:, ef, :cs], start=(ef == 0), stop=(ef == NE*FT-1))
            for dd in range(4):
                yo = ysb[ny % 2]; ny += 1
                kb.copy(yo[:, :cs], pY[dd][:, :cs], e='dve')
                kb.dma(yv[:, dq*4+dd, c0:c0+cs], yo[:, :cs], is_output=True)
    kb.finish()
    return kb

def np_silu(x): return x / (1 + np.exp(-x))


NEGM = -1.0e4

def qrows_for_keyrow(rp, rows=64, kh=8):
    res = []
    for r in range(rows):
        st = min(max(r - kh // 2, 0), rows - kh)
        if st <= rp <= st + kh - 1:
            res.append(r)
    return res

def start_of(r, rows=64, kh=8):
    return min(max(r - kh // 2, 0), rows - kh)

def build_attn(NH=8):
    kb = KB()
    W = 64; ROWS = 64; LL = 4096; LC = 256
    qT_d = kb.dram_in("qT", [NH, 128, LL], BF16)
    kT_d = kb.dram_in("kT", [NH, 128, LL + LC], BF16)
    v_d = kb.dram_in("v", [LL + LC, NH, 128], BF16)
    bias_d = kb.dram_in("biasT", [NH, 64, 15 * 64])
    o_d = kb.dram_out("o", [LL, NH * 128], BF16)
    scale = 128 ** -0.5
    qT = [kb.sb([128, LL], BF16, name=f"a_qT{i}") for i in range(2)]
    kT = [kb.sb([128, LL + LC], BF16, name=f"a_kT{i}") for i in range(2)]
    Vl = [kb.sb([64, ROWS, 129], BF16, name=f"a_Vl{i}") for i in range(2)]
    Vc = [kb.sb([128, 2, 129], BF16, name=f"a_Vc{i}") for i in range(2)]
    bias = [kb.sb([64, 15 * 64], name=f"a_bias{i}") for i in range(2)]
    for i in range(2):
        kb.memset(Vl[i][:, :, 128:129], 1.0); kb.memset(Vc[i][:, :, 128:129], 1.0)
    RING = 16
    PT = [kb.sb([64, 15 * 64], BF16, name=f"a_PT{i}") for i in range(RING)]
    PcT = [[kb.sb([128, 512], BF16, name=f"a_Pc{g}_{ct}") for ct in range(2)] for g in range(2)]
    tmp = [kb.sb([64, 512], name=f"a_tmp{i}") for i in range(2)]
    osb = [kb.sb([64, 8, 128], BF16, name=f"a_osb{i}") for i in range(2)]
    rec = [kb.sb([64, 1], name=f"a_rec{i}") for i in range(2)]
    pS = [kb.ps([64, 512], name=f"a_pS{i}") for i in range(2)]
    pC = [kb.ps([128, 512], name=f"a_pC{i}") for i in range(2)]
    pO = [kb.ps([64, 129], name=f"a_pO{i}") for i in range(2)]
    vlat = v_d[0:LL].rearrange("(r c) h d -> c r h d", c=64)
    vctx = v_d[LL:LL + LC].rearrange("(t p) h d -> p t h d", p=128)
    ov = o_d.rearrange("(r c) f -> c r f", c=64)
    ns = 0; no = 0
    for h in range(NH):
        b = h % 2
        kb.dma(qT[b].all, qT_d[h]); kb.dma(kT[b].all, kT_d[h])
        kb.dma(Vl[b][:, :, 0:128], vlat[:, :, h, :]); kb.dma(Vc[b][:, :, 0:128], vctx[:, :, h, :])
        kb.dma(bias[b].all, bias_d[h])
        done_ctx = set()
        for rp in range(ROWS):
            qr = qrows_for_keyrow(rp)
            lo, hi = qr[0], qr[-1]
            ptile = PT[rp % RING]
            for p0 in range(lo, hi + 1, 8):
                p1 = min(p0 + 8, hi + 1); n = p1 - p0
                S = pS[ns % 2]; t = tmp[ns % 2]; ns += 1
                kb.mm(S[:, :n*64], kT[b][:, rp*64:(rp+1)*64], qT[b][:, p0*64:p1*64])
                j0 = p0 - rp + 7
                kb.stt(t[:, :n*64], S[:, :n*64], scale, bias[b][:, j0*64:(j0+n)*64], ALU.mult, ALU.add)
                kb.act(ptile[:, j0*64:(j0+n)*64], t[:, :n*64], AF.Exp)
            for r in range(ROWS):
                st = start_of(r)
                if st + 7 != rp:
                    continue
                g = r // 8
                if g not in done_ctx:
                    done_ctx.add(g)
                    for ct in range(2):
                        C = pC[ct]
                        kb.mm(C.all, kT[b][:, LL+ct*128:LL+(ct+1)*128], qT[b][:, g*512:(g+1)*512])
                        kb.act(PcT[g % 2][ct].all, C.all, AF.Exp, scale=scale)
                O = pO[no % 2]; rc = rec[no % 2]; no += 1
                for i, rk in enumerate(range(st, st + 8)):
                    j = r - rk + 7
                    kb.mm(O.all, PT[rk % RING][:, j*64:(j+1)*64], Vl[b][:, rk, :], start=(i == 0), stop=False)
                for ct in range(2):
                    kb.mm(O.all, PcT[g % 2][ct][:, (r % 8)*64:(r % 8 + 1)*64], Vc[b][:, ct, :], start=False, stop=(ct == 1))
                kb.recip(rc.all, O[:, 128:129])
                ob = osb[g % 2]
                kb.ts(ob[:, r % 8, :], O[:, 0:128], rc[:, 0:1], ALU.mult)
                if r % 8 == 7:
                    kb.dma(ov[:, g*8:(g+1)*8, h*128:(h+1)*128], ob.all, is_output=True)
    kb.finish()
    return kb

def host_bias(rpb):
    H = rpb.shape[0]
    col = np.arange(64)
    cs = np.clip(col - 8, 0, 48)
    col_ok = (col[None, :] >= cs[:, None]) & (col[None, :] < cs[:, None] + 16)
    dc = np.clip(col[None, :] - col[:, None] + 15, 0, 30)
    out = np.empty((H, 64, 15, 64), np.float32)
    for j in range(15):
        dr = 14 - j
        bq = rpb[:, dr, :][:, dc]
        bq = np.where(col_ok[None], bq, np.float32(NEGM))
        out[:, :, j, :] = bq.transpose(0, 2, 1)
    return out.reshape(H, 64, 15 * 64)

def attn_ref(q, k, v, kc, vc, rpb):
    scale = 128 ** -0.5
    out = np.zeros((4096, 128))
    col = np.arange(64); cs = np.clip(col - 8, 0, 48)
    col_ok = (col[None, :] >= cs[:, None]) & (col[None, :] < cs[:, None] + 16)
    dc = np.clip(col[None, :] - col[:, None] + 15, 0, 30)
    for r in range(64):
        st = start_of(r)
        qr = q[r*64:(r+1)*64]
        kb_ = k[st*64:(st+8)*64]; vb = v[st*64:(st+8)*64]
        dr = st + np.arange(8) - r + 7
        bias = rpb[dr][:, dc]
        bias = np.where(col_ok[None], bias, -1e30).transpose(1, 0, 2).reshape(64, 512)
        s = np.concatenate([qr @ kb_.T * scale + bias, qr @ kc.T * scale], -1)
        p = np.exp(s - s.max(-1, keepdims=True)); p /= p.sum(-1, keepdims=True)
        out[r*64:(r+1)*64] = p[:, :512] @ vb + p[:, 512:] @ vc
    return out


def combine_y(kb, x_d, yg_d, out_tr, outv, vt, NT, segs_g2, ngroups=4, tag="cy"):
    xv = x_d.rearrange("(kt p) t -> p kt t", p=128)
    yv = yg_d.rearrange("g (kt p) t -> p g kt t", p=128)
    xt = [kb.sb([128, 512], name=f"{tag}_x{i}") for i in range(2)]
    yt = [kb.sb([128, ngroups, 512], name=f"{tag}_y{i}") for i in range(2)]
    n = 0
    for (s0, sz, g2c) in segs_g2:
        for (c0, cs) in chunks(sz):
            a = s0 + c0
            for kt in range(16):
                x, y = xt[n % 2], yt[n % 2]; n += 1
                kb.dma(x[:, :cs], xv[:, kt, a:a+cs])
                kb.dma(y[:, :, :cs], yv[:, :, kt, a:a+cs])
                kb.tt(y[:, 0, :cs], y[:, 0, :cs], y[:, 1, :cs], ALU.add)
                kb.tt(y[:, 2, :cs], y[:, 2, :cs], y[:, 3, :cs], ALU.add, e='pool')
                kb.tt(y[:, 0, :cs], y[:, 0, :cs], y[:, 2, :cs], ALU.add)
                kb.stt(x[:, :cs], y[:, 0, :cs], vt[:, kt, g2c:g2c+1], x[:, :cs], ALU.mult, ALU.add)
                kb.dma(V(out_tr.tile, outv[:, kt, a:a+cs]), x[:, :cs], is_output=True)

def build_front1(NT=2176, NLAT=2048):
    kb = KB()
    x1T_d = kb.dram_in("x1T", [2048, NT]); yg_d = kb.dram_in("ygT", [4, 2048, NT])
    vecs_d = kb.dram_in("vecs", [2048, 7]); qkg_d = kb.dram_in("qkg", [128, 2]); W_d = kb.dram_in("wqkv", [2048, 6144])
    x2T_d = kb.dram_out("x2T", [2048, NT])
    outs = [kb.dram_out(n, [2048, NT], BF16) for n in ["qT", "kT", "vT"]]
    c = consts(kb)
    vt = kb.sb([128, 16, 7], name="vecs_sb"); kb.dma(vt.all, vecs_d.rearrange("(kt p) v -> p kt v", p=128))
    qkg = kb.sb([128, 2], name="qkg_sb"); kb.dma(qkg.all, qkg_d)
    x2tr = dtrack(kb, x2T_d, "x2T_dram"); x2v = x2T_d.rearrange("(kt p) t -> p kt t", p=128)
    combine_y(kb, x1T_d, yg_d, x2tr, x2v, vt, NT, [(0, NLAT, 0), (NLAT, NT - NLAT, 1)])
    hT = kb.sb([128, 16, NT], BF16, name="hT")
    def x_src(kt, a, cs, dst):
        kb.dma(dst, V(x2tr.tile, x2v[:, kt, a:a+cs]))
    def sink(a, cs, h32):
        kb.copy(hT[:, :, a:a+cs], h32[:, :, :cs], e='pool')
    norm_mod2(kb, c, x_src, sink, vt, [(0, NLAT, 2, 3), (NLAT, NT - NLAT, 4, 5)], 6, tag="n1")
    ost = [kb.sb([128, NT], BF16, name=f"f1_ost{i}") for i in range(2)]
    sq = [kb.sb([128, 512], name=f"f1_sq{i}") for i in range(2)]
    rstd = [kb.sb([128, 512], name=f"f1_rstd{i}") for i in range(2)]
    pN = kb.ps([128, 512], name="f1_pN")
    st = {'n': 0}
    def evac(j, nsz, c0, cs, pv):
        o = ost[j % 2]; i = st['n'] % 2; st['n'] += 1
        which = j // 16
        if which < 2:
            kb.act(sq[i][:, :cs], pv, AF.Square)
            kb.mm(pN[:, :cs], c['ones'].all, sq[i][:, :cs])
            kb.act(rstd[i][:, :cs], pN[:, :cs], AF.Sqrt, bias=c['eps'].all, scale=1.0 / 128)
            kb.recip(rstd[i][:, :cs], rstd[i][:, :cs])
            kb.stt(o[:, c0:c0+cs], pv, qkg[:, which:which+1], rstd[i][:, :cs], ALU.mult, ALU.mult)
        else:
            kb.copy(o[:, c0:c0+cs], pv, e='act')
        if c0 + cs == NT:
            jj = j % 16
            kb.dma(outs[which][jj*128:(jj+1)*128, :], o.all, is_output=True)
    proj(kb, hT, W_d, NT, 6144, 16, evac, tag="qkv")
    kb.finish()
    return kb

def build_back1(NT=2048):
    kb = KB()
    oT_d = kb.dram_in("oT", [2048, NT], BF16); xT_d = kb.dram_in("xT", [2048, NT])
    vecs_d = kb.dram_in("vecs", [2048, 4])
    wout_d = kb.dram_in("wout", [2048, 2048]); wr_d = kb.dram_in("wr", [2048, 36])
    x1T_d = kb.dram_out("x1T", [2048, NT]); h2T_d = kb.dram_out("h2T", [2048, NT], BF16); cw_d = kb.dram_out("cw", [NT, 32])
    c = consts(kb)
    vt = kb.sb([128, 16, 4], name="vecs_sb"); kb.dma(vt.all, vecs_d.rearrange("(kt p) v -> p kt v", p=128))
    aT = kb.sb([128, 16, NT], BF16, name="aT")
    ov = oT_d.rearrange("(kt p) t -> p kt t", p=128)
    for kt in range(16):
        kb.dma(aT[:, kt, :], ov[:, kt, :])
    back_tail(kb, c, aT, 16, wout_d, xT_d, x1T_d, h2T_d, cw_d, vt, wr_d, NT, [(0, NT, 1, 2)], [0], 3)
    kb.finish()
    return kb

def build_final(NT=2048):
    kb = KB()
    xT_d = kb.dram_in("xT", [2048, NT]); yg_d = kb.dram_in("ygT", [4, 2048, NT]); vecs_d = kb.dram_in("vecs", [128, 16])
    out_d = kb.dram_out("outT", [2048, NT])
    vt = kb.sb([128, 16, 1], name="vecs_sb"); kb.dma(vt[:, :, 0], vecs_d)
    otr = dtrack(kb, out_d, "out_dram"); ov = out_d.rearrange("(kt p) t -> p kt t", p=128)
    combine_y(kb, xT_d, yg_d, otr, ov, vt, NT, [(0, NT, 0)])
    kb.finish()
    return kb


def build_ada():
    kb = KB()
    NCOL = 1536
    cT = kb.dram_in("cT", [2048, 5])
    w = kb.dram_in("w", [2, 2048, NCOL])
    b = kb.dram_in("b", [2, 1, NCOL])
    out = kb.dram_out("mod", [2, 5, NCOL])
    ct = kb.sb([128, 16, 5]); st = kb.sb([128, 16, 5])
    kb.dma(ct.all, cT.rearrange("(kt p) m -> p kt m", p=128))
    kb.act(st.all, ct.all, AF.Silu)
    wpool = [kb.sb([128, NCOL], name=f"wp{i}") for i in range(4)]
    wi = 0
    for l in range(2):
        bt = kb.sb([5, NCOL])
        kb.dma(bt.all, b[l].broadcast_to([5, NCOL]))
        ot = kb.sb([5, NCOL])
        pss = [kb.ps([5, 512]) for _ in range(3)]
        for kt in range(16):
            wt = wpool[wi % 4]; wi += 1
            kb.dma(wt.all, w[l, kt*128:(kt+1)*128, :])
            for c in range(3):
                kb.mm(pss[c].all, st[:, kt, :], wt[:, c*512:(c+1)*512], start=(kt == 0), stop=(kt == 15))
        for c in range(3):
            kb.tt(ot[:, c*512:(c+1)*512], pss[c].all, bt[:, c*512:(c+1)*512], ALU.add)
        kb.dma(out[l], ot.all, is_output=True)
    kb.finish()
    return kb


import numpy as np

def build_s5(L=4352):
    kb = KB()
    uT_d = kb.dram_in("uT", [1024, L]); prm_d = kb.dram_in("prm", [128, 32, 3])
    bT_d = kb.dram_in("bT", [32, 32, 2, 128]); cT_d = kb.dram_in("cT", [128, 32, 2, 32])
    yT_d = kb.dram_out("yT", [1024, L])
    s5_part(kb, uT_d, prm_d, bT_d, cT_d, yT_d, L, 32)
    kb.finish()
    return kb

def build_gla(L=4352):
    kb = KB()
    qT_d = kb.dram_in("qT", [512, L]); kT_d = kb.dram_in("kT", [512, L]); v_d = kb.dram_in("v", [L, 1024])
    lrT_d = kb.dram_in("lrT", [16, L]); wg_d = kb.dram_in("wg", [16, 512]); bg_d = kb.dram_in("bg", [128, 4])
    rc_d = kb.dram_in("rc", [128, L]); rs_d = kb.dram_in("rs", [128, L])
    cm_d = kb.dram_in("cmask", [64, 64]); id_d = kb.dram_in("identm", [128, 128])
    o_d = kb.dram_out("o", [L, 1024])
    gla_part(kb, qT_d, kT_d, v_d, lrT_d, wg_d, bg_d, rc_d, rs_d, cm_d, id_d, o_d, L)
    kb.finish()
    return kb

def _c(a):
    return np.ascontiguousarray(a)

def kernel(x, c, ctx, c_ctx, ada_w, ada_b, norm1_g, norm2_g, ab_w_in, ab_w_out,
           s5_lam_re, s5_lam_im, s5_log_dt, s5_b_re, s5_b_im, s5_c_re, s5_c_im, s5_d, s5_w_glu,
           gla_w_gate2, gla_b_gate, gla_norm_g, na_w_qkv, na_w_out, na_q_norm, na_k_norm, na_rpb,
           moe_w_route_group, moe_w_route_expert, moe_w_gate, moe_w_up, moe_w_down):
    f32 = np.float32
    A = lambda a: np.asarray(a, dtype=f32) if np.asarray(a).dtype != f32 else np.asarray(a)
    x = A(x); c = A(c); ctx = A(ctx); c_ctx = A(c_ctx)
    B, SEQ, D = x.shape; CTX = ctx.shape[1]
    NC = 8; HL = SEQ // 2; HC = CTX // 2; NT = HL + HC
    cT = _c(np.concatenate([c.T, c_ctx[:, None]], axis=1))
    ada_w = A(ada_w); ada_b = A(ada_b)
    res = run(build_ada(), [{"cT": cT, "w": _c(ada_w[:, :, i*1536:(i+1)*1536]), "b": _c(ada_b[:, None, i*1536:(i+1)*1536])}
                            for i in range(NC)])
    mod = np.concatenate([r["mod"] for r in res], axis=2)
    def modv(l, k, row):
        return mod[l, row, k*D:(k+1)*D]
    cores = [(cc // 2, cc % 2) for cc in range(NC)]
    xT_core = []
    for (b, h) in cores:
        xT_core.append(_c(np.concatenate([x[b, h*HL:(h+1)*HL], ctx[b, h*HC:(h+1)*HC]], axis=0).T))
    w_in = A(ab_w_in)[0]
    ims = []
    for ci, (b, h) in enumerate(cores):
        vecs = np.stack([modv(0, 0, b), modv(0, 1, b), modv(0, 0, 4), modv(0, 1, 4), A(norm1_g)[0]], axis=1)
        ims.append({"xT": xT_core[ci], "vecs": _c(vecs), "W": w_in})
    res = run(build_front(NT, HL, 4128), ims)
    pT = [r["pT"] for r in res]
    px_lat = [np.concatenate([pT[2*b][:, :HL], pT[2*b+1][:, :HL]], axis=1) for b in range(B)]
    px_ctx = [np.concatenate([pT[2*b][:, HL:], pT[2*b+1][:, HL:]], axis=1) for b in range(B)]
    def seq_T(b, d, rows):
        cpart = px_ctx[b][rows]; lpart = px_lat[b][rows]
        if d == 1:
            cpart = cpart[:, ::-1]; lpart = lpart[:, ::-1]
        return np.concatenate([cpart, lpart], axis=1)
    L = CTX + SEQ
    ims = []
    for cc in range(NC):
        b, d = cc // 2, cc % 2
        prm, bT, cTt = s5_host_params(A(s5_lam_re)[0, d], A(s5_lam_im)[0, d], A(s5_log_dt)[0, d], A(s5_b_re)[0, d], A(s5_b_im)[0, d],
                                      A(s5_c_re)[0, d], A(s5_c_im)[0, d], NGP=32)
        ims.append({"uT": _c(seq_T(b, d, slice(0, 1024))), "prm": prm, "bT": bT, "cT": cTt})
    res = run(build_s5(L), ims)
    def unflip(a, d):
        cpart, lpart = a[:, :CTX], a[:, CTX:]
        if d == 1:
            cpart = cpart[:, ::-1]; lpart = lpart[:, ::-1]
        return cpart, lpart
    yS = [[unflip(res[2*b+d]["yT"], d) for d in range(2)] for b in range(B)]
    o0 = 1024
    pos = np.arange(SEQ)
    cmask = np.triu(np.ones((64, 64), f32)); identm = np.eye(128, dtype=f32)
    ims = []
    for cc in range(NC):
        b, d = cc // 2, cc % 2
        p_lat = pos[::-1] if d == 1 else pos
        prow = np.concatenate([np.zeros(CTX, np.int64), p_lat // 64]); pcol = np.concatenate([np.zeros(CTX, np.int64), p_lat % 64])
        is_ctx = np.arange(L) < CTX
        rc, rs = rope_tables(prow, pcol, is_ctx)
        ims.append({"qT": _c(seq_T(b, d, slice(o0, o0 + 512))), "kT": _c(seq_T(b, d, slice(o0 + 512, o0 + 1024))),
                    "v": _c(seq_T(b, d, slice(o0 + 1024, o0 + 2048)).T),
                    "lrT": _c(seq_T(b, d, slice(o0 + 3072 + d*16, o0 + 3072 + (d+1)*16))),
                    "wg": _c(A(gla_w_gate2)[0, d]), "bg": _c(A(gla_b_gate)[0, d].reshape(4, 128).T),
                    "rc": rc, "rs": rs, "cmask": cmask, "identm": identm})
    res = run(build_gla(L), ims)
    oG = [[unflip(_c(res[2*b+d]["o"].T), d) for d in range(2)] for b in range(B)]
    def tokpart(pair, h):
        return np.concatenate([pair[1][:, h*HL:(h+1)*HL], pair[0][:, h*HC:(h+1)*HC]], axis=1)
    wr0 = _c(np.concatenate([A(moe_w_route_group)[0], A(moe_w_route_expert)[0]], axis=1))
    v8 = _c(np.stack([A(s5_d)[0], A(gla_norm_g)[0]], axis=1))
    ims = []
    for ci, (b, h) in enumerate(cores):
        vecs = np.stack([modv(0, 2, b), modv(0, 2, 4), modv(0, 3, b), modv(0, 4, b), modv(0, 3, 4), modv(0, 4, 4), A(norm2_g)[0]], axis=1)
        pxp = (px_ctx[b], px_lat[b])
        ims.append({"yfT": _c(tokpart(yS[b][0], h)), "ybT": _c(tokpart(yS[b][1], h)),
                    "uT": _c(tokpart((pxp[0][0:1024], pxp[1][0:1024]), h)),
                    "ofT": _c(tokpart(oG[b][0], h)), "obT": _c(tokpart(oG[b][1], h)),
                    "rT": _c(tokpart((pxp[0][o0+2048:o0+3072], pxp[1][o0+2048:o0+3072]), h)),
                    "xT": xT_core[ci], "vecs": _c(vecs), "v8": v8, "wglu": A(s5_w_glu)[0], "wout": A(ab_w_out)[0], "wr": wr0})
    res = run(build_back0(NT, HL), ims)
    x1T = [r["x1T"] for r in res]; h2T = [r["h2T"] for r in res]; cw = [r["cw"] for r in res]
    del pT, px_lat, px_ctx, yS, oG, ims
    def run_moe(layer, h2T, cw, ntok_core):
        NTOK = 4 * ntok_core
        ims = []
        for cc in range(NC):
            g, hh = cc // 2, cc % 2
            hT = _c(np.concatenate([h2T[4*hh + i] for i in range(4)], axis=1))
            cwT = _c(np.concatenate([cw[4*hh + i] for i in range(4)], axis=0)[:, g*8:(g+1)*8].T)
            ims.append({"hT": hT, "cwT": cwT, "wg": A(moe_w_gate)[layer, g*8:(g+1)*8], "wu": A(moe_w_up)[layer, g*8:(g+1)*8],
                        "wd": A(moe_w_down)[layer, g*8:(g+1)*8]})
        res = run(build_moe(NTOK, 8, 1024, 2048), ims)
        out = []
        for ci in range(NC):
            hh, i = ci // 4, ci % 4
            out.append(_c(np.stack([res[2*g + hh]["yT"][:, i*ntok_core:(i+1)*ntok_core] for g in range(4)], axis=0)))
        return out
    yg0 = run_moe(0, h2T, cw, NT)
    qkg = _c(np.stack([A(na_q_norm)[0], A(na_k_norm)[0]], axis=1))
    ims = []
    for ci, (b, h) in enumerate(cores):
        vecs = np.stack([modv(0, 5, b), modv(0, 5, 4), modv(1, 0, b), modv(1, 1, b), modv(1, 0, 4), modv(1, 1, 4), A(norm1_g)[1]], axis=1)
        ims.append({"x1T": x1T[ci], "ygT": yg0[ci], "vecs": _c(vecs), "qkg": qkg, "wqkv": A(na_w_qkv)[0]})
    res = run(build_front1(NT, HL), ims)
    x2T = [_c(r["x2T"][:, :HL]) for r in res]
    def full(name, b):
        return np.concatenate([res[2*b][name][:, :HL], res[2*b+1][name][:, :HL], res[2*b][name][:, HL:], res[2*b+1][name][:, HL:]], axis=1)
    del yg0, x1T, h2T, cw
    biasT = host_bias(A(na_rpb)[0])
    ims = []
    for cc in range(NC):
        b, hg = cc // 2, cc % 2
        qf = full("qT", b).reshape(16, 128, L)[hg*8:(hg+1)*8]
        kf = full("kT", b).reshape(16, 128, L)[hg*8:(hg+1)*8]
        vf = full("vT", b).reshape(16, 128, L)[hg*8:(hg+1)*8]
        ims.append({"qT": _c(qf[:, :, :SEQ]), "kT": _c(kf), "v": _c(vf.transpose(2, 0, 1)), "biasT": _c(biasT[hg*8:(hg+1)*8])})
    res = run(build_attn(8), ims)
    wr1 = _c(np.concatenate([A(moe_w_route_group)[1], A(moe_w_route_expert)[1]], axis=1))
    ims = []
    for ci, (b, h) in enumerate(cores):
        oT = _c(np.concatenate([res[2*b][ "o"][h*HL:(h+1)*HL], res[2*b+1]["o"][h*HL:(h+1)*HL]], axis=1).T)
        vecs = np.stack([modv(1, 2, b), modv(1, 3, b), modv(1, 4, b), A(norm2_g)[1]], axis=1)
        ims.append({"oT": oT, "xT": x2T[ci], "vecs": _c(vecs), "wout": A(na_w_out)[0], "wr": wr1})
    res = run(build_back1(HL), ims)
    x3T = [r["x1T"] for r in res]; h2T = [r["h2T"] for r in res]; cw = [r["cw"] for r in res]
    yg1 = run_moe(1, h2T, cw, HL)
    ims = []
    for ci, (b, h) in enumerate(cores):
        ims.append({"xT": x3T[ci], "ygT": yg1[ci], "vecs": _c(modv(1, 5, b).reshape(16, 128).T)})
    res = run(build_final(HL), ims)
    out = np.empty((B, SEQ, D), f32)
    for ci, (b, h) in enumerate(cores):
        out[b, h*HL:(h+1)*HL] = res[ci]["outT"].T
    return out
```

```python
import math, time, sys


import sys
import numpy as np
import concourse.bass as bass
import concourse.mybir as mybir
from concourse.bass_utils import run_bass_kernel_spmd

F32 = mybir.dt.float32
BF16 = mybir.dt.bfloat16
I32 = mybir.dt.int32
AF = mybir.ActivationFunctionType
ALU = mybir.AluOpType
AX = mybir.AxisListType


class V:
    def __init__(self, tile, ap):
        self.tile = tile
        self.ap = ap

    def __getitem__(self, idx):
        return V(self.tile, self.ap[idx])


class T:
    def __init__(self, kb, tensor, name):
        self.kb = kb
        self.t = tensor
        self.name = name
        self.writes = {}
        self.reads = {}

    def __getitem__(self, idx):
        return V(self, self.t[idx])

    @property
    def all(self):
        return V(self, self.t[:])


def _ap(x):
    return x.ap if isinstance(x, V) else x


class KB:
    NDMA = 28

    def __init__(self):
        self.nc = bass.Bass("TRN2", target_bir_lowering=False)
        nc = self.nc
        self.eng = {'pe': nc.tensor, 'act': nc.scalar, 'dve': nc.vector, 'pool': nc.gpsimd, 'sp': nc.sync}
        self.sems = {}
        self.cnt = {}
        for e in ['pe', 'act', 'dve', 'pool']:
            self.sems[e] = nc.alloc_semaphore(name=f"sem_{e}")
            self.cnt[e] = 0
        for i in range(self.NDMA):
            self.sems[('d', i)] = nc.alloc_semaphore(name=f"sem_d{i}")
            self.cnt[('d', i)] = 0
        self.rr = 0
        self.waited = {e: {} for e in self.eng}
        self.out_tokens = []
        self.ntiles = 0
        self.ninstr = 0
        self.cms = []

    def sb(self, shape, dtype=F32, name=None):
        self.ntiles += 1
        name = "s_" + (name or f"t{self.ntiles}")
        cm = self.nc.sbuf_tensor(name, list(shape), dtype)
        t = cm.__enter__()
        self.cms.append(cm)
        return T(self, t, name)

    def mark(self):
        return len(self.cms)

    def barrier(self):
        for e in self.eng:
            for sk, val in self.cnt.items():
                if val > 0:
                    self._wait(e, sk, val)

    def release(self, mark):
        self.barrier()
        while len(self.cms) > mark:
            cm = self.cms.pop()
            cm.__exit__(None, None, None)

    def ps(self, shape, dtype=F32, name=None):
        self.ntiles += 1
        name = "ps_" + (name or f"p{self.ntiles}")
        cm = self.nc.psum_tensor(name, list(shape), dtype)
        t = cm.__enter__()
        self.cms.append(cm)
        return T(self, t, name)

    def dram_in(self, name, shape, dtype=F32):
        return self.nc.dram_tensor(name, list(shape), dtype, kind="ExternalInput").ap()

    def dram_out(self, name, shape, dtype=F32):
        return self.nc.dram_tensor(name, list(shape), dtype, kind="ExternalOutput").ap()

    def _wait(self, e, sk, val):
        w = self.waited[e]
        if w.get(sk, 0) >= val:
            return
        self.eng[e].wait_ge(self.sems[sk], val)
        w[sk] = val

    def issue(self, e, fn, outs, ins, dma=False):
        deps = []
        for v in ins:
            if isinstance(v, V):
                deps.extend(v.tile.writes.items())
        for v in outs:
            if isinstance(v, V):
                deps.extend(v.tile.writes.items())
                deps.extend(v.tile.reads.items())
        for sk, val in deps:
            if sk == e and e == 'pe':
                continue
            self._wait(e, sk, val)
        if dma:
            i = self.rr
            self.rr = (self.rr + 1) % self.NDMA
            sk = ('d', i)
            self._wait(e, sk, self.cnt[sk])
            inst = fn()
            self.cnt[sk] += 16
            inst.then_inc(self.sems[sk], 16)
        else:
            sk = e
            inst = fn()
            self.cnt[sk] += 1
            inst.then_inc(self.sems[sk], 1)
        tok = (sk, self.cnt[sk])
        self.ninstr += 1
        for v in ins:
            if isinstance(v, V):
                r = v.tile.reads
                if r.get(sk, 0) < tok[1]:
                    r[sk] = tok[1]
        for v in outs:
            if isinstance(v, V):
                w = v.tile.writes
                if w.get(sk, 0) < tok[1]:
                    w[sk] = tok[1]
        return tok

    def dma(self, out, in_, e='sp', is_output=False, **kw):
        tok = self.issue(e, lambda: self.eng[e].dma_start(out=_ap(out), in_=_ap(in_), **kw), [out], [in_], dma=True)
        if is_output:
            self.out_tokens.append(tok)
        return tok

    def mm(self, out, lhsT, rhs, start=True, stop=True, **kw):
        return self.issue('pe', lambda: self.nc.tensor.matmul(_ap(out), _ap(lhsT), _ap(rhs), start=start, stop=stop, **kw),
                          [out], [lhsT, rhs])

    def transpose(self, out, in_, ident):
        return self.issue('pe', lambda: self.nc.tensor.transpose(_ap(out), _ap(in_), _ap(ident)), [out], [in_, ident])

    def act(self, out, in_, func, bias=None, scale=None, accum_out=None, e='act'):
        kw = {}
        ins = [in_]
        outs = [out]
        if bias is not None:
            kw['bias'] = _ap(bias)
            if isinstance(bias, V):
                ins.append(bias)
        if scale is not None:
            kw['scale'] = _ap(scale)
            if isinstance(scale, V):
                ins.append(scale)
        if accum_out is not None:
            kw['accum_out'] = _ap(accum_out)
            outs.append(accum_out)
        return self.issue('act', lambda: self.nc.scalar.activation(out=_ap(out), in_=_ap(in_), func=func, **kw), outs, ins)

    def ts(self, out, in0, s1, op0, s2=None, op1=None, e='dve', accum_out=None):
        ins = [in0] + [s for s in (s1, s2) if isinstance(s, V)]
        kw = {}
        outs = [out]
        if op1 is not None:
            kw['op1'] = op1
        if accum_out is not None:
            kw['accum_out'] = _ap(accum_out)
            outs.append(accum_out)
        return self.issue(e, lambda: self.eng[e].tensor_scalar(out=_ap(out), in0=_ap(in0), scalar1=_ap(s1), scalar2=_ap(s2),
                                                               op0=op0, **kw), outs, ins)

    def tt(self, out, in0, in1, op, e='dve'):
        return self.issue(e, lambda: self.eng[e].tensor_tensor(out=_ap(out), in0=_ap(in0), in1=_ap(in1), op=op), [out], [in0, in1])

    def stt(self, out, in0, scalar, in1, op0, op1, e='dve'):
        ins = [in0, in1] + ([scalar] if isinstance(scalar, V) else [])
        return self.issue(e, lambda: self.eng[e].scalar_tensor_tensor(out=_ap(out), in0=_ap(in0), scalar=_ap(scalar), in1=_ap(in1),
                                                                      op0=op0, op1=op1), [out], ins)

    def copy(self, out, in_, e='dve'):
        if e == 'act':
            return self.act(out, in_, AF.Copy)
        return self.issue(e, lambda: self.eng[e].tensor_copy(out=_ap(out), in_=_ap(in_)), [out], [in_])

    def memset(self, out, val, e='dve'):
        return self.issue(e, lambda: self.eng[e].memset(_ap(out), val), [out], [])

    def recip(self, out, in_):
        return self.issue('dve', lambda: self.nc.vector.reciprocal(out=_ap(out), in_=_ap(in_)), [out], [in_])

    def scan(self, out, d0, d1, initial, op0=ALU.mult, op1=ALU.add):
        ins = [d0, d1] + ([initial] if isinstance(initial, V) else [])
        return self.issue('dve', lambda: self.nc.vector.tensor_tensor_scan(out=_ap(out), data0=_ap(d0), data1=_ap(d1),
                                                                           initial=_ap(initial), op0=op0, op1=op1), [out], ins)

    def reduce(self, out, in_, op, axis=AX.X, e='dve'):
        return self.issue(e, lambda: self.eng[e].tensor_reduce(out=_ap(out), in_=_ap(in_), axis=axis, op=op), [out], [in_])

    def iota(self, out, pattern, base=0, channel_multiplier=0, **kw):
        return self.issue('pool', lambda: self.nc.gpsimd.iota(_ap(out), pattern, base=base, channel_multiplier=channel_multiplier, **kw),
                          [out], [])

    def affine_select(self, out, in_, pattern, compare_op, fill, base=0, channel_multiplier=0):
        return self.issue('pool', lambda: self.nc.gpsimd.affine_select(out=_ap(out), in_=_ap(in_), pattern=pattern,
                                                                       compare_op=compare_op, fill=fill, base=base,
                                                                       channel_multiplier=channel_multiplier), [out], [in_])

    def finish(self):
        last = {}
        for sk, val in self.out_tokens:
            last[sk] = max(last.get(sk, 0), val)
        for sk, val in last.items():
            self._wait('sp', sk, val)
        return self.nc


def run(kb_or_nc, in_maps, n=8):
    nc = kb_or_nc.nc if isinstance(kb_or_nc, KB) else kb_or_nc
    import time as _time
    _t = _time.time()
    res = run_bass_kernel_spmd(nc, in_maps, core_ids=list(range(n)))
    nb = sum(a.nbytes for m in in_maps for a in m.values())
    print(f"[launch] {_time.time() - _t:.1f}s in_bytes={nb/1e6:.0f}MB", file=sys.stderr, flush=True)
    return res.results


def chunks(NT, sz=512):
    return [(s, min(sz, NT - s)) for s in range(0, NT, sz)]

def load_vecs(kb, vecs_d, nv):
    vt = kb.sb([128, 16, nv], name="vecs")
    kb.dma(vt.all, vecs_d.rearrange("(kt p) v -> p kt v", p=128))
    return vt

def consts(kb):
    c = {}
    c['ones'] = kb.sb([128, 128], name="ones"); kb.memset(c['ones'].all, 1.0)
    c['eps'] = kb.sb([128, 1], name="eps"); kb.memset(c['eps'].all, 1e-6)
    return c

def norm_mod(kb, c, xT, hT, vt, NT, seg_cols, gcol, D=2048, xdt=F32):
    KT = D // 128
    gsc = {}
    for (s0, sz, shc, scc) in seg_cols:
        if scc not in gsc:
            g = kb.sb([128, KT], name=f"gsc{scc}")
            kb.stt(g.all, vt[:, :, scc], 1.0, vt[:, :, gcol], ALU.add, ALU.mult)
            gsc[scc] = g
    sq = [kb.sb([128, 512], name=f"sq{i}") for i in range(2)]
    tmp = [kb.sb([128, 512], name=f"nm_tmp{i}") for i in range(2)]
    rstd = kb.sb([128, 512], name="rstd")
    ps = kb.ps([128, 512], name="ps_norm")
    n = 0
    for (s0, sz, shc, scc) in seg_cols:
        for (c0, cs) in chunks(sz):
            a = s0 + c0
            for kt in range(KT):
                q = sq[kt % 2]
                kb.act(q[:, :cs], xT[:, kt, a:a+cs], AF.Square)
                kb.mm(ps[:, :cs], c['ones'].all, q[:, :cs], start=(kt == 0), stop=(kt == KT-1))
            kb.act(rstd[:, :cs], ps[:, :cs], AF.Sqrt, bias=c['eps'].all, scale=1.0 / D)
            kb.recip(rstd[:, :cs], rstd[:, :cs])
            for kt in range(KT):
                t = tmp[kt % 2]
                kb.stt(t[:, :cs], xT[:, kt, a:a+cs], gsc[scc][:, kt:kt+1], rstd[:, :cs], ALU.mult, ALU.mult)
                kb.act(hT[:, kt, a:a+cs], t[:, :cs], AF.Identity, bias=vt[:, kt, shc:shc+1])

def proj(kb, hT, W_d, NT, n_out, KT, evac, wname="w", tag="", pss=None, wbufs=None):
    if wbufs is None:
        wst = [kb.sb([128, KT, 128], F32, name=f"{tag}wst{i}") for i in range(2)]
        wbf = [kb.sb([128, KT, 128], BF16, name=f"{tag}wbf{i}") for i in range(2)]
    else:
        wst, wbf = wbufs
    if pss is None:
        pss = [kb.ps([128, 512], name=f"{tag}pp{i}") for i in range(3)]
    pi = 0
    Wv = W_d.rearrange("(kt p) n -> p kt n", p=128)
    nj = (n_out + 127) // 128
    for j in range(nj):
        nsz = min(128, n_out - j * 128)
        ws, wb = wst[j % 2], wbf[j % 2]
        kb.dma(ws[:, :KT, :nsz], Wv[:, :, j*128:j*128+nsz])
        kb.copy(wb[:, :KT, :nsz], ws[:, :KT, :nsz], e='pool')
        for (c0, cs) in chunks(NT):
            p = pss[pi % 3]; pi += 1
            for kt in range(KT):
                kb.mm(p[:nsz, :cs], wb[:, kt, :nsz], hT[:, kt, c0:c0+cs], start=(kt == 0), stop=(kt == KT-1))
            evac(j, nsz, c0, cs, p[:nsz, :cs])

def build_front(NT=2176, NLAT=2048, n_out=4128):
    kb = KB()
    xT_d = kb.dram_in("xT", [2048, NT])
    vecs_d = kb.dram_in("vecs", [2048, 5])
    W_d = kb.dram_in("W", [2048, n_out])
    pT_d = kb.dram_out("pT", [n_out, NT])
    c = consts(kb)
    vt = load_vecs(kb, vecs_d, 5)
    xv = xT_d.rearrange("(kt p) t -> p kt t", p=128)
    hT = kb.sb([128, 16, NT], BF16, name="hT")
    def x_src(kt, a, cs, dst):
        kb.dma(dst, xv[:, kt, a:a+cs])
    def sink(a, cs, h32):
        kb.copy(hT[:, :, a:a+cs], h32[:, :, :cs], e='pool')
    m = kb.mark()
    norm_mod2(kb, c, x_src, sink, vt, [(0, NLAT, 0, 1), (NLAT, NT - NLAT, 2, 3)], 4, tag="n1")
    kb.release(m)
    ost = [kb.sb([128, NT], F32, name=f"ost{i}") for i in range(2)]
    state = {'n': 0}
    def evac(j, nsz, c0, cs, pv):
        o = ost[j % 2]
        if state['n'] % 2 == 0:
            kb.copy(o[:nsz, c0:c0+cs], pv, e='act')
        else:
            kb.copy(o[:nsz, c0:c0+cs], pv, e='dve')
        state['n'] += 1
        if c0 + cs == NT:
            kb.dma(pT_d[j*128:j*128+nsz, :], o[:nsz, :], is_output=True)
    proj(kb, hT, W_d, NT, n_out, 16, evac)
    kb.finish()
    return kb

def ref_front(x, vecs, W):
    x = x.astype(np.float64)
    g = vecs[:, 4]
    y = x / np.sqrt((x * x).mean(-1, keepdims=True) + 1e-6) * g
    NT = x.shape[0]
    return y


GC = 2 * math.sqrt(2 / math.pi)

def dtrack(kb, ap, name):
    return V(T(kb, None, name), ap)

def norm_mod2(kb, c, x_src, hT_sink, vt, seg_cols, gcol, D=2048, tag="nm"):
    KT = D // 128
    gsc = {}
    for (s0, sz, shc, scc) in seg_cols:
        if scc not in gsc:
            g = kb.sb([128, KT], name=f"{tag}gsc{scc}")
            kb.stt(g.all, vt[:, :, scc], 1.0, vt[:, :, gcol], ALU.add, ALU.mult)
            gsc[scc] = g
    xc = kb.sb([128, KT, 512], name=f"{tag}_xc")
    h32 = kb.sb([128, KT, 512], name=f"{tag}_h32")
    sq = [kb.sb([128, 512], name=f"{tag}_sq{i}") for i in range(2)]
    rstd = kb.sb([128, 512], name=f"{tag}_rstd")
    ps = kb.ps([128, 512], name=f"{tag}_ps")
    for (s0, sz, shc, scc) in seg_cols:
        for (c0, cs) in chunks(sz):
            a = s0 + c0
            for kt in range(KT):
                x_src(kt, a, cs, xc[:, kt, :cs])
                q = sq[kt % 2]
                kb.act(q[:, :cs], xc[:, kt, :cs], AF.Square)
                kb.mm(ps[:, :cs], c['ones'].all, q[:, :cs], start=(kt == 0), stop=(kt == KT-1))
            kb.act(rstd[:, :cs], ps[:, :cs], AF.Sqrt, bias=c['eps'].all, scale=1.0 / D)
            kb.recip(rstd[:, :cs], rstd[:, :cs])
            for kt in range(KT):
                kb.stt(xc[:, kt, :cs], xc[:, kt, :cs], gsc[scc][:, kt:kt+1], rstd[:, :cs], ALU.mult, ALU.mult)
                kb.act(h32[:, kt, :cs], xc[:, kt, :cs], AF.Identity, bias=vt[:, kt, shc:shc+1])
            hT_sink(a, cs, h32)

def gelu_tanh(kb, out, x, t1, t2):
    kb.tt(t1, x, x, ALU.mult, e='pool')
    kb.ts(t1, t1, 0.044715, ALU.mult, 1.0, ALU.add)
    kb.tt(t1, t1, x, ALU.mult, e='pool')
    kb.act(t2, t1, AF.Sigmoid, scale=GC)
    kb.tt(out, t2, x, ALU.mult)

def routing(kb, lg, cw_out, wk):
    L = wk['L']; kb.copy(L[:, :], lg, e='act')
    gmax, ngmax, gm, eg, se, pen = wk['gmax'], wk['ngmax'], wk['gm'], wk['eg'], wk['se'], wk['pen']
    kb.reduce(gmax.all, L[:, 0:4], ALU.max)
    kb.ts(ngmax.all, gmax.all, -1.0, ALU.mult)
    kb.ts(gm.all, L[:, 0:4], gmax[:, 0:1], ALU.is_equal)
    kb.act(eg.all, L[:, 0:4], AF.Exp, bias=ngmax[:, 0:1], accum_out=se.all)
    kb.recip(se.all, se.all)
    kb.ts(pen.all, gm.all, 1e30, ALU.mult, -1e30, ALU.add)
    elm = wk['elm']
    for g in range(4):
        kb.ts(elm[:, g*8:(g+1)*8], L[:, 4+g*8:4+(g+1)*8], pen[:, g:g+1], ALU.add)
    top8 = wk['top8']
    kb.issue('dve', lambda: kb.nc.vector.max(out=top8.all.ap, in_=elm.all.ap), [top8.all], [elm.all])
    d, w1, w2, m1, m2 = wk['d'], wk['w1'], wk['w2'], wk['m1'], wk['m2']
    kb.tt(d.all, top8[:, 1:2], top8[:, 0:1], ALU.subtract)
    kb.act(d.all, d.all, AF.Exp)
    kb.ts(w1.all, d.all, 1.0, ALU.add)
    kb.recip(w1.all, w1.all)
    kb.tt(w2.all, d.all, w1.all, ALU.mult)
    kb.tt(w1.all, w1.all, se.all, ALU.mult)
    kb.tt(w2.all, w2.all, se.all, ALU.mult)
    kb.ts(m1.all, elm.all, top8[:, 0:1], ALU.is_equal, w1[:, 0:1], ALU.mult)
    kb.ts(m2.all, elm.all, top8[:, 1:2], ALU.is_equal, w2[:, 0:1], ALU.mult)
    kb.tt(cw_out, m1.all, m2.all, ALU.add)

def routing_wk(kb):
    def s(n, w): return kb.sb([128, w], name="rt_" + n)
    return {'L': s('L', 36), 'gmax': s('gmax', 1), 'ngmax': s('ngmax', 1), 'gm': s('gm', 4), 'eg': s('eg', 4), 'se': s('se', 1),
            'pen': s('pen', 4), 'elm': s('elm', 32), 'top8': s('top8', 8), 'd': s('d', 1), 'w1': s('w1', 1), 'w2': s('w2', 1),
            'm1': s('m1', 32), 'm2': s('m2', 32)}

def back_tail(kb, c, aT, KTa, wout_d, xT_d, x1T_d, h2T_d, cw_d, vt, wr_d, NT, segs, g1cols, gcol, pss=None, wbufs=None):
    x1tr = dtrack(kb, x1T_d, "x1T_dram")
    x1v = x1T_d.rearrange("(kt p) t -> p kt t", p=128)
    xv = xT_d.rearrange("(kt p) t -> p kt t", p=128)
    xin = [kb.sb([128, 512], name=f"bt_xin{i}") for i in range(3)]
    xo = [kb.sb([128, 512], name=f"bt_xo{i}") for i in range(3)]
    st = {'n': 0}
    def seg_of(tok):
        for i, (s0, sz, _, _) in enumerate(segs):
            if s0 <= tok < s0 + sz: return i
    def evac(j, nsz, c0, cs, pv):
        i = st['n'] % 3; st['n'] += 1
        kb.dma(xin[i][:, :cs], xv[:, j, c0:c0+cs])
        sg = seg_of(c0)
        kb.stt(xo[i][:, :cs], pv, vt[:, j, g1cols[sg]:g1cols[sg]+1], xin[i][:, :cs], ALU.mult, ALU.add)
        kb.dma(V(x1tr.tile, x1v[:, j, c0:c0+cs]), xo[i][:, :cs], is_output=True)
    proj(kb, aT, wout_d, NT, 2048, KTa, evac, tag="wo", pss=pss, wbufs=wbufs)
    wr = kb.sb([128, 16, 36], name="bt_wr"); kb.dma(wr.all, wr_d.rearrange("(kt p) n -> p kt n", p=128))
    hb = kb.sb([128, 16, 512], BF16, name="bt_hb")
    cwsb = kb.sb([128, 32], name="bt_cw")
    psr = kb.ps([128, 36], name="bt_psr")
    wk = routing_wk(kb)
    h2v = h2T_d.rearrange("(kt p) t -> p kt t", p=128)
    def x_src(kt, a, cs, dst):
        kb.dma(dst, V(x1tr.tile, x1v[:, kt, a:a+cs]))
    def sink(a, cs, h32):
        kb.copy(hb[:, :, :cs], h32[:, :, :cs], e='pool')
        kb.dma(h2v[:, :, a:a+cs], hb[:, :, :cs], is_output=True)
        for tt_ in range(cs // 128):
            for kt in range(16):
                kb.mm(psr.all, h32[:, kt, tt_*128:(tt_+1)*128], wr[:, kt, :], start=(kt == 0), stop=(kt == 15))
            routing(kb, psr.all, cwsb.all, wk)
            kb.dma(cw_d[a+tt_*128:a+(tt_+1)*128, :], cwsb.all, is_output=True)
    norm_mod2(kb, c, x_src, sink, vt, segs, gcol, tag="n2")

def build_back0(NT=2176, NLAT=2048):
    kb = KB()
    D = {}
    for n in ["yfT", "ybT", "uT", "ofT", "obT", "rT"]:
        D[n] = kb.dram_in(n, [1024, NT])
    xT_d = kb.dram_in("xT", [2048, NT])
    vecs_d = kb.dram_in("vecs", [2048, 7])
    v8_d = kb.dram_in("v8", [1024, 2])
    wglu_d = kb.dram_in("wglu", [1024, 1024]); wout_d = kb.dram_in("wout", [2048, 2048]); wr_d = kb.dram_in("wr", [2048, 36])
    x1T_d = kb.dram_out("x1T", [2048, NT]); h2T_d = kb.dram_out("h2T", [2048, NT], BF16); cw_d = kb.dram_out("cw", [NT, 32])
    c = consts(kb)
    vt = kb.sb([128, 16, 7], name="vecs_sb"); kb.dma(vt.all, vecs_d.rearrange("(kt p) v -> p kt v", p=128))
    v8 = kb.sb([128, 8, 2], name="v8_sb"); kb.dma(v8.all, v8_d.rearrange("(kt p) v -> p kt v", p=128))
    aT = kb.sb([128, 16, NT], BF16, name="aT")
    mk = kb.mark()
    gT = kb.sb([128, 8, NT], BF16, name="gT")
    def ld(n): return [kb.sb([128, 512], name=f"ld_{n}{i}") for i in range(2)]
    A, Bt, Ct = ld("a"), ld("b"), ld("c")
    t1 = kb.sb([128, 512], name="b0_t1"); t2 = kb.sb([128, 512], name="b0_t2")
    views = {n: D[n].rearrange("(kt p) t -> p kt t", p=128) for n in D}
    n = 0
    for (c0, cs) in chunks(NT):
        for kt in range(8):
            a, b, u = A[n % 2], Bt[n % 2], Ct[n % 2]; n += 1
            kb.dma(a[:, :cs], views["yfT"][:, kt, c0:c0+cs]); kb.dma(b[:, :cs], views["ybT"][:, kt, c0:c0+cs])
            kb.dma(u[:, :cs], views["uT"][:, kt, c0:c0+cs])
            kb.tt(a[:, :cs], a[:, :cs], b[:, :cs], ALU.add)
            kb.stt(a[:, :cs], u[:, :cs], v8[:, kt, 0:1], a[:, :cs], ALU.mult, ALU.add)
            gelu_tanh(kb, gT[:, kt, c0:c0+cs], a[:, :cs], t1[:, :cs], t2[:, :cs])
    def evac_glu(j, nsz, c0, cs, pv):
        kb.act(t2[:, :cs], pv, AF.Sigmoid)
        kb.tt(aT[:, j, c0:c0+cs], t2[:, :cs], gT[:, j, c0:c0+cs], ALU.mult)
    pss = [kb.ps([128, 512], name=f"shp{i}") for i in range(3)]
    wbufs = ([kb.sb([128, 16, 128], F32, name=f"shwst{i}") for i in range(2)], [kb.sb([128, 16, 128], BF16, name=f"shwbf{i}") for i in range(2)])
    proj(kb, gT, wglu_d, NT, 1024, 8, evac_glu, tag="glu", pss=pss, wbufs=wbufs)
    o2 = [kb.sb([128, 512], name=f"b0_o{i}") for i in range(2)]
    r2 = [kb.sb([128, 512], name=f"b0_r{i}") for i in range(2)]
    sq = kb.sb([128, 512], name="b0_sq"); rstd = kb.sb([128, 512], name="b0_rstd")
    psn = kb.ps([128, 512], name="b0_psn")
    for (c0, cs) in chunks(NT):
        for h in range(4):
            for i in range(2):
                kt = 2 * h + i
                a, b = A[i], Bt[i]
                kb.dma(a[:, :cs], views["ofT"][:, kt, c0:c0+cs]); kb.dma(b[:, :cs], views["obT"][:, kt, c0:c0+cs])
                kb.dma(r2[i][:, :cs], views["rT"][:, kt, c0:c0+cs])
                kb.tt(o2[i][:, :cs], a[:, :cs], b[:, :cs], ALU.add)
                kb.act(sq[:, :cs], o2[i][:, :cs], AF.Square)
                kb.mm(psn[:, :cs], c['ones'].all, sq[:, :cs], start=(i == 0), stop=(i == 1))
            kb.act(rstd[:, :cs], psn[:, :cs], AF.Sqrt, bias=c['eps'].all, scale=1.0 / 256)
            kb.recip(rstd[:, :cs], rstd[:, :cs])
            for i in range(2):
                kt = 2 * h + i
                kb.stt(o2[i][:, :cs], o2[i][:, :cs], v8[:, kt, 1:2], rstd[:, :cs], ALU.mult, ALU.mult)
                kb.act(t2[:, :cs], r2[i][:, :cs], AF.Sigmoid)
                kb.tt(t1[:, :cs], r2[i][:, :cs], t2[:, :cs], ALU.mult, e='pool')
                kb.tt(aT[:, 8 + kt, c0:c0+cs], o2[i][:, :cs], t1[:, :cs], ALU.mult)
    kb.release(mk)
    segs = [(0, NLAT, 2, 3), (NLAT, NT - NLAT, 4, 5)]
    back_tail(kb, c, aT, 16, wout_d, xT_d, x1T_d, h2T_d, cw_d, vt, wr_d, NT, segs, [0, 1], 6)
    kb.finish()
    return kb

def np_gelu(x): return 0.5 * x * (1 + np.tanh(math.sqrt(2 / math.pi) * (x + 0.044715 * x ** 3)))
def np_sig(x): return 1 / (1 + np.exp(-x))

def ref_routing(h, wr):
    lg = h @ wr
    gl = lg[:, :4]; gp = np.exp(gl - gl.max(-1, keepdims=True)); gp /= gp.sum(-1, keepdims=True)
    gi = gp.argmax(-1); ptop = gp.max(-1)
    el = lg[:, 4:].reshape(-1, 4, 8)[np.arange(len(h)), gi]
    order = np.argsort(-el, axis=-1)[:, :2]
    ev = np.take_along_axis(el, order, -1)
    w = np.exp(ev - ev.max(-1, keepdims=True)); w /= w.sum(-1, keepdims=True); w *= ptop[:, None]
    cw = np.zeros((len(h), 32))
    for k in range(2):
        cw[np.arange(len(h)), gi * 8 + order[:, k]] = w[:, k]
    return cw

def ref_back0(I, NLAT):
    f = lambda n: I[n].astype(np.float64).T
    vecs = I["vecs"].astype(np.float64); v8 = I["v8"].astype(np.float64)
    y = f("yfT") + f("ybT") + v8[:, 0] * f("uT")
    g = np_gelu(y); aS = g * np_sig(g @ I["wglu"].astype(np.float64))
    o = (f("ofT") + f("obT")).reshape(-1, 4, 256)
    o = o / np.sqrt((o * o).mean(-1, keepdims=True) + 1e-6)
    r = f("rT")
    aG = o.reshape(-1, 1024) * v8[:, 1] * (r * np_sig(r))
    a = np.concatenate([aS, aG], -1)
    ox = a @ I["wout"].astype(np.float64)
    x = f("xT"); NT = x.shape[0]
    g1 = np.where(np.arange(NT)[:, None] < NLAT, vecs[:, 0], vecs[:, 1])
    x1 = x + g1 * ox
    yn = x1 / np.sqrt((x1 * x1).mean(-1, keepdims=True) + 1e-6) * vecs[:, 6]
    sh = np.where(np.arange(NT)[:, None] < NLAT, vecs[:, 2], vecs[:, 4]); sc = np.where(np.arange(NT)[:, None] < NLAT, vecs[:, 3], vecs[:, 5])
    h2 = yn * (1 + sc) + sh
    return x1, h2, ref_routing(h2, I["wr"].astype(np.float64))


TWO_PI = 2 * math.pi

def sin_rr(kb, out, ang, shift, shape, wk):
    a, n, m = wk['a'], wk['n'], wk['m']
    sl = tuple(slice(0, s) for s in shape)
    A, N, M = a[sl], n[sl], m[sl]
    kb.ts(A, ang, 1.0 / TWO_PI, ALU.mult, shift / TWO_PI, ALU.add)
    kb.copy(N, A)
    kb.copy(M, N)
    kb.tt(A, A, M, ALU.subtract)
    kb.ts(M, A, 0.5, ALU.is_gt)
    kb.tt(A, A, M, ALU.subtract)
    kb.ts(M, A, -0.5, ALU.is_lt)
    kb.tt(A, A, M, ALU.add)
    kb.ts(A, A, 0.4999999, ALU.min, -0.4999999, ALU.max)
    kb.act(out, A, AF.Sin, scale=TWO_PI)

def s5_part(kb, uT_d, prm_d, bT_d, cT_d, yT_d, L, NGP=32, T=512):
    prm = kb.sb([128, NGP, 3], name="s5prm"); kb.dma(prm.all, prm_d)
    bT = kb.sb([32, NGP, 2, 128], name="s5bT"); kb.dma(bT.all, bT_d)
    cT = kb.sb([128, NGP, 2, 32], name="s5cT"); kb.dma(cT.all, cT_d)
    kb.ts(cT[:, :, 1, :], cT[:, :, 1, :], -1.0, ALU.mult)
    def sm(name): return kb.sb([128, NGP], name="s5_" + name)
    lr, dt, mag, th, sn, cs = sm("lr"), sm("dt"), sm("mag"), sm("th"), sm("sn"), sm("cs")
    are, aim, den, fre, fim, nfre, t1, t2 = sm("are"), sm("aim"), sm("den"), sm("fre"), sm("fim"), sm("nfre"), sm("t1"), sm("t2")
    wk_s = {'a': kb.sb([128, NGP], name="wka"), 'n': kb.sb([128, NGP], I32, name="wkn"), 'm': kb.sb([128, NGP], name="wkm")}
    kb.ts(lr.all, prm[:, :, 0], -1e-4, ALU.min)
    kb.act(dt.all, prm[:, :, 2], AF.Exp)
    kb.tt(t1.all, lr.all, dt.all, ALU.mult)
    kb.act(mag.all, t1.all, AF.Exp)
    kb.tt(th.all, prm[:, :, 1], dt.all, ALU.mult)
    sin_rr(kb, sn.all, th.all, 0.0, (128, NGP), wk_s)
    sin_rr(kb, cs.all, th.all, math.pi / 2, (128, NGP), wk_s)
    kb.tt(are.all, mag.all, cs.all, ALU.mult)
    kb.tt(aim.all, mag.all, sn.all, ALU.mult)
    kb.ts(are.all, are.all, -1.0, ALU.add)
    li = prm[:, :, 1]
    kb.tt(den.all, lr.all, lr.all, ALU.mult)
    kb.tt(t1.all, li, li, ALU.mult)
    kb.tt(den.all, den.all, t1.all, ALU.add)
    kb.recip(den.all, den.all)
    kb.tt(t1.all, are.all, lr.all, ALU.mult)
    kb.tt(t2.all, aim.all, li, ALU.mult)
    kb.tt(t1.all, t1.all, t2.all, ALU.add)
    kb.tt(fre.all, t1.all, den.all, ALU.mult)
    kb.tt(t1.all, aim.all, lr.all, ALU.mult)
    kb.tt(t2.all, are.all, li, ALU.mult)
    kb.tt(t1.all, t1.all, t2.all, ALU.subtract)
    kb.tt(fim.all, t1.all, den.all, ALU.mult)
    kb.ts(nfre.all, fre.all, -1.0, ALU.mult)
    taui = kb.sb([128, T], I32, name="taui")
    kb.iota(taui.all, [[1, T]], base=1, channel_multiplier=0)
    tau = kb.sb([128, T], name="tau"); kb.copy(tau.all, taui.all)
    ones = kb.sb([128, T], name="onesT"); kb.memset(ones.all, 1.0)
    def big(name, dt_=F32): return kb.sb([128, T], dt_, name="s5_" + name)
    ang, c, s, wr, wi, rt = big("ang"), big("c"), big("s"), big("wr"), big("wi"), big("rt")
    wk = {'a': big("wa"), 'n': big("wn", I32), 'm': big("wm")}
    bre, bim = big("bre"), big("bim")
    p1, p2, p3, p4 = big("p1"), big("p2"), big("p3"), big("p4")
    xre, xim = big("xre"), big("xim")
    kre, kim = big("kre"), big("kim")
    hre = [big("hre0"), big("hre1")]; him = [big("him0"), big("him1")]
    psA = [kb.ps([128, 512], name=f"s5A{i}") for i in range(2)]
    psB = [kb.ps([128, 512], name=f"s5B{i}") for i in range(2)]
    psY = [kb.ps([32, 512], name=f"s5Y{i}") for i in range(2)]
    ut = [kb.sb([32, L], name=f"s5u{i}") for i in range(2)]
    ysb = [kb.sb([32, L], name=f"s5y{i}") for i in range(2)]
    tl = chunks(L, T)
    unit = 0
    for gp in range(NGP):
        u = ut[gp % 2]; yo = ysb[gp % 2]
        kb.dma(u.all, uT_d[gp*32:(gp+1)*32, :])
        kb.ts(ang.all, tau.all, th[:, gp:gp+1], ALU.mult)
        sin_rr(kb, s.all, ang.all, 0.0, (128, T), wk)
        sin_rr(kb, c.all, ang.all, math.pi / 2, (128, T), wk)
        kb.ts(wk['a'].all, c.all, fre[:, gp:gp+1], ALU.mult)
        kb.stt(wr.all, s.all, fim[:, gp:gp+1], wk['a'].all, ALU.mult, ALU.add)
        kb.ts(wk['m'].all, c.all, fim[:, gp:gp+1], ALU.mult)
        kb.stt(wi.all, s.all, nfre[:, gp:gp+1], wk['m'].all, ALU.mult, ALU.add)
        kb.ts(rt.all, ones.all, mag[:, gp:gp+1], ALU.mult)
        for ti, (t0, ts_) in enumerate(tl):
            A = psA[unit % 2]; B = psB[unit % 2]; Y = psY[unit % 2]; unit += 1
            kb.mm(A[:, :ts_], bT[:, gp, 0, :], u[:, t0:t0+ts_])
            kb.mm(B[:, :ts_], bT[:, gp, 1, :], u[:, t0:t0+ts_])
            kb.copy(bre[:, :ts_], A[:, :ts_], e='act')
            kb.copy(bim[:, :ts_], B[:, :ts_], e='act')
            kb.tt(p1[:, :ts_], bre[:, :ts_], wr[:, :ts_], ALU.mult, e='pool')
            kb.tt(p2[:, :ts_], bim[:, :ts_], wi[:, :ts_], ALU.mult, e='pool')
            kb.tt(p3[:, :ts_], bre[:, :ts_], wi[:, :ts_], ALU.mult, e='pool')
            kb.tt(p4[:, :ts_], bim[:, :ts_], wr[:, :ts_], ALU.mult, e='pool')
            kb.tt(xre[:, :ts_], p1[:, :ts_], p2[:, :ts_], ALU.subtract)
            kb.tt(xim[:, :ts_], p3[:, :ts_], p4[:, :ts_], ALU.add)
            if ti == 0:
                ir, ii = 0.0, 0.0
            else:
                pt = tl[ti-1][1]
                ir = hre[(ti-1) % 2][:, pt-1:pt]; ii = him[(ti-1) % 2][:, pt-1:pt]
            kb.scan(kre[:, :ts_], rt[:, :ts_], xre[:, :ts_], ir)
            kb.scan(kim[:, :ts_], rt[:, :ts_], xim[:, :ts_], ii)
            kb.tt(p1[:, :ts_], kre[:, :ts_], c[:, :ts_], ALU.mult, e='pool')
            kb.tt(p2[:, :ts_], kim[:, :ts_], s[:, :ts_], ALU.mult, e='pool')
            kb.tt(p3[:, :ts_], kre[:, :ts_], s[:, :ts_], ALU.mult, e='pool')
            kb.tt(p4[:, :ts_], kim[:, :ts_], c[:, :ts_], ALU.mult, e='pool')
            hr = hre[ti % 2]; hi = him[ti % 2]
            kb.tt(hr[:, :ts_], p1[:, :ts_], p2[:, :ts_], ALU.subtract)
            kb.tt(hi[:, :ts_], p3[:, :ts_], p4[:, :ts_], ALU.add)
            kb.mm(Y[:, :ts_], cT[:, gp, 0, :], hr[:, :ts_], start=True, stop=False)
            kb.mm(Y[:, :ts_], cT[:, gp, 1, :], hi[:, :ts_], start=False, stop=True)
            kb.copy(yo[:, t0:t0+ts_], Y[:, :ts_], e='act')
        kb.dma(yT_d[gp*32:(gp+1)*32, :], yo.all, is_output=True)

def s5_host_params(lam_re, lam_im, log_dt, b_re, b_im, c_re, c_im, NGP=32):
    G = NGP * 2
    P, I = 64, 16
    def gpl(a):
        return np.ascontiguousarray(a.reshape(NGP, 2, P).transpose(1, 2, 0).reshape(128, NGP))
    prm = np.stack([gpl(lam_re), gpl(lam_im), gpl(np.repeat(log_dt[:, None], P, axis=1))], axis=-1).astype(np.float32)
    bT = np.zeros((32, NGP, 2, 128), np.float32)
    cT = np.zeros((128, NGP, 2, 32), np.float32)
    for k, (br, cr) in enumerate([(b_re, c_re), (b_im, c_im)]):
        brr = br.reshape(NGP, 2, P, I); crr = cr.reshape(NGP, 2, I, P)
        for g2 in range(2):
            bT[g2*16:(g2+1)*16, :, k, g2*64:(g2+1)*64] = brr[:, g2].transpose(2, 0, 1)
            cT[g2*64:(g2+1)*64, :, k, g2*16:(g2+1)*16] = crr[:, g2].transpose(2, 0, 1)
    return prm, bT, cT

def s5_ref(u, lam_re, lam_im, log_dt, b_re, b_im, c_re, c_im):
    lr = np.minimum(lam_re.astype(np.float64), -1e-4); li = lam_im.astype(np.float64)
    dt = np.exp(log_dt.astype(np.float64))[:, None]
    lam = lr + 1j * li
    abar = np.exp(lam * dt)
    f = (abar - 1) / lam
    B = b_re.astype(np.float64) + 1j * b_im
    C = c_re.astype(np.float64) + 1j * c_im
    L = u.shape[0]
    x = f[None] * np.einsum('lgi,gpi->lgp', u, B)
    h = np.zeros_like(x[0]); ys = []
    for t in range(L):
        h = abar * h + x[t]
        ys.append(np.einsum('gp,gip->gi', h, C).real)
    return np.stack(ys)


def gla_part(kb, qT_d, kT_d, v_d, lrT_d, wg_d, bg_d, rc_d, rs_d, cmask_d, ident_d, o_d, L, T=512):
    H = 4
    cmask = kb.sb([64, 64], name="cmask"); kb.dma(cmask.all, cmask_d)
    identf = kb.sb([128, 128], name="identf"); kb.dma(identf.all, ident_d)
    ident = kb.sb([128, 128], BF16, name="ident"); kb.copy(ident.all, identf.all)
    wg = kb.sb([16, 512], name="wg"); kb.dma(wg.all, wg_d)
    bg = kb.sb([128, 4], name="bg"); kb.dma(bg.all, bg_d)
    rm = kb.sb([128, T], name="rm"); kb.memset(rm.all, 1.0)
    kb.memset(rm[:, 0:T:64], 0.0)
    def big(name, dt_=F32, n=4): return kb.sb([128, n, T], dt_, name="g_" + name)
    q32, qsw, k32, ksw = big("q32"), big("qsw"), big("k32"), big("ksw")
    rc = kb.sb([128, T], name="g_rc"); rs = kb.sb([128, T], name="g_rs")
    lr = kb.sb([16, T], name="g_lr")
    t1, t2 = big("t1"), big("t2")
    la, b16, eb, enb = big("la"), big("b16"), big("eb"), big("enb")
    qinb, kinb = big("qinb", BF16), big("kinb", BF16)
    kin32 = big("kin32")
    S32 = kb.sb([128, 4, 256], name="g_S32"); kb.memset(S32.all, 0.0)
    Sb = kb.sb([128, 4, 256], BF16, name="g_Sb"); kb.memset(Sb.all, 0.0)
    vst = [kb.sb([64, 1024], name=f"g_vst{i}") for i in range(2)]
    vb = [kb.sb([64, 1024], BF16, name=f"g_vb{i}") for i in range(2)]
    osb = [kb.sb([64, 1024], name=f"g_osb{i}") for i in range(2)]
    attb = [kb.sb([64, 64], BF16, name=f"g_attb{i}") for i in range(2)]
    kout = [kb.sb([128, 64], BF16, name=f"g_kout{i}") for i in range(2)]
    koT = [kb.sb([64, 128], BF16, name=f"g_koT{i}") for i in range(2)]
    pAtt = [kb.ps([64, 64], name=f"g_pAtt{i}") for i in range(2)]
    pKo = [kb.ps([64, 128], BF16, name=f"g_pKo{i}") for i in range(2)]
    pO = kb.ps([64, 1024], name="g_pO")
    pU = kb.ps([128, 256], name="g_pU")
    pZ = kb.ps([128, T], name="g_pZ")
    qv = qT_d.rearrange("(h p) t -> p h t", p=128)
    kv = kT_d.rearrange("(h p) t -> p h t", p=128)
    scale = 128 ** -0.5
    cn = 0
    for (t0, ts_) in chunks(L, T):
        kb.dma(q32[:, :, :ts_], qv[:, :, t0:t0+ts_])
        kb.dma(k32[:, :, :ts_], kv[:, :, t0:t0+ts_])
        for blk in range(4):
            src = blk ^ 1
            kb.dma(qsw[blk*32:(blk+1)*32, :, :ts_], qv[src*32:(src+1)*32, :, t0:t0+ts_])
            kb.dma(ksw[blk*32:(blk+1)*32, :, :ts_], kv[src*32:(src+1)*32, :, t0:t0+ts_])
        kb.dma(rc[:, :ts_], rc_d[:, t0:t0+ts_]); kb.dma(rs[:, :ts_], rs_d[:, t0:t0+ts_])
        kb.dma(lr[:, :ts_], lrT_d[:, t0:t0+ts_])
        for h in range(H):
            kb.mm(pZ[:, :ts_], wg[:, h*128:(h+1)*128], lr[:, :ts_])
            kb.act(la[:, h, :ts_], pZ[:, :ts_], AF.Sigmoid, bias=bg[:, h:h+1])
        for h in range(H):
            kb.act(la[:, h, :ts_], la[:, h, :ts_], AF.Ln)
            kb.scan(b16[:, h, :ts_], rm[:, :ts_], la[:, h, :ts_], 0.0)
        for h in range(H):
            kb.act(eb[:, h, :ts_], b16[:, h, :ts_], AF.Exp, scale=1.0 / 16)
            kb.act(enb[:, h, :ts_], b16[:, h, :ts_], AF.Exp, scale=-1.0 / 16)
        for h in range(H):
            kb.tt(t1[:, h, :ts_], q32[:, h, :ts_], rc[:, :ts_], ALU.mult)
            kb.tt(t2[:, h, :ts_], qsw[:, h, :ts_], rs[:, :ts_], ALU.mult, e='pool')
            kb.tt(t1[:, h, :ts_], t1[:, h, :ts_], t2[:, h, :ts_], ALU.add)
            kb.stt(qinb[:, h, :ts_], t1[:, h, :ts_], scale, eb[:, h, :ts_], ALU.mult, ALU.mult)
        for h in range(H):
            kb.tt(t1[:, h, :ts_], k32[:, h, :ts_], rc[:, :ts_], ALU.mult)
            kb.tt(t2[:, h, :ts_], ksw[:, h, :ts_], rs[:, :ts_], ALU.mult, e='pool')
            kb.tt(t1[:, h, :ts_], t1[:, h, :ts_], t2[:, h, :ts_], ALU.add)
            kb.tt(kin32[:, h, :ts_], t1[:, h, :ts_], enb[:, h, :ts_], ALU.mult)
            kb.copy(kinb[:, h, :ts_], kin32[:, h, :ts_], e='pool')
        for c in range(ts_ // 64):
            c0 = c * 64
            vs, vbb, ob = vst[cn % 2], vb[cn % 2], osb[cn % 2]
            kb.dma(vs.all, v_d[t0+c0:t0+c0+64, :])
            kb.copy(vbb.all, vs.all, e='pool')
            for h in range(H):
                i2 = (cn * 4 + h) % 2
                dec = eb[:, h, c0+63:c0+64]
                kb.mm(pAtt[i2].all, kinb[:, h, c0:c0+64], qinb[:, h, c0:c0+64])
                kb.tt(attb[i2].all, pAtt[i2].all, cmask.all, ALU.mult)
                kb.ts(kout[i2].all, kin32[:, h, c0:c0+64], dec, ALU.mult, e='pool')
                kb.transpose(pKo[i2].all, kout[i2].all, ident.all)
                kb.copy(koT[i2].all, pKo[i2].all, e='act')
                kb.mm(pO[:, h*256:(h+1)*256], attb[i2].all, vbb[:, h*256:(h+1)*256], start=True, stop=False)
                kb.mm(pO[:, h*256:(h+1)*256], qinb[:, h, c0:c0+64], Sb[:, h, :], start=False, stop=True)
                kb.mm(pU.all, koT[i2].all, vbb[:, h*256:(h+1)*256])
                kb.stt(S32[:, h, :], S32[:, h, :], dec, pU.all, ALU.mult, ALU.add)
                kb.copy(Sb[:, h, :], S32[:, h, :], e='act')
            kb.copy(ob[:, 0:512], pO[:, 0:512], e='act')
            kb.copy(ob[:, 512:1024], pO[:, 512:1024], e='dve')
            kb.dma(o_d[t0+c0:t0+c0+64, :], ob.all, is_output=True)
            cn += 1

def rope_tables(pos_row, pos_col, is_ctx):
    nf = 32
    inv = (10000.0 ** (-np.arange(nf, dtype=np.float32) / nf)).astype(np.float32)
    L = len(pos_row)
    C = np.ones((128, L), np.float32); S = np.zeros((128, L), np.float32)
    for half, pos in enumerate([pos_row, pos_col]):
        ang = pos.astype(np.float32)[None, :] * inv[:, None]
        c = np.cos(ang).astype(np.float32); s = np.sin(ang).astype(np.float32)
        C[half*64:half*64+32] = c; C[half*64+32:half*64+64] = c
        S[half*64:half*64+32] = -s; S[half*64+32:half*64+64] = s
    C[:, is_ctx] = 1.0; S[:, is_ctx] = 0.0
    return C, S

def gla_ref(q, k, v, lowrank, wg, bg, C, S):
    L = q.shape[0]
    def rope(z):
        sw = z.reshape(L, 4, 2, 2, 32)[:, :, :, ::-1, :].reshape(L, 4, 128)
        return z * C.T[:, None, :] + sw * S.T[:, None, :]
    q = rope(q) * 128 ** -0.5; k = rope(k)
    z = lowrank @ wg + bg
    la = -np.logaddexp(0, -z) / 16.0
    a = np.exp(la).reshape(L, 4, 128)
    St = np.zeros((4, 128, 256)); o = np.zeros((L, 4, 256))
    for t in range(L):
        St = a[t][:, :, None] * St + k[t][:, :, None] * v[t][:, None, :]
        o[t] = np.einsum('hd,hde->he', q[t], St)
    return o


def build_moe(NTOK=8704, NE=8, F=1024, D=2048):
    kb = KB(); nc = kb.nc
    KT = D // 128; FT = F // 128
    hT_d = kb.dram_in("hT", [D, NTOK], BF16)
    cwT_d = kb.dram_in("cwT", [NE, NTOK])
    wg_d = kb.dram_in("wg", [NE, D, F]); wu_d = kb.dram_in("wu", [NE, D, F]); wd_d = kb.dram_in("wd", [NE, F, D])
    yT_d = kb.dram_out("yT", [D, NTOK], BF16)
    sg_d = nc.dram_tensor("scr_g", [NE * FT, 128, KT * 128], BF16, kind="Internal").ap()
    su_d = nc.dram_tensor("scr_u", [NE * FT, 128, KT * 128], BF16, kind="Internal").ap()
    sd_d = nc.dram_tensor("scr_d", [NE * FT, 128, D], BF16, kind="Internal").ap()
    hv = hT_d.rearrange("(kt p) t -> p kt t", p=128)
    yv = yT_d.rearrange("(kt p) t -> p kt t", p=128)
    mk = kb.mark()
    st32 = [kb.sb([128, KT * 128], name=f"m_st32_{i}") for i in range(6)]
    st16 = [kb.sb([128, KT * 128], BF16, name=f"m_st16_{i}") for i in range(6)]
    engs = ['act', 'dve', 'pool']
    n = 0
    for e in range(NE):
        for f in range(FT):
            for (src, dst) in ((wg_d, sg_d), (wu_d, su_d)):
                i = n % 6; n += 1
                a32 = V(st32[i], st32[i].t[:, :].rearrange("p (kt n) -> p kt n", n=128))
                kb.dma(a32, src[e, :, f*128:(f+1)*128].rearrange("(kt p) n -> p kt n", p=128))
                kb.copy(st16[i].all, st32[i].all, e=engs[n % 3])
                kb.dma(dst[e*FT+f], st16[i].all, e='pool')
            i = n % 6; n += 1
            kb.dma(st32[i].all, wd_d[e, f*128:(f+1)*128, :])
            kb.copy(st16[i].all, st32[i].all, e=engs[n % 3])
            kb.dma(sd_d[e*FT+f], st16[i].all, e='pool')
    kb.release(mk)
    hT = [kb.sb([128, KT, 512], BF16, name=f"m_hT{i}") for i in range(2)]
    aT = kb.sb([128, NE * FT, 512], BF16, name="m_aT")
    cwb = [kb.sb([128, 512], name=f"m_cwb{i}") for i in range(2)]
    wgb = [kb.sb([128, KT, 128], BF16, name=f"m_wgb{i}") for i in range(3)]
    wub = [kb.sb([128, KT, 128], BF16, name=f"m_wub{i}") for i in range(3)]
    wdb = [kb.sb([128, 512], BF16, name=f"m_wdb{i}") for i in range(4)]
    sg = [kb.sb([128, 512], name=f"m_sg{i}") for i in range(2)]
    t1 = [kb.sb([128, 512], name=f"m_t1{i}") for i in range(2)]
    ysb = [kb.sb([128, 512], BF16, name=f"m_ysb{i}") for i in range(2)]
    pG = [kb.ps([128, 512], name=f"m_pG{i}") for i in range(2)]
    pU = [kb.ps([128, 512], name=f"m_pU{i}") for i in range(2)]
    pY = [kb.ps([128, 512], name=f"m_pY{i}") for i in range(4)]
    sgv = sg_d.rearrange("s p (kt n) -> s p kt n", n=128)
    suv = su_d.rearrange("s p (kt n) -> s p kt n", n=128)
    n = 0; nd = 0; ny = 0
    for ci, (c0, cs) in enumerate(chunks(NTOK)):
        h = hT[ci % 2]
        kb.dma(h[:, :, :cs], hv[:, :, c0:c0+cs])
        for e in range(NE):
            cw = cwb[e % 2]
            kb.dma(cw[:, :cs], cwT_d[e:e+1, c0:c0+cs].broadcast_to([128, cs]))
            for f in range(FT):
                i = n % 2; j3 = n % 3; n += 1
                kb.dma(wgb[j3].all, sgv[e*FT+f], e='sp')
                kb.dma(wub[j3].all, suv[e*FT+f], e='pool')
                for kt in range(KT):
                    kb.mm(pG[i][:, :cs], wgb[j3][:, kt, :], h[:, kt, :cs], start=(kt == 0), stop=(kt == KT-1))
                for kt in range(KT):
                    kb.mm(pU[i][:, :cs], wub[j3][:, kt, :], h[:, kt, :cs], start=(kt == 0), stop=(kt == KT-1))
                kb.act(sg[i][:, :cs], pG[i][:, :cs], AF.Silu)
                kb.tt(t1[i][:, :cs], sg[i][:, :cs], pU[i][:, :cs], ALU.mult)
                kb.tt(aT[:, e*FT+f, :cs], t1[i][:, :cs], cw[:, :cs], ALU.mult)
        for dq in range(D // 512):
            for ef in range(NE * FT):
                j = nd % 4; nd += 1
                kb.dma(wdb[j].all, sd_d[ef, :, dq*512:(dq+1)*512], e='sp' if nd % 2 else 'pool')
                for dd in range(4):
                    kb.mm(pY[dd][:, :cs], wdb[j][:, dd*128:(dd+1)*128], aT[:, ef, :cs], start=(ef == 0), stop=(ef == NE*FT-1))
            for dd in range(4):
                yo = ysb[ny % 2]; ny += 1
                kb.copy(yo[:, :cs], pY[dd][:, :cs], e='act')
                kb.dma(yv[:, dq*4+dd, c0:c0+cs], yo[:, :cs], is_output=True)
    kb.finish()
    return kb

def np_silu(x): return x / (1 + np.exp(-x))


NEGM = -1.0e4

def qrows_for_keyrow(rp, rows=64, kh=8):
    res = []
    for r in range(rows):
        st = min(max(r - kh // 2, 0), rows - kh)
        if st <= rp <= st + kh - 1:
            res.append(r)
    return res

def start_of(r, rows=64, kh=8):
    return min(max(r - kh // 2, 0), rows - kh)

def build_attn(NH=8):
    kb = KB()
    W = 64; ROWS = 64; LL = 4096; LC = 256
    qT_d = kb.dram_in("qT", [NH, 128, LL], BF16)
    kT_d = kb.dram_in("kT", [NH, 128, LL + LC], BF16)
    v_d = kb.dram_in("v", [LL + LC, NH, 128], BF16)
    bias_d = kb.dram_in("biasT", [NH, 64, 15 * 64])
    o_d = kb.dram_out("o", [LL, NH * 128], BF16)
    scale = 128 ** -0.5
    qT = [kb.sb([128, LL], BF16, name=f"a_qT{i}") for i in range(2)]
    kT = [kb.sb([128, LL + LC], BF16, name=f"a_kT{i}") for i in range(2)]
    Vl = [kb.sb([64, ROWS, 129], BF16, name=f"a_Vl{i}") for i in range(2)]
    Vc = [kb.sb([128, 2, 129], BF16, name=f"a_Vc{i}") for i in range(2)]
    bias = [kb.sb([64, 15 * 64], name=f"a_bias{i}") for i in range(2)]
    for i in range(2):
        kb.memset(Vl[i][:, :, 128:129], 1.0); kb.memset(Vc[i][:, :, 128:129], 1.0)
    RING = 16
    PT = [kb.sb([64, 15 * 64], BF16, name=f"a_PT{i}") for i in range(RING)]
    PcT = [[kb.sb([128, 512], BF16, name=f"a_Pc{g}_{ct}") for ct in range(2)] for g in range(2)]
    tmp = [kb.sb([64, 512], name=f"a_tmp{i}") for i in range(2)]
    osb = [kb.sb([64, 8, 128], BF16, name=f"a_osb{i}") for i in range(2)]
    rec = [kb.sb([64, 1], name=f"a_rec{i}") for i in range(2)]
    pS = [kb.ps([64, 512], name=f"a_pS{i}") for i in range(2)]
    pC = [kb.ps([128, 512], name=f"a_pC{i}") for i in range(2)]
    pO = [kb.ps([64, 129], name=f"a_pO{i}") for i in range(2)]
    vlat = v_d[0:LL].rearrange("(r c) h d -> c r h d", c=64)
    vctx = v_d[LL:LL + LC].rearrange("(t p) h d -> p t h d", p=128)
    ov = o_d.rearrange("(r c) f -> c r f", c=64)
    ns = 0; no = 0
    for h in range(NH):
        b = h % 2
        kb.dma(qT[b].all, qT_d[h]); kb.dma(kT[b].all, kT_d[h])
        kb.dma(Vl[b][:, :, 0:128], vlat[:, :, h, :]); kb.dma(Vc[b][:, :, 0:128], vctx[:, :, h, :])
        kb.dma(bias[b].all, bias_d[h])
        done_ctx = set()
        for rp in range(ROWS):
            qr = qrows_for_keyrow(rp)
            lo, hi = qr[0], qr[-1]
            ptile = PT[rp % RING]
            for p0 in range(lo, hi + 1, 8):
                p1 = min(p0 + 8, hi + 1); n = p1 - p0
                S = pS[ns % 2]; t = tmp[ns % 2]; ns += 1
                kb.mm(S[:, :n*64], kT[b][:, rp*64:(rp+1)*64], qT[b][:, p0*64:p1*64])
                j0 = p0 - rp + 7
                kb.stt(t[:, :n*64], S[:, :n*64], scale, bias[b][:, j0*64:(j0+n)*64], ALU.mult, ALU.add)
                kb.act(ptile[:, j0*64:(j0+n)*64], t[:, :n*64], AF.Exp)
            for r in range(ROWS):
                st = start_of(r)
                if st + 7 != rp:
                    continue
                g = r // 8
                if g not in done_ctx:
                    done_ctx.add(g)
                    for ct in range(2):
                        C = pC[ct]
                        kb.mm(C.all, kT[b][:, LL+ct*128:LL+(ct+1)*128], qT[b][:, g*512:(g+1)*512])
                        kb.act(PcT[g % 2][ct].all, C.all, AF.Exp, scale=scale)
                O = pO[no % 2]; rc = rec[no % 2]; no += 1
                for i, rk in enumerate(range(st, st + 8)):
                    j = r - rk + 7
                    kb.mm(O.all, PT[rk % RING][:, j*64:(j+1)*64], Vl[b][:, rk, :], start=(i == 0), stop=False)
                for ct in range(2):
                    kb.mm(O.all, PcT[g % 2][ct][:, (r % 8)*64:(r % 8 + 1)*64], Vc[b][:, ct, :], start=False, stop=(ct == 1))
                kb.recip(rc.all, O[:, 128:129])
                ob = osb[g % 2]
                kb.ts(ob[:, r % 8, :], O[:, 0:128], rc[:, 0:1], ALU.mult)
                if r % 8 == 7:
                    kb.dma(ov[:, g*8:(g+1)*8, h*128:(h+1)*128], ob.all, is_output=True)
    kb.finish()
    return kb

def host_bias(rpb):
    H = rpb.shape[0]
    col = np.arange(64)
    cs = np.clip(col - 8, 0, 48)
    col_ok = (col[None, :] >= cs[:, None]) & (col[None, :] < cs[:, None] + 16)
    dc = np.clip(col[None, :] - col[:, None] + 15, 0, 30)
    out = np.empty((H, 64, 15, 64), np.float32)
    for j in range(15):
        dr = 14 - j
        bq = rpb[:, dr, :][:, dc]
        bq = np.where(col_ok[None], bq, np.float32(NEGM))
        out[:, :, j, :] = bq.transpose(0, 2, 1)
    return out.reshape(H, 64, 15 * 64)

def attn_ref(q, k, v, kc, vc, rpb):
    scale = 128 ** -0.5
    out = np.zeros((4096, 128))
    col = np.arange(64); cs = np.clip(col - 8, 0, 48)
    col_ok = (col[None, :] >= cs[:, None]) & (col[None, :] < cs[:, None] + 16)
    dc = np.clip(col[None, :] - col[:, None] + 15, 0, 30)
    for r in range(64):
        st = start_of(r)
        qr = q[r*64:(r+1)*64]
        kb_ = k[st*64:(st+8)*64]; vb = v[st*64:(st+8)*64]
        dr = st + np.arange(8) - r + 7
        bias = rpb[dr][:, dc]
        bias = np.where(col_ok[None], bias, -1e30).transpose(1, 0, 2).reshape(64, 512)
        s = np.concatenate([qr @ kb_.T * scale + bias, qr @ kc.T * scale], -1)
        p = np.exp(s - s.max(-1, keepdims=True)); p /= p.sum(-1, keepdims=True)
        out[r*64:(r+1)*64] = p[:, :512] @ vb + p[:, 512:] @ vc
    return out


def combine_y(kb, x_d, yg_d, out_tr, outv, vt, NT, segs_g2, ngroups=4, tag="cy"):
    xv = x_d.rearrange("(kt p) t -> p kt t", p=128)
    yv = yg_d.rearrange("g (kt p) t -> p g kt t", p=128)
    xt = [kb.sb([128, 512], name=f"{tag}_x{i}") for i in range(2)]
    yt = [kb.sb([128, ngroups, 512], BF16, name=f"{tag}_y{i}") for i in range(2)]
    ys = [kb.sb([128, 2, 512], name=f"{tag}_ys{i}") for i in range(2)]
    n = 0
    for (s0, sz, g2c) in segs_g2:
        for (c0, cs) in chunks(sz):
            a = s0 + c0
            for kt in range(16):
                x, y, s2 = xt[n % 2], yt[n % 2], ys[n % 2]; n += 1
                kb.dma(x[:, :cs], xv[:, kt, a:a+cs])
                kb.dma(y[:, :, :cs], yv[:, :, kt, a:a+cs])
                kb.tt(s2[:, 0, :cs], y[:, 0, :cs], y[:, 1, :cs], ALU.add)
                kb.tt(s2[:, 1, :cs], y[:, 2, :cs], y[:, 3, :cs], ALU.add, e='pool')
                kb.tt(s2[:, 0, :cs], s2[:, 0, :cs], s2[:, 1, :cs], ALU.add)
                kb.stt(x[:, :cs], s2[:, 0, :cs], vt[:, kt, g2c:g2c+1], x[:, :cs], ALU.mult, ALU.add)
                kb.dma(V(out_tr.tile, outv[:, kt, a:a+cs]), x[:, :cs], is_output=True)

def build_front1(NT=2176, NLAT=2048):
    kb = KB()
    x1T_d = kb.dram_in("x1T", [2048, NT]); yg_d = kb.dram_in("ygT", [4, 2048, NT], BF16)
    vecs_d = kb.dram_in("vecs", [2048, 7]); qkg_d = kb.dram_in("qkg", [128, 2]); W_d = kb.dram_in("wqkv", [2048, 6144])
    x2T_d = kb.dram_out("x2T", [2048, NT])
    outs = [kb.dram_out(n, [2048, NT], BF16) for n in ["qT", "kT", "vT"]]
    c = consts(kb)
    vt = kb.sb([128, 16, 7], name="vecs_sb"); kb.dma(vt.all, vecs_d.rearrange("(kt p) v -> p kt v", p=128))
    qkg = kb.sb([128, 2], name="qkg_sb"); kb.dma(qkg.all, qkg_d)
    x2tr = dtrack(kb, x2T_d, "x2T_dram"); x2v = x2T_d.rearrange("(kt p) t -> p kt t", p=128)
    combine_y(kb, x1T_d, yg_d, x2tr, x2v, vt, NT, [(0, NLAT, 0), (NLAT, NT - NLAT, 1)])
    hT = kb.sb([128, 16, NT], BF16, name="hT")
    def x_src(kt, a, cs, dst):
        kb.dma(dst, V(x2tr.tile, x2v[:, kt, a:a+cs]))
    def sink(a, cs, h32):
        kb.copy(hT[:, :, a:a+cs], h32[:, :, :cs], e='pool')
    norm_mod2(kb, c, x_src, sink, vt, [(0, NLAT, 2, 3), (NLAT, NT - NLAT, 4, 5)], 6, tag="n1")
    ost = [kb.sb([128, NT], BF16, name=f"f1_ost{i}") for i in range(2)]
    sq = [kb.sb([128, 512], name=f"f1_sq{i}") for i in range(2)]
    rstd = [kb.sb([128, 512], name=f"f1_rstd{i}") for i in range(2)]
    pN = kb.ps([128, 512], name="f1_pN")
    st = {'n': 0}
    def evac(j, nsz, c0, cs, pv):
        o = ost[j % 2]; i = st['n'] % 2; st['n'] += 1
        which = j // 16
        if which < 2:
            kb.act(sq[i][:, :cs], pv, AF.Square)
            kb.mm(pN[:, :cs], c['ones'].all, sq[i][:, :cs])
            kb.act(rstd[i][:, :cs], pN[:, :cs], AF.Sqrt, bias=c['eps'].all, scale=1.0 / 128)
            kb.recip(rstd[i][:, :cs], rstd[i][:, :cs])
            kb.stt(o[:, c0:c0+cs], pv, qkg[:, which:which+1], rstd[i][:, :cs], ALU.mult, ALU.mult)
        else:
            kb.copy(o[:, c0:c0+cs], pv, e='act')
        if c0 + cs == NT:
            jj = j % 16
            kb.dma(outs[which][jj*128:(jj+1)*128, :], o.all, is_output=True)
    proj(kb, hT, W_d, NT, 6144, 16, evac, tag="qkv")
    kb.finish()
    return kb

def build_back1(NT=2048):
    kb = KB()
    oT_d = kb.dram_in("oT", [2048, NT], BF16); xT_d = kb.dram_in("xT", [2048, NT])
    vecs_d = kb.dram_in("vecs", [2048, 4])
    wout_d = kb.dram_in("wout", [2048, 2048]); wr_d = kb.dram_in("wr", [2048, 36])
    x1T_d = kb.dram_out("x1T", [2048, NT]); h2T_d = kb.dram_out("h2T", [2048, NT], BF16); cw_d = kb.dram_out("cw", [NT, 32])
    c = consts(kb)
    vt = kb.sb([128, 16, 4], name="vecs_sb"); kb.dma(vt.all, vecs_d.rearrange("(kt p) v -> p kt v", p=128))
    aT = kb.sb([128, 16, NT], BF16, name="aT")
    ov = oT_d.rearrange("(kt p) t -> p kt t", p=128)
    for kt in range(16):
        kb.dma(aT[:, kt, :], ov[:, kt, :])
    back_tail(kb, c, aT, 16, wout_d, xT_d, x1T_d, h2T_d, cw_d, vt, wr_d, NT, [(0, NT, 1, 2)], [0], 3)
    kb.finish()
    return kb

def build_final(NT=2048):
    kb = KB()
    xT_d = kb.dram_in("xT", [2048, NT]); yg_d = kb.dram_in("ygT", [4, 2048, NT], BF16); vecs_d = kb.dram_in("vecs", [128, 16])
    out_d = kb.dram_out("outT", [2048, NT])
    vt = kb.sb([128, 16, 1], name="vecs_sb"); kb.dma(vt[:, :, 0], vecs_d)
    otr = dtrack(kb, out_d, "out_dram"); ov = out_d.rearrange("(kt p) t -> p kt t", p=128)
    combine_y(kb, xT_d, yg_d, otr, ov, vt, NT, [(0, NT, 0)])
    kb.finish()
    return kb


def build_ada():
    kb = KB()
    NCOL = 1536
    cT = kb.dram_in("cT", [2048, 5])
    w = kb.dram_in("w", [2, 2048, NCOL])
    b = kb.dram_in("b", [2, 1, NCOL])
    out = kb.dram_out("mod", [2, 5, NCOL])
    ct = kb.sb([128, 16, 5]); st = kb.sb([128, 16, 5])
    kb.dma(ct.all, cT.rearrange("(kt p) m -> p kt m", p=128))
    kb.act(st.all, ct.all, AF.Silu)
    wpool = [kb.sb([128, NCOL], name=f"wp{i}") for i in range(4)]
    wi = 0
    for l in range(2):
        bt = kb.sb([5, NCOL])
        kb.dma(bt.all, b[l].broadcast_to([5, NCOL]))
        ot = kb.sb([5, NCOL])
        pss = [kb.ps([5, 512]) for _ in range(3)]
        for kt in range(16):
            wt = wpool[wi % 4]; wi += 1
            kb.dma(wt.all, w[l, kt*128:(kt+1)*128, :])
            for c in range(3):
                kb.mm(pss[c].all, st[:, kt, :], wt[:, c*512:(c+1)*512], start=(kt == 0), stop=(kt == 15))
        for c in range(3):
            kb.tt(ot[:, c*512:(c+1)*512], pss[c].all, bt[:, c*512:(c+1)*512], ALU.add)
        kb.dma(out[l], ot.all, is_output=True)
    kb.finish()
    return kb


import numpy as np

def build_s5gla(L=4352):
    kb = KB()
    uT_d = kb.dram_in("uT", [1024, L]); prm_d = kb.dram_in("prm", [128, 32, 3])
    bT_d = kb.dram_in("bT", [32, 32, 2, 128]); cT_d = kb.dram_in("cT", [128, 32, 2, 32])
    yT_d = kb.dram_out("yT", [1024, L])
    qT_d = kb.dram_in("qT", [512, L]); kT_d = kb.dram_in("kT", [512, L]); v_d = kb.dram_in("v", [L, 1024])
    lrT_d = kb.dram_in("lrT", [16, L]); wg_d = kb.dram_in("wg", [16, 512]); bg_d = kb.dram_in("bg", [128, 4])
    rc_d = kb.dram_in("rc", [128, L]); rs_d = kb.dram_in("rs", [128, L])
    cm_d = kb.dram_in("cmask", [64, 64]); id_d = kb.dram_in("identm", [128, 128])
    o_d = kb.dram_out("o", [L, 1024])
    mk = kb.mark()
    s5_part(kb, uT_d, prm_d, bT_d, cT_d, yT_d, L, 32)
    kb.release(mk)
    gla_part(kb, qT_d, kT_d, v_d, lrT_d, wg_d, bg_d, rc_d, rs_d, cm_d, id_d, o_d, L)
    kb.finish()
    return kb

def _c(a):
    return np.ascontiguousarray(a)

def kernel(x, c, ctx, c_ctx, ada_w, ada_b, norm1_g, norm2_g, ab_w_in, ab_w_out,
           s5_lam_re, s5_lam_im, s5_log_dt, s5_b_re, s5_b_im, s5_c_re, s5_c_im, s5_d, s5_w_glu,
           gla_w_gate2, gla_b_gate, gla_norm_g, na_w_qkv, na_w_out, na_q_norm, na_k_norm, na_rpb,
           moe_w_route_group, moe_w_route_expert, moe_w_gate, moe_w_up, moe_w_down):
    f32 = np.float32
    A = lambda a: np.asarray(a, dtype=f32) if np.asarray(a).dtype != f32 else np.asarray(a)
    x = A(x); c = A(c); ctx = A(ctx); c_ctx = A(c_ctx)
    B, SEQ, D = x.shape; CTX = ctx.shape[1]
    NC = 8; HL = SEQ // 2; HC = CTX // 2; NT = HL + HC
    cT = _c(np.concatenate([c.T, c_ctx[:, None]], axis=1))
    ada_w = A(ada_w); ada_b = A(ada_b)
    res = run(build_ada(), [{"cT": cT, "w": _c(ada_w[:, :, i*1536:(i+1)*1536]), "b": _c(ada_b[:, None, i*1536:(i+1)*1536])}
                            for i in range(NC)])
    mod = np.concatenate([r["mod"] for r in res], axis=2)
    def modv(l, k, row):
        return mod[l, row, k*D:(k+1)*D]
    cores = [(cc // 2, cc % 2) for cc in range(NC)]
    xT_core = []
    for (b, h) in cores:
        xT_core.append(_c(np.concatenate([x[b, h*HL:(h+1)*HL], ctx[b, h*HC:(h+1)*HC]], axis=0).T))
    w_in = A(ab_w_in)[0]
    ims = []
    for ci, (b, h) in enumerate(cores):
        vecs = np.stack([modv(0, 0, b), modv(0, 1, b), modv(0, 0, 4), modv(0, 1, 4), A(norm1_g)[0]], axis=1)
        ims.append({"xT": xT_core[ci], "vecs": _c(vecs), "W": w_in})
    res = run(build_front(NT, HL, 4128), ims)
    pT = [r["pT"] for r in res]
    px_lat = [np.concatenate([pT[2*b][:, :HL], pT[2*b+1][:, :HL]], axis=1) for b in range(B)]
    px_ctx = [np.concatenate([pT[2*b][:, HL:], pT[2*b+1][:, HL:]], axis=1) for b in range(B)]
    def seq_T(b, d, rows):
        cpart = px_ctx[b][rows]; lpart = px_lat[b][rows]
        if d == 1:
            cpart = cpart[:, ::-1]; lpart = lpart[:, ::-1]
        return np.concatenate([cpart, lpart], axis=1)
    L = CTX + SEQ
    ims = []
    for cc in range(NC):
        b, d = cc // 2, cc % 2
        prm, bT, cTt = s5_host_params(A(s5_lam_re)[0, d], A(s5_lam_im)[0, d], A(s5_log_dt)[0, d], A(s5_b_re)[0, d], A(s5_b_im)[0, d],
                                      A(s5_c_re)[0, d], A(s5_c_im)[0, d], NGP=32)
        ims.append({"uT": _c(seq_T(b, d, slice(0, 1024))), "prm": prm, "bT": bT, "cT": cTt})
    ims_s5 = ims
    o0 = 1024
    pos = np.arange(SEQ)
    cmask = np.triu(np.ones((64, 64), f32)); identm = np.eye(128, dtype=f32)
    ims = []
    for cc in range(NC):
        b, d = cc // 2, cc % 2
        p_lat = pos[::-1] if d == 1 else pos
        prow = np.concatenate([np.zeros(CTX, np.int64), p_lat // 64]); pcol = np.concatenate([np.zeros(CTX, np.int64), p_lat % 64])
        is_ctx = np.arange(L) < CTX
        rc, rs = rope_tables(prow, pcol, is_ctx)
        ims.append({**ims_s5[cc], "qT": _c(seq_T(b, d, slice(o0, o0 + 512))), "kT": _c(seq_T(b, d, slice(o0 + 512, o0 + 1024))),
                    "v": _c(seq_T(b, d, slice(o0 + 1024, o0 + 2048)).T),
                    "lrT": _c(seq_T(b, d, slice(o0 + 3072 + d*16, o0 + 3072 + (d+1)*16))),
                    "wg": _c(A(gla_w_gate2)[0, d]), "bg": _c(A(gla_b_gate)[0, d].reshape(4, 128).T),
                    "rc": rc, "rs": rs, "cmask": cmask, "identm": identm})
    res = run(build_s5gla(L), ims)
    def unflip(a, d):
        cpart, lpart = a[:, :CTX], a[:, CTX:]
        if d == 1:
            cpart = cpart[:, ::-1]; lpart = lpart[:, ::-1]
        return cpart, lpart
    yS = [[unflip(res[2*b+d]["yT"], d) for d in range(2)] for b in range(B)]
    oG = [[unflip(_c(res[2*b+d]["o"].T), d) for d in range(2)] for b in range(B)]
    def tokpart(pair, h):
        return np.concatenate([pair[1][:, h*HL:(h+1)*HL], pair[0][:, h*HC:(h+1)*HC]], axis=1)
    wr0 = _c(np.concatenate([A(moe_w_route_group)[0], A(moe_w_route_expert)[0]], axis=1))
    v8 = _c(np.stack([A(s5_d)[0], A(gla_norm_g)[0]], axis=1))
    ims = []
    for ci, (b, h) in enumerate(cores):
        vecs = np.stack([modv(0, 2, b), modv(0, 2, 4), modv(0, 3, b), modv(0, 4, b), modv(0, 3, 4), modv(0, 4, 4), A(norm2_g)[0]], axis=1)
        pxp = (px_ctx[b], px_lat[b])
        ims.append({"yfT": _c(tokpart(yS[b][0], h)), "ybT": _c(tokpart(yS[b][1], h)),
                    "uT": _c(tokpart((pxp[0][0:1024], pxp[1][0:1024]), h)),
                    "ofT": _c(tokpart(oG[b][0], h)), "obT": _c(tokpart(oG[b][1], h)),
                    "rT": _c(tokpart((pxp[0][o0+2048:o0+3072], pxp[1][o0+2048:o0+3072]), h)),
                    "xT": xT_core[ci], "vecs": _c(vecs), "v8": v8, "wglu": A(s5_w_glu)[0], "wout": A(ab_w_out)[0], "wr": wr0})
    res = run(build_back0(NT, HL), ims)
    x1T = [r["x1T"] for r in res]; h2T = [r["h2T"] for r in res]; cw = [r["cw"] for r in res]
    del pT, px_lat, px_ctx, yS, oG, ims
    def run_moe(layer, h2T, cw, ntok_core):
        NTOK = 4 * ntok_core
        ims = []
        for cc in range(NC):
            g, hh = cc // 2, cc % 2
            hT = _c(np.concatenate([h2T[4*hh + i] for i in range(4)], axis=1))
            cwT = _c(np.concatenate([cw[4*hh + i] for i in range(4)], axis=0)[:, g*8:(g+1)*8].T)
            ims.append({"hT": hT, "cwT": cwT, "wg": A(moe_w_gate)[layer, g*8:(g+1)*8], "wu": A(moe_w_up)[layer, g*8:(g+1)*8],
                        "wd": A(moe_w_down)[layer, g*8:(g+1)*8]})
        res = run(build_moe(NTOK, 8, 1024, 2048), ims)
        out = []
        for ci in range(NC):
            hh, i = ci // 4, ci % 4
            out.append(_c(np.stack([res[2*g + hh]["yT"][:, i*ntok_core:(i+1)*ntok_core] for g in range(4)], axis=0)))
        return out
    yg0 = run_moe(0, h2T, cw, NT)
    qkg = _c(np.stack([A(na_q_norm)[0], A(na_k_norm)[0]], axis=1))
    ims = []
    for ci, (b, h) in enumerate(cores):
        vecs = np.stack([modv(0, 5, b), modv(0, 5, 4), modv(1, 0, b), modv(1, 1, b), modv(1, 0, 4), modv(1, 1, 4), A(norm1_g)[1]], axis=1)
        ims.append({"x1T": x1T[ci], "ygT": yg0[ci], "vecs": _c(vecs), "qkg": qkg, "wqkv": A(na_w_qkv)[0]})
    res = run(build_front1(NT, HL), ims)
    x2T = [_c(r["x2T"][:, :HL]) for r in res]
    def full(name, b):
        return np.concatenate([res[2*b][name][:, :HL], res[2*b+1][name][:, :HL], res[2*b][name][:, HL:], res[2*b+1][name][:, HL:]], axis=1)
    del yg0, x1T, h2T, cw
    biasT = host_bias(A(na_rpb)[0])
    ims = []
    for cc in range(NC):
        b, hg = cc // 2, cc % 2
        qf = full("qT", b).reshape(16, 128, L)[hg*8:(hg+1)*8]
        kf = full("kT", b).reshape(16, 128, L)[hg*8:(hg+1)*8]
        vf = full("vT", b).reshape(16, 128, L)[hg*8:(hg+1)*8]
        ims.append({"qT": _c(qf[:, :, :SEQ]), "kT": _c(kf), "v": _c(vf.transpose(2, 0, 1)), "biasT": _c(biasT[hg*8:(hg+1)*8])})
    res = run(build_attn(8), ims)
    wr1 = _c(np.concatenate([A(moe_w_route_group)[1], A(moe_w_route_expert)[1]], axis=1))
    ims = []
    for ci, (b, h) in enumerate(cores):
        oT = _c(np.concatenate([res[2*b][ "o"][h*HL:(h+1)*HL], res[2*b+1]["o"][h*HL:(h+1)*HL]], axis=1).T)
        vecs = np.stack([modv(1, 2, b), modv(1, 3, b), modv(1, 4, b), A(norm2_g)[1]], axis=1)
        ims.append({"oT": oT, "xT": x2T[ci], "vecs": _c(vecs), "wout": A(na_w_out)[0], "wr": wr1})
    res = run(build_back1(HL), ims)
    x3T = [r["x1T"] for r in res]; h2T = [r["h2T"] for r in res]; cw = [r["cw"] for r in res]
    yg1 = run_moe(1, h2T, cw, HL)
    ims = []
    for ci, (b, h) in enumerate(cores):
        ims.append({"xT": x3T[ci], "ygT": yg1[ci], "vecs": _c(modv(1, 5, b).reshape(16, 128).T)})
    res = run(build_final(HL), ims)
    out = np.empty((B, SEQ, D), f32)
    for ci, (b, h) in enumerate(cores):
        out[b, h*HL:(h+1)*HL] = res[ci]["outT"].T
    return out
```

```python
import math, time, sys


import sys
import numpy as np
import concourse.bass as bass
import concourse.mybir as mybir
from concourse.bass_utils import run_bass_kernel_spmd

F32 = mybir.dt.float32
BF16 = mybir.dt.bfloat16
I32 = mybir.dt.int32
AF = mybir.ActivationFunctionType
ALU = mybir.AluOpType
AX = mybir.AxisListType


class V:
    def __init__(self, tile, ap):
        self.tile = tile
        self.ap = ap

    def __getitem__(self, idx):
        return V(self.tile, self.ap[idx])


class T:
    def __init__(self, kb, tensor, name):
        self.kb = kb
        self.t = tensor
        self.name = name
        self.writes = {}
        self.reads = {}

    def __getitem__(self, idx):
        return V(self, self.t[idx])

    @property
    def all(self):
        return V(self, self.t[:])


def _ap(x):
    return x.ap if isinstance(x, V) else x


class KB:
    NDMA = 28

    def __init__(self):
        self.nc = bass.Bass("TRN2", target_bir_lowering=False)
        nc = self.nc
        self.eng = {'pe': nc.tensor, 'act': nc.scalar, 'dve': nc.vector, 'pool': nc.gpsimd, 'sp': nc.sync}
        self.sems = {}
        self.cnt = {}
        for e in ['pe', 'act', 'dve', 'pool']:
            self.sems[e] = nc.alloc_semaphore(name=f"sem_{e}")
            self.cnt[e] = 0
        for i in range(self.NDMA):
            self.sems[('d', i)] = nc.alloc_semaphore(name=f"sem_d{i}")
            self.cnt[('d', i)] = 0
        self.rr = 0
        self.waited = {e: {} for e in self.eng}
        self.out_tokens = []
        self.ntiles = 0
        self.ninstr = 0
        self.cms = []

    def sb(self, shape, dtype=F32, name=None):
        self.ntiles += 1
        name = "s_" + (name or f"t{self.ntiles}")
        cm = self.nc.sbuf_tensor(name, list(shape), dtype)
        t = cm.__enter__()
        self.cms.append(cm)
        return T(self, t, name)

    def mark(self):
        return len(self.cms)

    def barrier(self):
        for e in self.eng:
            for sk, val in self.cnt.items():
                if val > 0:
                    self._wait(e, sk, val)

    def release(self, mark):
        self.barrier()
        while len(self.cms) > mark:
            cm = self.cms.pop()
            cm.__exit__(None, None, None)

    def ps(self, shape, dtype=F32, name=None):
        self.ntiles += 1
        name = "ps_" + (name or f"p{self.ntiles}")
        cm = self.nc.psum_tensor(name, list(shape), dtype)
        t = cm.__enter__()
        self.cms.append(cm)
        return T(self, t, name)

    def dram_in(self, name, shape, dtype=F32):
        return self.nc.dram_tensor(name, list(shape), dtype, kind="ExternalInput").ap()

    def dram_out(self, name, shape, dtype=F32):
        return self.nc.dram_tensor(name, list(shape), dtype, kind="ExternalOutput").ap()

    def _wait(self, e, sk, val):
        w = self.waited[e]
        if w.get(sk, 0) >= val:
            return
        self.eng[e].wait_ge(self.sems[sk], val)
        w[sk] = val

    def issue(self, e, fn, outs, ins, dma=False):
        deps = []
        for v in ins:
            if isinstance(v, V):
                deps.extend(v.tile.writes.items())
        for v in outs:
            if isinstance(v, V):
                deps.extend(v.tile.writes.items())
                deps.extend(v.tile.reads.items())
        for sk, val in deps:
            if sk == e and e == 'pe':
                continue
            self._wait(e, sk, val)
        if dma:
            i = self.rr
            self.rr = (self.rr + 1) % self.NDMA
            sk = ('d', i)
            self._wait(e, sk, self.cnt[sk])
            inst = fn()
            self.cnt[sk] += 16
            inst.then_inc(self.sems[sk], 16)
        else:
            sk = e
            inst = fn()
            self.cnt[sk] += 1
            inst.then_inc(self.sems[sk], 1)
        tok = (sk, self.cnt[sk])
        self.ninstr += 1
        for v in ins:
            if isinstance(v, V):
                r = v.tile.reads
                if r.get(sk, 0) < tok[1]:
                    r[sk] = tok[1]
        for v in outs:
            if isinstance(v, V):
                w = v.tile.writes
                if w.get(sk, 0) < tok[1]:
                    w[sk] = tok[1]
        return tok

    def dma(self, out, in_, e='sp', is_output=False, **kw):
        tok = self.issue(e, lambda: self.eng[e].dma_start(out=_ap(out), in_=_ap(in_), **kw), [out], [in_], dma=True)
        if is_output:
            self.out_tokens.append(tok)
        return tok

    def mm(self, out, lhsT, rhs, start=True, stop=True, **kw):
        return self.issue('pe', lambda: self.nc.tensor.matmul(_ap(out), _ap(lhsT), _ap(rhs), start=start, stop=stop, **kw),
                          [out], [lhsT, rhs])

    def transpose(self, out, in_, ident):
        return self.issue('pe', lambda: self.nc.tensor.transpose(_ap(out), _ap(in_), _ap(ident)), [out], [in_, ident])

    def act(self, out, in_, func, bias=None, scale=None, accum_out=None, e='act'):
        kw = {}
        ins = [in_]
        outs = [out]
        if bias is not None:
            kw['bias'] = _ap(bias)
            if isinstance(bias, V):
                ins.append(bias)
        if scale is not None:
            kw['scale'] = _ap(scale)
            if isinstance(scale, V):
                ins.append(scale)
        if accum_out is not None:
            kw['accum_out'] = _ap(accum_out)
            outs.append(accum_out)
        return self.issue('act', lambda: self.nc.scalar.activation(out=_ap(out), in_=_ap(in_), func=func, **kw), outs, ins)

    def ts(self, out, in0, s1, op0, s2=None, op1=None, e='dve', accum_out=None):
        ins = [in0] + [s for s in (s1, s2) if isinstance(s, V)]
        kw = {}
        outs = [out]
        if op1 is not None:
            kw['op1'] = op1
        if accum_out is not None:
            kw['accum_out'] = _ap(accum_out)
            outs.append(accum_out)
        return self.issue(e, lambda: self.eng[e].tensor_scalar(out=_ap(out), in0=_ap(in0), scalar1=_ap(s1), scalar2=_ap(s2),
                                                               op0=op0, **kw), outs, ins)

    def tt(self, out, in0, in1, op, e='dve'):
        return self.issue(e, lambda: self.eng[e].tensor_tensor(out=_ap(out), in0=_ap(in0), in1=_ap(in1), op=op), [out], [in0, in1])

    def stt(self, out, in0, scalar, in1, op0, op1, e='dve'):
        ins = [in0, in1] + ([scalar] if isinstance(scalar, V) else [])
        return self.issue(e, lambda: self.eng[e].scalar_tensor_tensor(out=_ap(out), in0=_ap(in0), scalar=_ap(scalar), in1=_ap(in1),
                                                                      op0=op0, op1=op1), [out], ins)

    def copy(self, out, in_, e='dve'):
        if e == 'act':
            return self.act(out, in_, AF.Copy)
        return self.issue(e, lambda: self.eng[e].tensor_copy(out=_ap(out), in_=_ap(in_)), [out], [in_])

    def memset(self, out, val, e='dve'):
        return self.issue(e, lambda: self.eng[e].memset(_ap(out), val), [out], [])

    def recip(self, out, in_):
        return self.issue('dve', lambda: self.nc.vector.reciprocal(out=_ap(out), in_=_ap(in_)), [out], [in_])

    def scan(self, out, d0, d1, initial, op0=ALU.mult, op1=ALU.add):
        ins = [d0, d1] + ([initial] if isinstance(initial, V) else [])
        return self.issue('dve', lambda: self.nc.vector.tensor_tensor_scan(out=_ap(out), data0=_ap(d0), data1=_ap(d1),
                                                                           initial=_ap(initial), op0=op0, op1=op1), [out], ins)

    def reduce(self, out, in_, op, axis=AX.X, e='dve'):
        return self.issue(e, lambda: self.eng[e].tensor_reduce(out=_ap(out), in_=_ap(in_), axis=axis, op=op), [out], [in_])

    def iota(self, out, pattern, base=0, channel_multiplier=0, **kw):
        return self.issue('pool', lambda: self.nc.gpsimd.iota(_ap(out), pattern, base=base, channel_multiplier=channel_multiplier, **kw),
                          [out], [])

    def affine_select(self, out, in_, pattern, compare_op, fill, base=0, channel_multiplier=0):
        return self.issue('pool', lambda: self.nc.gpsimd.affine_select(out=_ap(out), in_=_ap(in_), pattern=pattern,
                                                                       compare_op=compare_op, fill=fill, base=base,
                                                                       channel_multiplier=channel_multiplier), [out], [in_])

    def finish(self):
        last = {}
        for sk, val in self.out_tokens:
            last[sk] = max(last.get(sk, 0), val)
        for sk, val in last.items():
            self._wait('sp', sk, val)
        return self.nc


def run(kb_or_nc, in_maps, n=8):
    nc = kb_or_nc.nc if isinstance(kb_or_nc, KB) else kb_or_nc
    import time as _time
    _t = _time.time()
    res = run_bass_kernel_spmd(nc, in_maps, core_ids=list(range(n)))
    nb = sum(a.nbytes for m in in_maps for a in m.values())
    print(f"[launch] {_time.time() - _t:.1f}s in_bytes={nb/1e6:.0f}MB", file=sys.stderr, flush=True)
    return res.results


def chunks(NT, sz=512):
    return [(s, min(sz, NT - s)) for s in range(0, NT, sz)]

def load_vecs(kb, vecs_d, nv):
    vt = kb.sb([128, 16, nv], name="vecs")
    kb.dma(vt.all, vecs_d.rearrange("(kt p) v -> p kt v", p=128))
    return vt

def consts(kb):
    c = {}
    c['ones'] = kb.sb([128, 128], name="ones"); kb.memset(c['ones'].all, 1.0)
    c['eps'] = kb.sb([128, 1], name="eps"); kb.memset(c['eps'].all, 1e-6)
    return c

def norm_mod(kb, c, xT, hT, vt, NT, seg_cols, gcol, D=2048, xdt=F32):
    KT = D // 128
    gsc = {}
    for (s0, sz, shc, scc) in seg_cols:
        if scc not in gsc:
            g = kb.sb([128, KT], name=f"gsc{scc}")
            kb.stt(g.all, vt[:, :, scc], 1.0, vt[:, :, gcol], ALU.add, ALU.mult)
            gsc[scc] = g
    sq = [kb.sb([128, 512], name=f"sq{i}") for i in range(2)]
    tmp = [kb.sb([128, 512], name=f"nm_tmp{i}") for i in range(2)]
    rstd = kb.sb([128, 512], name="rstd")
    ps = kb.ps([128, 512], name="ps_norm")
    n = 0
    for (s0, sz, shc, scc) in seg_cols:
        for (c0, cs) in chunks(sz):
            a = s0 + c0
            for kt in range(KT):
                q = sq[kt % 2]
                kb.act(q[:, :cs], xT[:, kt, a:a+cs], AF.Square)
                kb.mm(ps[:, :cs], c['ones'].all, q[:, :cs], start=(kt == 0), stop=(kt == KT-1))
            kb.act(rstd[:, :cs], ps[:, :cs], AF.Sqrt, bias=c['eps'].all, scale=1.0 / D)
            kb.recip(rstd[:, :cs], rstd[:, :cs])
            for kt in range(KT):
                t = tmp[kt % 2]
                kb.stt(t[:, :cs], xT[:, kt, a:a+cs], gsc[scc][:, kt:kt+1], rstd[:, :cs], ALU.mult, ALU.mult)
                kb.act(hT[:, kt, a:a+cs], t[:, :cs], AF.Identity, bias=vt[:, kt, shc:shc+1])

def proj(kb, hT, W_d, NT, n_out, KT, evac, wname="w", tag="", pss=None, wbufs=None):
    if wbufs is None:
        wst = [kb.sb([128, KT, 128], F32, name=f"{tag}wst{i}") for i in range(2)]
        wbf = [kb.sb([128, KT, 128], BF16, name=f"{tag}wbf{i}") for i in range(2)]
    else:
        wst, wbf = wbufs
    if pss is None:
        pss = [kb.ps([128, 512], name=f"{tag}pp{i}") for i in range(3)]
    pi = 0
    Wv = W_d.rearrange("(kt p) n -> p kt n", p=128)
    nj = (n_out + 127) // 128
    for j in range(nj):
        nsz = min(128, n_out - j * 128)
        ws, wb = wst[j % 2], wbf[j % 2]
        kb.dma(ws[:, :KT, :nsz], Wv[:, :, j*128:j*128+nsz])
        kb.copy(wb[:, :KT, :nsz], ws[:, :KT, :nsz], e='pool')
        for (c0, cs) in chunks(NT):
            p = pss[pi % 3]; pi += 1
            for kt in range(KT):
                kb.mm(p[:nsz, :cs], wb[:, kt, :nsz], hT[:, kt, c0:c0+cs], start=(kt == 0), stop=(kt == KT-1))
            evac(j, nsz, c0, cs, p[:nsz, :cs])

def build_front(NT=2176, NLAT=2048, n_out=4128):
    kb = KB()
    xT_d = kb.dram_in("xT", [2048, NT])
    vecs_d = kb.dram_in("vecs", [2048, 5])
    W_d = kb.dram_in("W", [2048, n_out])
    pT_d = kb.dram_out("pT", [n_out, NT])
    c = consts(kb)
    vt = load_vecs(kb, vecs_d, 5)
    xv = xT_d.rearrange("(kt p) t -> p kt t", p=128)
    hT = kb.sb([128, 16, NT], BF16, name="hT")
    def x_src(kt, a, cs, dst):
        kb.dma(dst, xv[:, kt, a:a+cs])
    def sink(a, cs, h32):
        kb.copy(hT[:, :, a:a+cs], h32[:, :, :cs], e='pool')
    m = kb.mark()
    norm_mod2(kb, c, x_src, sink, vt, [(0, NLAT, 0, 1), (NLAT, NT - NLAT, 2, 3)], 4, tag="n1")
    kb.release(m)
    ost = [kb.sb([128, NT], F32, name=f"ost{i}") for i in range(2)]
    state = {'n': 0}
    def evac(j, nsz, c0, cs, pv):
        o = ost[j % 2]
        if state['n'] % 2 == 0:
            kb.copy(o[:nsz, c0:c0+cs], pv, e='act')
        else:
            kb.copy(o[:nsz, c0:c0+cs], pv, e='dve')
        state['n'] += 1
        if c0 + cs == NT:
            kb.dma(pT_d[j*128:j*128+nsz, :], o[:nsz, :], is_output=True)
    proj(kb, hT, W_d, NT, n_out, 16, evac)
    kb.finish()
    return kb

def ref_front(x, vecs, W):
    x = x.astype(np.float64)
    g = vecs[:, 4]
    y = x / np.sqrt((x * x).mean(-1, keepdims=True) + 1e-6) * g
    NT = x.shape[0]
    return y


GC = 2 * math.sqrt(2 / math.pi)

def dtrack(kb, ap, name):
    return V(T(kb, None, name), ap)

def norm_mod2(kb, c, x_src, hT_sink, vt, seg_cols, gcol, D=2048, tag="nm"):
    KT = D // 128
    gsc = {}
    for (s0, sz, shc, scc) in seg_cols:
        if scc not in gsc:
            g = kb.sb([128, KT], name=f"{tag}gsc{scc}")
            kb.stt(g.all, vt[:, :, scc], 1.0, vt[:, :, gcol], ALU.add, ALU.mult)
            gsc[scc] = g
    xc = kb.sb([128, KT, 512], name=f"{tag}_xc")
    h32 = kb.sb([128, KT, 512], name=f"{tag}_h32")
    sq = [kb.sb([128, 512], name=f"{tag}_sq{i}") for i in range(2)]
    rstd = kb.sb([128, 512], name=f"{tag}_rstd")
    ps = kb.ps([128, 512], name=f"{tag}_ps")
    for (s0, sz, shc, scc) in seg_cols:
        for (c0, cs) in chunks(sz):
            a = s0 + c0
            for kt in range(KT):
                x_src(kt, a, cs, xc[:, kt, :cs])
                q = sq[kt % 2]
                kb.act(q[:, :cs], xc[:, kt, :cs], AF.Square)
                kb.mm(ps[:, :cs], c['ones'].all, q[:, :cs], start=(kt == 0), stop=(kt == KT-1))
            kb.act(rstd[:, :cs], ps[:, :cs], AF.Sqrt, bias=c['eps'].all, scale=1.0 / D)
            kb.recip(rstd[:, :cs], rstd[:, :cs])
            for kt in range(KT):
                kb.stt(xc[:, kt, :cs], xc[:, kt, :cs], gsc[scc][:, kt:kt+1], rstd[:, :cs], ALU.mult, ALU.mult)
                kb.act(h32[:, kt, :cs], xc[:, kt, :cs], AF.Identity, bias=vt[:, kt, shc:shc+1])
            hT_sink(a, cs, h32)

def gelu_tanh(kb, out, x, t1, t2):
    kb.tt(t1, x, x, ALU.mult, e='pool')
    kb.ts(t1, t1, 0.044715, ALU.mult, 1.0, ALU.add)
    kb.tt(t1, t1, x, ALU.mult, e='pool')
    kb.act(t2, t1, AF.Sigmoid, scale=GC)
    kb.tt(out, t2, x, ALU.mult)

def routing(kb, lg, cw_out, wk):
    L = wk['L']; kb.copy(L[:, :], lg, e='act')
    gmax, ngmax, gm, eg, se, pen = wk['gmax'], wk['ngmax'], wk['gm'], wk['eg'], wk['se'], wk['pen']
    kb.reduce(gmax.all, L[:, 0:4], ALU.max)
    kb.ts(ngmax.all, gmax.all, -1.0, ALU.mult)
    kb.ts(gm.all, L[:, 0:4], gmax[:, 0:1], ALU.is_equal)
    kb.act(eg.all, L[:, 0:4], AF.Exp, bias=ngmax[:, 0:1], accum_out=se.all)
    kb.recip(se.all, se.all)
    kb.ts(pen.all, gm.all, 1e30, ALU.mult, -1e30, ALU.add)
    elm = wk['elm']
    for g in range(4):
        kb.ts(elm[:, g*8:(g+1)*8], L[:, 4+g*8:4+(g+1)*8], pen[:, g:g+1], ALU.add)
    top8 = wk['top8']
    kb.issue('dve', lambda: kb.nc.vector.max(out=top8.all.ap, in_=elm.all.ap), [top8.all], [elm.all])
    d, w1, w2, m1, m2 = wk['d'], wk['w1'], wk['w2'], wk['m1'], wk['m2']
    kb.tt(d.all, top8[:, 1:2], top8[:, 0:1], ALU.subtract)
    kb.act(d.all, d.all, AF.Exp)
    kb.ts(w1.all, d.all, 1.0, ALU.add)
    kb.recip(w1.all, w1.all)
    kb.tt(w2.all, d.all, w1.all, ALU.mult)
    kb.tt(w1.all, w1.all, se.all, ALU.mult)
    kb.tt(w2.all, w2.all, se.all, ALU.mult)
    kb.ts(m1.all, elm.all, top8[:, 0:1], ALU.is_equal, w1[:, 0:1], ALU.mult)
    kb.ts(m2.all, elm.all, top8[:, 1:2], ALU.is_equal, w2[:, 0:1], ALU.mult)
    kb.tt(cw_out, m1.all, m2.all, ALU.add)

def routing_wk(kb):
    def s(n, w): return kb.sb([128, w], name="rt_" + n)
    return {'L': s('L', 36), 'gmax': s('gmax', 1), 'ngmax': s('ngmax', 1), 'gm': s('gm', 4), 'eg': s('eg', 4), 'se': s('se', 1),
            'pen': s('pen', 4), 'elm': s('elm', 32), 'top8': s('top8', 8), 'd': s('d', 1), 'w1': s('w1', 1), 'w2': s('w2', 1),
            'm1': s('m1', 32), 'm2': s('m2', 32)}

def back_tail(kb, c, aT, KTa, wout_d, xT_d, x1T_d, h2T_d, cw_d, vt, wr_d, NT, segs, g1cols, gcol, pss=None, wbufs=None):
    x1tr = dtrack(kb, x1T_d, "x1T_dram")
    x1v = x1T_d.rearrange("(kt p) t -> p kt t", p=128)
    xv = xT_d.rearrange("(kt p) t -> p kt t", p=128)
    xin = [kb.sb([128, 512], name=f"bt_xin{i}") for i in range(3)]
    xo = [kb.sb([128, 512], name=f"bt_xo{i}") for i in range(3)]
    st = {'n': 0}
    def seg_of(tok):
        for i, (s0, sz, _, _) in enumerate(segs):
            if s0 <= tok < s0 + sz: return i
    def evac(j, nsz, c0, cs, pv):
        i = st['n'] % 3; st['n'] += 1
        kb.dma(xin[i][:, :cs], xv[:, j, c0:c0+cs])
        sg = seg_of(c0)
        kb.stt(xo[i][:, :cs], pv, vt[:, j, g1cols[sg]:g1cols[sg]+1], xin[i][:, :cs], ALU.mult, ALU.add)
        kb.dma(V(x1tr.tile, x1v[:, j, c0:c0+cs]), xo[i][:, :cs], is_output=True)
    proj(kb, aT, wout_d, NT, 2048, KTa, evac, tag="wo", pss=pss, wbufs=wbufs)
    wr = kb.sb([128, 16, 36], name="bt_wr"); kb.dma(wr.all, wr_d.rearrange("(kt p) n -> p kt n", p=128))
    hb = kb.sb([128, 16, 512], BF16, name="bt_hb")
    cwsb = kb.sb([128, 32], name="bt_cw")
    psr = kb.ps([128, 36], name="bt_psr")
    wk = routing_wk(kb)
    h2v = h2T_d.rearrange("(kt p) t -> p kt t", p=128)
    def x_src(kt, a, cs, dst):
        kb.dma(dst, V(x1tr.tile, x1v[:, kt, a:a+cs]))
    def sink(a, cs, h32):
        kb.copy(hb[:, :, :cs], h32[:, :, :cs], e='pool')
        kb.dma(h2v[:, :, a:a+cs], hb[:, :, :cs], is_output=True)
        for tt_ in range(cs // 128):
            for kt in range(16):
                kb.mm(psr.all, h32[:, kt, tt_*128:(tt_+1)*128], wr[:, kt, :], start=(kt == 0), stop=(kt == 15))
            routing(kb, psr.all, cwsb.all, wk)
            kb.dma(cw_d[a+tt_*128:a+(tt_+1)*128, :], cwsb.all, is_output=True)
    norm_mod2(kb, c, x_src, sink, vt, segs, gcol, tag="n2")

def build_back0(NT=2176, NLAT=2048):
    kb = KB()
    D = {}
    for n in ["yfT", "ybT", "uT", "ofT", "obT", "rT"]:
        D[n] = kb.dram_in(n, [1024, NT])
    xT_d = kb.dram_in("xT", [2048, NT])
    vecs_d = kb.dram_in("vecs", [2048, 7])
    v8_d = kb.dram_in("v8", [1024, 2])
    wglu_d = kb.dram_in("wglu", [1024, 1024]); wout_d = kb.dram_in("wout", [2048, 2048]); wr_d = kb.dram_in("wr", [2048, 36])
    x1T_d = kb.dram_out("x1T", [2048, NT]); h2T_d = kb.dram_out("h2T", [2048, NT], BF16); cw_d = kb.dram_out("cw", [NT, 32])
    c = consts(kb)
    vt = kb.sb([128, 16, 7], name="vecs_sb"); kb.dma(vt.all, vecs_d.rearrange("(kt p) v -> p kt v", p=128))
    v8 = kb.sb([128, 8, 2], name="v8_sb"); kb.dma(v8.all, v8_d.rearrange("(kt p) v -> p kt v", p=128))
    aT = kb.sb([128, 16, NT], BF16, name="aT")
    mk = kb.mark()
    gT = kb.sb([128, 8, NT], BF16, name="gT")
    def ld(n): return [kb.sb([128, 512], name=f"ld_{n}{i}") for i in range(2)]
    A, Bt, Ct = ld("a"), ld("b"), ld("c")
    t1 = kb.sb([128, 512], name="b0_t1"); t2 = kb.sb([128, 512], name="b0_t2")
    views = {n: D[n].rearrange("(kt p) t -> p kt t", p=128) for n in D}
    n = 0
    for (c0, cs) in chunks(NT):
        for kt in range(8):
            a, b, u = A[n % 2], Bt[n % 2], Ct[n % 2]; n += 1
            kb.dma(a[:, :cs], views["yfT"][:, kt, c0:c0+cs]); kb.dma(b[:, :cs], views["ybT"][:, kt, c0:c0+cs])
            kb.dma(u[:, :cs], views["uT"][:, kt, c0:c0+cs])
            kb.tt(a[:, :cs], a[:, :cs], b[:, :cs], ALU.add)
            kb.stt(a[:, :cs], u[:, :cs], v8[:, kt, 0:1], a[:, :cs], ALU.mult, ALU.add)
            gelu_tanh(kb, gT[:, kt, c0:c0+cs], a[:, :cs], t1[:, :cs], t2[:, :cs])
    def evac_glu(j, nsz, c0, cs, pv):
        kb.act(t2[:, :cs], pv, AF.Sigmoid)
        kb.tt(aT[:, j, c0:c0+cs], t2[:, :cs], gT[:, j, c0:c0+cs], ALU.mult)
    pss = [kb.ps([128, 512], name=f"shp{i}") for i in range(3)]
    wbufs = ([kb.sb([128, 16, 128], F32, name=f"shwst{i}") for i in range(2)], [kb.sb([128, 16, 128], BF16, name=f"shwbf{i}") for i in range(2)])
    proj(kb, gT, wglu_d, NT, 1024, 8, evac_glu, tag="glu", pss=pss, wbufs=wbufs)
    o2 = [kb.sb([128, 512], name=f"b0_o{i}") for i in range(2)]
    r2 = [kb.sb([128, 512], name=f"b0_r{i}") for i in range(2)]
    sq = kb.sb([128, 512], name="b0_sq"); rstd = kb.sb([128, 512], name="b0_rstd")
    psn = kb.ps([128, 512], name="b0_psn")
    for (c0, cs) in chunks(NT):
        for h in range(4):
            for i in range(2):
                kt = 2 * h + i
                a, b = A[i], Bt[i]
                kb.dma(a[:, :cs], views["ofT"][:, kt, c0:c0+cs]); kb.dma(b[:, :cs], views["obT"][:, kt, c0:c0+cs])
                kb.dma(r2[i][:, :cs], views["rT"][:, kt, c0:c0+cs])
                kb.tt(o2[i][:, :cs], a[:, :cs], b[:, :cs], ALU.add)
                kb.act(sq[:, :cs], o2[i][:, :cs], AF.Square)
                kb.mm(psn[:, :cs], c['ones'].all, sq[:, :cs], start=(i == 0), stop=(i == 1))
            kb.act(rstd[:, :cs], psn[:, :cs], AF.Sqrt, bias=c['eps'].all, scale=1.0 / 256)
            kb.recip(rstd[:, :cs], rstd[:, :cs])
            for i in range(2):
                kt = 2 * h + i
                kb.stt(o2[i][:, :cs], o2[i][:, :cs], v8[:, kt, 1:2], rstd[:, :cs], ALU.mult, ALU.mult)
                kb.act(t2[:, :cs], r2[i][:, :cs], AF.Sigmoid)
                kb.tt(t1[:, :cs], r2[i][:, :cs], t2[:, :cs], ALU.mult, e='pool')
                kb.tt(aT[:, 8 + kt, c0:c0+cs], o2[i][:, :cs], t1[:, :cs], ALU.mult)
    kb.release(mk)
    segs = [(0, NLAT, 2, 3), (NLAT, NT - NLAT, 4, 5)]
    back_tail(kb, c, aT, 16, wout_d, xT_d, x1T_d, h2T_d, cw_d, vt, wr_d, NT, segs, [0, 1], 6)
    kb.finish()
    return kb

def np_gelu(x): return 0.5 * x * (1 + np.tanh(math.sqrt(2 / math.pi) * (x + 0.044715 * x ** 3)))
def np_sig(x): return 1 / (1 + np.exp(-x))

def ref_routing(h, wr):
    lg = h @ wr
    gl = lg[:, :4]; gp = np.exp(gl - gl.max(-1, keepdims=True)); gp /= gp.sum(-1, keepdims=True)
    gi = gp.argmax(-1); ptop = gp.max(-1)
    el = lg[:, 4:].reshape(-1, 4, 8)[np.arange(len(h)), gi]
    order = np.argsort(-el, axis=-1)[:, :2]
    ev = np.take_along_axis(el, order, -1)
    w = np.exp(ev - ev.max(-1, keepdims=True)); w /= w.sum(-1, keepdims=True); w *= ptop[:, None]
    cw = np.zeros((len(h), 32))
    for k in range(2):
        cw[np.arange(len(h)), gi * 8 + order[:, k]] = w[:, k]
    return cw

def ref_back0(I, NLAT):
    f = lambda n: I[n].astype(np.float64).T
    vecs = I["vecs"].astype(np.float64); v8 = I["v8"].astype(np.float64)
    y = f("yfT") + f("ybT") + v8[:, 0] * f("uT")
    g = np_gelu(y); aS = g * np_sig(g @ I["wglu"].astype(np.float64))
    o = (f("ofT") + f("obT")).reshape(-1, 4, 256)
    o = o / np.sqrt((o * o).mean(-1, keepdims=True) + 1e-6)
    r = f("rT")
    aG = o.reshape(-1, 1024) * v8[:, 1] * (r * np_sig(r))
    a = np.concatenate([aS, aG], -1)
    ox = a @ I["wout"].astype(np.float64)
    x = f("xT"); NT = x.shape[0]
    g1 = np.where(np.arange(NT)[:, None] < NLAT, vecs[:, 0], vecs[:, 1])
    x1 = x + g1 * ox
    yn = x1 / np.sqrt((x1 * x1).mean(-1, keepdims=True) + 1e-6) * vecs[:, 6]
    sh = np.where(np.arange(NT)[:, None] < NLAT, vecs[:, 2], vecs[:, 4]); sc = np.where(np.arange(NT)[:, None] < NLAT, vecs[:, 3], vecs[:, 5])
    h2 = yn * (1 + sc) + sh
    return x1, h2, ref_routing(h2, I["wr"].astype(np.float64))


TWO_PI = 2 * math.pi

def sin_rr(kb, out, ang, shift, shape, wk):
    a, n, m = wk['a'], wk['n'], wk['m']
    sl = tuple(slice(0, s) for s in shape)
    A, N, M = a[sl], n[sl], m[sl]
    kb.ts(A, ang, 1.0 / TWO_PI, ALU.mult, shift / TWO_PI, ALU.add)
    kb.copy(N, A)
    kb.copy(M, N)
    kb.tt(A, A, M, ALU.subtract)
    kb.ts(M, A, 0.5, ALU.is_gt)
    kb.tt(A, A, M, ALU.subtract)
    kb.ts(M, A, -0.5, ALU.is_lt)
    kb.tt(A, A, M, ALU.add)
    kb.ts(A, A, 0.4999999, ALU.min, -0.4999999, ALU.max)
    kb.act(out, A, AF.Sin, scale=TWO_PI)

def s5_part(kb, uT_d, prm_d, bT_d, cT_d, yT_d, L, NGP=32, T=512):
    prm = kb.sb([128, NGP, 3], name="s5prm"); kb.dma(prm.all, prm_d)
    bT = kb.sb([32, NGP, 2, 128], name="s5bT"); kb.dma(bT.all, bT_d)
    cT = kb.sb([128, NGP, 2, 32], name="s5cT"); kb.dma(cT.all, cT_d)
    kb.ts(cT[:, :, 1, :], cT[:, :, 1, :], -1.0, ALU.mult)
    def sm(name): return kb.sb([128, NGP], name="s5_" + name)
    lr, dt, mag, th, sn, cs = sm("lr"), sm("dt"), sm("mag"), sm("th"), sm("sn"), sm("cs")
    are, aim, den, fre, fim, nfre, t1, t2 = sm("are"), sm("aim"), sm("den"), sm("fre"), sm("fim"), sm("nfre"), sm("t1"), sm("t2")
    wk_s = {'a': kb.sb([128, NGP], name="wka"), 'n': kb.sb([128, NGP], I32, name="wkn"), 'm': kb.sb([128, NGP], name="wkm")}
    kb.ts(lr.all, prm[:, :, 0], -1e-4, ALU.min)
    kb.act(dt.all, prm[:, :, 2], AF.Exp)
    kb.tt(t1.all, lr.all, dt.all, ALU.mult)
    kb.act(mag.all, t1.all, AF.Exp)
    kb.tt(th.all, prm[:, :, 1], dt.all, ALU.mult)
    sin_rr(kb, sn.all, th.all, 0.0, (128, NGP), wk_s)
    sin_rr(kb, cs.all, th.all, math.pi / 2, (128, NGP), wk_s)
    kb.tt(are.all, mag.all, cs.all, ALU.mult)
    kb.tt(aim.all, mag.all, sn.all, ALU.mult)
    kb.ts(are.all, are.all, -1.0, ALU.add)
    li = prm[:, :, 1]
    kb.tt(den.all, lr.all, lr.all, ALU.mult)
    kb.tt(t1.all, li, li, ALU.mult)
    kb.tt(den.all, den.all, t1.all, ALU.add)
    kb.recip(den.all, den.all)
    kb.tt(t1.all, are.all, lr.all, ALU.mult)
    kb.tt(t2.all, aim.all, li, ALU.mult)
    kb.tt(t1.all, t1.all, t2.all, ALU.add)
    kb.tt(fre.all, t1.all, den.all, ALU.mult)
    kb.tt(t1.all, aim.all, lr.all, ALU.mult)
    kb.tt(t2.all, are.all, li, ALU.mult)
    kb.tt(t1.all, t1.all, t2.all, ALU.subtract)
    kb.tt(fim.all, t1.all, den.all, ALU.mult)
    kb.ts(nfre.all, fre.all, -1.0, ALU.mult)
    taui = kb.sb([128, T], I32, name="taui")
    kb.iota(taui.all, [[1, T]], base=1, channel_multiplier=0)
    tau = kb.sb([128, T], name="tau"); kb.copy(tau.all, taui.all)
    ones = kb.sb([128, T], name="onesT"); kb.memset(ones.all, 1.0)
    def big(name, dt_=F32): return kb.sb([128, T], dt_, name="s5_" + name)
    wk = {'a': big("wa"), 'n': big("wn", I32), 'm': big("wm")}
    ch = []
    for j in range(2):
        d = {n: big(f"{n}_{j}") for n in ["ang", "c", "s", "wr", "wi", "rt", "bre", "bim", "p1", "p2", "p3", "p4", "xre", "xim",
                                          "kre", "kim", "q1", "q2", "q3", "q4", "hre0", "hre1", "him0", "him1"]}
        d["A"] = kb.ps([128, 512], name=f"s5A{j}"); d["B"] = kb.ps([128, 512], name=f"s5B{j}"); d["Y"] = kb.ps([32, 512], name=f"s5Y{j}")
        d["u"] = [kb.sb([32, T], name=f"s5u{j}_{i}") for i in range(2)]; d["yo"] = [kb.sb([32, T], name=f"s5y{j}_{i}") for i in range(2)]
        ch.append(d)
    tl = chunks(L, T)

    def tables(j, gp):
        d = ch[j]
        kb.ts(d["ang"].all, tau.all, th[:, gp:gp+1], ALU.mult)
        sin_rr(kb, d["s"].all, d["ang"].all, 0.0, (128, T), wk)
        sin_rr(kb, d["c"].all, d["ang"].all, math.pi / 2, (128, T), wk)
        kb.ts(wk['a'].all, d["c"].all, fre[:, gp:gp+1], ALU.mult)
        kb.stt(d["wr"].all, d["s"].all, fim[:, gp:gp+1], wk['a'].all, ALU.mult, ALU.add)
        kb.ts(wk['m'].all, d["c"].all, fim[:, gp:gp+1], ALU.mult)
        kb.stt(d["wi"].all, d["s"].all, nfre[:, gp:gp+1], wk['m'].all, ALU.mult, ALU.add)
        kb.ts(d["rt"].all, ones.all, mag[:, gp:gp+1], ALU.mult)

    def unit_gen(j, gp, ti):
        d = ch[j]; t0, ts_ = tl[ti]
        A, B, Y, u, yo = d["A"], d["B"], d["Y"], d["u"][ti % 2], d["yo"][ti % 2]
        kb.dma(u[:, :ts_], uT_d[gp*32:(gp+1)*32, t0:t0+ts_])
        c, s, wr, wi, rt = d["c"], d["s"], d["wr"], d["wi"], d["rt"]
        kb.mm(A[:, :ts_], bT[:, gp, 0, :], u[:, :ts_])
        kb.mm(B[:, :ts_], bT[:, gp, 1, :], u[:, :ts_]); yield
        kb.copy(d["bre"][:, :ts_], A[:, :ts_], e='act')
        kb.copy(d["bim"][:, :ts_], B[:, :ts_], e='act'); yield
        kb.tt(d["p1"][:, :ts_], d["bre"][:, :ts_], wr[:, :ts_], ALU.mult, e='pool')
        kb.tt(d["p2"][:, :ts_], d["bim"][:, :ts_], wi[:, :ts_], ALU.mult); yield
        kb.tt(d["p3"][:, :ts_], d["bre"][:, :ts_], wi[:, :ts_], ALU.mult, e='pool')
        kb.tt(d["p4"][:, :ts_], d["bim"][:, :ts_], wr[:, :ts_], ALU.mult); yield
        kb.tt(d["xre"][:, :ts_], d["p1"][:, :ts_], d["p2"][:, :ts_], ALU.subtract); yield
        kb.tt(d["xim"][:, :ts_], d["p3"][:, :ts_], d["p4"][:, :ts_], ALU.add); yield
        if ti == 0:
            ir, ii = 0.0, 0.0
        else:
            pt = tl[ti-1][1]
            ir = d[f"hre{(ti-1) % 2}"][:, pt-1:pt]; ii = d[f"him{(ti-1) % 2}"][:, pt-1:pt]
        kb.scan(d["kre"][:, :ts_], rt[:, :ts_], d["xre"][:, :ts_], ir); yield
        kb.scan(d["kim"][:, :ts_], rt[:, :ts_], d["xim"][:, :ts_], ii); yield
        kb.tt(d["q1"][:, :ts_], d["kre"][:, :ts_], c[:, :ts_], ALU.mult, e='pool')
        kb.tt(d["q3"][:, :ts_], d["kre"][:, :ts_], s[:, :ts_], ALU.mult); yield
        kb.tt(d["q2"][:, :ts_], d["kim"][:, :ts_], s[:, :ts_], ALU.mult); yield
        kb.tt(d["q4"][:, :ts_], d["kim"][:, :ts_], c[:, :ts_], ALU.mult); yield
        hr = d[f"hre{ti % 2}"]; hi = d[f"him{ti % 2}"]
        kb.tt(hi[:, :ts_], d["q3"][:, :ts_], d["q4"][:, :ts_], ALU.add); yield
        kb.tt(hr[:, :ts_], d["q1"][:, :ts_], d["q2"][:, :ts_], ALU.subtract); yield
        kb.mm(Y[:, :ts_], cT[:, gp, 0, :], hr[:, :ts_], start=True, stop=False)
        kb.mm(Y[:, :ts_], cT[:, gp, 1, :], hi[:, :ts_], start=False, stop=True); yield
        kb.copy(yo[:, :ts_], Y[:, :ts_], e='act')
        kb.dma(yT_d[gp*32:(gp+1)*32, t0:t0+ts_], yo[:, :ts_], is_output=True); yield

    for g0 in range(0, NGP, 2):
        gps = [g for g in (g0, g0 + 1) if g < NGP]
        for j, gp in enumerate(gps):
            tables(j, gp)
        for ti in range(len(tl)):
            gens = [unit_gen(j, gp, ti) for j, gp in enumerate(gps)]
            alive = list(gens)
            while alive:
                for g in list(alive):
                    try:
                        next(g)
                    except StopIteration:
                        alive.remove(g)

def s5_host_params(lam_re, lam_im, log_dt, b_re, b_im, c_re, c_im, NGP=32):
    G = NGP * 2
    P, I = 64, 16
    def gpl(a):
        return np.ascontiguousarray(a.reshape(NGP, 2, P).transpose(1, 2, 0).reshape(128, NGP))
    prm = np.stack([gpl(lam_re), gpl(lam_im), gpl(np.repeat(log_dt[:, None], P, axis=1))], axis=-1).astype(np.float32)
    bT = np.zeros((32, NGP, 2, 128), np.float32)
    cT = np.zeros((128, NGP, 2, 32), np.float32)
    for k, (br, cr) in enumerate([(b_re, c_re), (b_im, c_im)]):
        brr = br.reshape(NGP, 2, P, I); crr = cr.reshape(NGP, 2, I, P)
        for g2 in range(2):
            bT[g2*16:(g2+1)*16, :, k, g2*64:(g2+1)*64] = brr[:, g2].transpose(2, 0, 1)
            cT[g2*64:(g2+1)*64, :, k, g2*16:(g2+1)*16] = crr[:, g2].transpose(2, 0, 1)
    return prm, bT, cT

def s5_ref(u, lam_re, lam_im, log_dt, b_re, b_im, c_re, c_im):
    lr = np.minimum(lam_re.astype(np.float64), -1e-4); li = lam_im.astype(np.float64)
    dt = np.exp(log_dt.astype(np.float64))[:, None]
    lam = lr + 1j * li
    abar = np.exp(lam * dt)
    f = (abar - 1) / lam
    B = b_re.astype(np.float64) + 1j * b_im
    C = c_re.astype(np.float64) + 1j * c_im
    L = u.shape[0]
    x = f[None] * np.einsum('lgi,gpi->lgp', u, B)
    h = np.zeros_like(x[0]); ys = []
    for t in range(L):
        h = abar * h + x[t]
        ys.append(np.einsum('gp,gip->gi', h, C).real)
    return np.stack(ys)


def gla_part(kb, qT_d, kT_d, v_d, lrT_d, wg_d, bg_d, rc_d, rs_d, cmask_d, ident_d, o_d, L, T=512):
    H = 4
    cmask = kb.sb([64, 64], name="cmask"); kb.dma(cmask.all, cmask_d)
    identf = kb.sb([128, 128], name="identf"); kb.dma(identf.all, ident_d)
    ident = kb.sb([128, 128], BF16, name="ident"); kb.copy(ident.all, identf.all)
    wg = kb.sb([16, 512], name="wg"); kb.dma(wg.all, wg_d)
    bg = kb.sb([128, 4], name="bg"); kb.dma(bg.all, bg_d)
    rm = kb.sb([128, T], name="rm"); kb.memset(rm.all, 1.0)
    kb.memset(rm[:, 0:T:64], 0.0)
    def big(name, dt_=F32, n=4): return kb.sb([128, n, T], dt_, name="g_" + name)
    q32, qsw, k32, ksw = big("q32"), big("qsw"), big("k32"), big("ksw")
    rc = kb.sb([128, T], name="g_rc"); rs = kb.sb([128, T], name="g_rs")
    lr = kb.sb([16, T], name="g_lr")
    t1, t2 = big("t1"), big("t2")
    la, b16, eb, enb = big("la"), big("b16"), big("eb"), big("enb")
    qinb, kinb = big("qinb", BF16), big("kinb", BF16)
    kin32 = big("kin32")
    S32 = kb.sb([128, 4, 256], name="g_S32"); kb.memset(S32.all, 0.0)
    Sb = kb.sb([128, 4, 256], BF16, name="g_Sb"); kb.memset(Sb.all, 0.0)
    vst = [kb.sb([64, 1024], name=f"g_vst{i}") for i in range(2)]
    vb = [kb.sb([64, 1024], BF16, name=f"g_vb{i}") for i in range(2)]
    osb = [kb.sb([64, 1024], name=f"g_osb{i}") for i in range(2)]
    attb = [kb.sb([64, 64], BF16, name=f"g_attb{i}") for i in range(2)]
    kout = [kb.sb([128, 64], BF16, name=f"g_kout{i}") for i in range(2)]
    koT = [kb.sb([64, 128], BF16, name=f"g_koT{i}") for i in range(2)]
    pAtt = [kb.ps([64, 64], name=f"g_pAtt{i}") for i in range(2)]
    pKo = [kb.ps([64, 128], BF16, name=f"g_pKo{i}") for i in range(2)]
    pO = kb.ps([64, 1024], name="g_pO")
    pU = kb.ps([128, 256], name="g_pU")
    pZ = kb.ps([128, T], name="g_pZ")
    qv = qT_d.rearrange("(h p) t -> p h t", p=128)
    kv = kT_d.rearrange("(h p) t -> p h t", p=128)
    scale = 128 ** -0.5
    cn = 0
    for (t0, ts_) in chunks(L, T):
        kb.dma(q32[:, :, :ts_], qv[:, :, t0:t0+ts_])
        kb.dma(k32[:, :, :ts_], kv[:, :, t0:t0+ts_])
        for blk in range(4):
            src = blk ^ 1
            kb.dma(qsw[blk*32:(blk+1)*32, :, :ts_], qv[src*32:(src+1)*32, :, t0:t0+ts_])
            kb.dma(ksw[blk*32:(blk+1)*32, :, :ts_], kv[src*32:(src+1)*32, :, t0:t0+ts_])
        kb.dma(rc[:, :ts_], rc_d[:, t0:t0+ts_]); kb.dma(rs[:, :ts_], rs_d[:, t0:t0+ts_])
        kb.dma(lr[:, :ts_], lrT_d[:, t0:t0+ts_])
        for h in range(H):
            kb.mm(pZ[:, :ts_], wg[:, h*128:(h+1)*128], lr[:, :ts_])
            kb.act(la[:, h, :ts_], pZ[:, :ts_], AF.Sigmoid, bias=bg[:, h:h+1])
        for h in range(H):
            kb.act(la[:, h, :ts_], la[:, h, :ts_], AF.Ln)
            kb.scan(b16[:, h, :ts_], rm[:, :ts_], la[:, h, :ts_], 0.0)
        for h in range(H):
            kb.act(eb[:, h, :ts_], b16[:, h, :ts_], AF.Exp, scale=1.0 / 16)
            kb.act(enb[:, h, :ts_], b16[:, h, :ts_], AF.Exp, scale=-1.0 / 16)
        for h in range(H):
            kb.tt(t1[:, h, :ts_], q32[:, h, :ts_], rc[:, :ts_], ALU.mult)
            kb.tt(t2[:, h, :ts_], qsw[:, h, :ts_], rs[:, :ts_], ALU.mult, e='pool')
            kb.tt(t1[:, h, :ts_], t1[:, h, :ts_], t2[:, h, :ts_], ALU.add)
            kb.stt(qinb[:, h, :ts_], t1[:, h, :ts_], scale, eb[:, h, :ts_], ALU.mult, ALU.mult)
        for h in range(H):
            kb.tt(t1[:, h, :ts_], k32[:, h, :ts_], rc[:, :ts_], ALU.mult)
            kb.tt(t2[:, h, :ts_], ksw[:, h, :ts_], rs[:, :ts_], ALU.mult, e='pool')
            kb.tt(t1[:, h, :ts_], t1[:, h, :ts_], t2[:, h, :ts_], ALU.add)
            kb.tt(kin32[:, h, :ts_], t1[:, h, :ts_], enb[:, h, :ts_], ALU.mult)
            kb.copy(kinb[:, h, :ts_], kin32[:, h, :ts_], e='pool')
        for c in range(ts_ // 64):
            c0 = c * 64
            vs, vbb, ob = vst[cn % 2], vb[cn % 2], osb[cn % 2]
            kb.dma(vs.all, v_d[t0+c0:t0+c0+64, :])
            kb.copy(vbb.all, vs.all, e='pool')
            for h in range(H):
                i2 = (cn * 4 + h) % 2
                dec = eb[:, h, c0+63:c0+64]
                kb.mm(pAtt[i2].all, kinb[:, h, c0:c0+64], qinb[:, h, c0:c0+64])
                kb.tt(attb[i2].all, pAtt[i2].all, cmask.all, ALU.mult)
                kb.ts(kout[i2].all, kin32[:, h, c0:c0+64], dec, ALU.mult, e='pool')
                kb.transpose(pKo[i2].all, kout[i2].all, ident.all)
                kb.copy(koT[i2].all, pKo[i2].all, e='act')
                kb.mm(pO[:, h*256:(h+1)*256], attb[i2].all, vbb[:, h*256:(h+1)*256], start=True, stop=False)
                kb.mm(pO[:, h*256:(h+1)*256], qinb[:, h, c0:c0+64], Sb[:, h, :], start=False, stop=True)
                kb.mm(pU.all, koT[i2].all, vbb[:, h*256:(h+1)*256])
                kb.stt(S32[:, h, :], S32[:, h, :], dec, pU.all, ALU.mult, ALU.add)
                kb.copy(Sb[:, h, :], S32[:, h, :], e='act')
            kb.copy(ob[:, 0:512], pO[:, 0:512], e='act')
            kb.copy(ob[:, 512:1024], pO[:, 512:1024], e='dve')
            kb.dma(o_d[t0+c0:t0+c0+64, :], ob.all, is_output=True)
            cn += 1

def rope_tables(pos_row, pos_col, is_ctx):
    nf = 32
    inv = (10000.0 ** (-np.arange(nf, dtype=np.float32) / nf)).astype(np.float32)
    L = len(pos_row)
    C = np.ones((128, L), np.float32); S = np.zeros((128, L), np.float32)
    for half, pos in enumerate([pos_row, pos_col]):
        ang = pos.astype(np.float32)[None, :] * inv[:, None]
        c = np.cos(ang).astype(np.float32); s = np.sin(ang).astype(np.float32)
        C[half*64:half*64+32] = c; C[half*64+32:half*64+64] = c
        S[half*64:half*64+32] = -s; S[half*64+32:half*64+64] = s
    C[:, is_ctx] = 1.0; S[:, is_ctx] = 0.0
    return C, S

def gla_ref(q, k, v, lowrank, wg, bg, C, S):
    L = q.shape[0]
    def rope(z):
        sw = z.reshape(L, 4, 2, 2, 32)[:, :, :, ::-1, :].reshape(L, 4, 128)
        return z * C.T[:, None, :] + sw * S.T[:, None, :]
    q = rope(q) * 128 ** -0.5; k = rope(k)
    z = lowrank @ wg + bg
    la = -np.logaddexp(0, -z) / 16.0
    a = np.exp(la).reshape(L, 4, 128)
    St = np.zeros((4, 128, 256)); o = np.zeros((L, 4, 256))
    for t in range(L):
        St = a[t][:, :, None] * St + k[t][:, :, None] * v[t][:, None, :]
        o[t] = np.einsum('hd,hde->he', q[t], St)
    return o


def build_moe(NTOK=8704, NE=8, F=1024, D=2048):
    kb = KB(); nc = kb.nc
    KT = D // 128; FT = F // 128
    hT_d = kb.dram_in("hT", [D, NTOK], BF16)
    cwT_d = kb.dram_in("cwT", [NE, NTOK])
    wg_d = kb.dram_in("wg", [NE, D, F]); wu_d = kb.dram_in("wu", [NE, D, F]); wd_d = kb.dram_in("wd", [NE, F, D])
    yT_d = kb.dram_out("yT", [D, NTOK], BF16)
    sg_d = nc.dram_tensor("scr_g", [NE * FT, 128, KT * 128], BF16, kind="Internal").ap()
    su_d = nc.dram_tensor("scr_u", [NE * FT, 128, KT * 128], BF16, kind="Internal").ap()
    sd_d = nc.dram_tensor("scr_d", [NE * FT, 128, D], BF16, kind="Internal").ap()
    hv = hT_d.rearrange("(kt p) t -> p kt t", p=128)
    yv = yT_d.rearrange("(kt p) t -> p kt t", p=128)
    mk = kb.mark()
    st32 = [kb.sb([128, KT * 128], name=f"m_st32_{i}") for i in range(6)]
    st16 = [kb.sb([128, KT * 128], BF16, name=f"m_st16_{i}") for i in range(6)]
    engs = ['act', 'dve', 'pool']
    n = 0
    for e in range(NE):
        for f in range(FT):
            for (src, dst) in ((wg_d, sg_d), (wu_d, su_d)):
                i = n % 6; n += 1
                a32 = V(st32[i], st32[i].t[:, :].rearrange("p (kt n) -> p kt n", n=128))
                kb.dma(a32, src[e, :, f*128:(f+1)*128].rearrange("(kt p) n -> p kt n", p=128))
                kb.copy(st16[i].all, st32[i].all, e=engs[n % 3])
                kb.dma(dst[e*FT+f], st16[i].all, e='pool')
            i = n % 6; n += 1
            kb.dma(st32[i].all, wd_d[e, f*128:(f+1)*128, :])
            kb.copy(st16[i].all, st32[i].all, e=engs[n % 3])
            kb.dma(sd_d[e*FT+f], st16[i].all, e='pool')
    kb.release(mk)
    hT = [kb.sb([128, KT, 512], BF16, name=f"m_hT{i}") for i in range(2)]
    aT = kb.sb([128, NE * FT, 512], BF16, name="m_aT")
    cwb = [kb.sb([128, 512], name=f"m_cwb{i}") for i in range(2)]
    wgb = [kb.sb([128, KT, 128], BF16, name=f"m_wgb{i}") for i in range(3)]
    wub = [kb.sb([128, KT, 128], BF16, name=f"m_wub{i}") for i in range(3)]
    wdb = [kb.sb([128, 512], BF16, name=f"m_wdb{i}") for i in range(4)]
    sg = [kb.sb([128, 512], name=f"m_sg{i}") for i in range(2)]
    t1 = [kb.sb([128, 512], name=f"m_t1{i}") for i in range(2)]
    ysb = [kb.sb([128, 512], BF16, name=f"m_ysb{i}") for i in range(2)]
    pG = [kb.ps([128, 512], name=f"m_pG{i}") for i in range(2)]
    pU = [kb.ps([128, 512], name=f"m_pU{i}") for i in range(2)]
    pY = [kb.ps([128, 512], name=f"m_pY{i}") for i in range(4)]
    sgv = sg_d.rearrange("s p (kt n) -> s p kt n", n=128)
    suv = su_d.rearrange("s p (kt n) -> s p kt n", n=128)
    n = 0; nd = 0; ny = 0
    for ci, (c0, cs) in enumerate(chunks(NTOK)):
        h = hT[ci % 2]
        kb.dma(h[:, :, :cs], hv[:, :, c0:c0+cs])
        for e in range(NE):
            cw = cwb[e % 2]
            kb.dma(cw[:, :cs], cwT_d[e:e+1, c0:c0+cs].broadcast_to([128, cs]))
            for f in range(FT):
                i = n % 2; j3 = n % 3; n += 1
                kb.dma(wgb[j3].all, sgv[e*FT+f], e='sp')
                kb.dma(wub[j3].all, suv[e*FT+f], e='pool')
                for kt in range(KT):
                    kb.mm(pG[i][:, :cs], wgb[j3][:, kt, :], h[:, kt, :cs], start=(kt == 0), stop=(kt == KT-1))
                for kt in range(KT):
                    kb.mm(pU[i][:, :cs], wub[j3][:, kt, :], h[:, kt, :cs], start=(kt == 0), stop=(kt == KT-1))
                kb.act(sg[i][:, :cs], pG[i][:, :cs], AF.Silu)
                kb.tt(t1[i][:, :cs], sg[i][:, :cs], pU[i][:, :cs], ALU.mult)
                kb.tt(aT[:, e*FT+f, :cs], t1[i][:, :cs], cw[:, :cs], ALU.mult)
        for dq in range(D // 512):
            for ef in range(NE * FT):
                j = nd % 4; nd += 1
                kb.dma(wdb[j].all, sd_d[ef, :, dq*512:(dq+1)*512], e='sp' if nd % 2 else 'pool')
                for dd in range(4):
                    kb.mm(pY[dd][:, :cs], wdb[j][:, dd*128:(dd+1)*128], aT[:, ef, :cs], start=(ef == 0), stop=(ef == NE*FT-1))
            for dd in range(4):
                yo = ysb[ny % 2]; ny += 1
                kb.copy(yo[:, :cs], pY[dd][:, :cs], e='act')
                kb.dma(yv[:, dq*4+dd, c0:c0+cs], yo[:, :cs], is_output=True)
    kb.finish()
    return kb

def np_silu(x): return x / (1 + np.exp(-x))


NEGM = -1.0e4

def qrows_for_keyrow(rp, rows=64, kh=8):
    res = []
    for r in range(rows):
        st = min(max(r - kh // 2, 0), rows - kh)
        if st <= rp <= st + kh - 1:
            res.append(r)
    return res

def start_of(r, rows=64, kh=8):
    return min(max(r - kh // 2, 0), rows - kh)

def build_attn(NH=8):
    kb = KB()
    W = 64; ROWS = 64; LL = 4096; LC = 256
    qT_d = kb.dram_in("qT", [NH, 128, LL], BF16)
    kT_d = kb.dram_in("kT", [NH, 128, LL + LC], BF16)
    v_d = kb.dram_in("v", [LL + LC, NH, 128], BF16)
    bias_d = kb.dram_in("biasT", [NH, 64, 15 * 64])
    o_d = kb.dram_out("o", [LL, NH * 128], BF16)
    scale = 128 ** -0.5
    qT = [kb.sb([128, LL], BF16, name=f"a_qT{i}") for i in range(2)]
    kT = [kb.sb([128, LL + LC], BF16, name=f"a_kT{i}") for i in range(2)]
    Vl = [kb.sb([64, ROWS, 129], BF16, name=f"a_Vl{i}") for i in range(2)]
    Vc = [kb.sb([128, 2, 129], BF16, name=f"a_Vc{i}") for i in range(2)]
    bias = [kb.sb([64, 15 * 64], name=f"a_bias{i}") for i in range(2)]
    for i in range(2):
        kb.memset(Vl[i][:, :, 128:129], 1.0); kb.memset(Vc[i][:, :, 128:129], 1.0)
    RING = 16
    PT = [kb.sb([64, 15 * 64], BF16, name=f"a_PT{i}") for i in range(RING)]
    PcT = [[kb.sb([128, 512], BF16, name=f"a_Pc{g}_{ct}") for ct in range(2)] for g in range(2)]
    tmp = [kb.sb([64, 512], name=f"a_tmp{i}") for i in range(2)]
    osb = [kb.sb([64, 8, 128], BF16, name=f"a_osb{i}") for i in range(2)]
    rec = [kb.sb([64, 1], name=f"a_rec{i}") for i in range(2)]
    pS = [kb.ps([64, 512], name=f"a_pS{i}") for i in range(2)]
    pC = [kb.ps([128, 512], name=f"a_pC{i}") for i in range(2)]
    pO = [kb.ps([64, 129], name=f"a_pO{i}") for i in range(2)]
    vlat = v_d[0:LL].rearrange("(r c) h d -> c r h d", c=64)
    vctx = v_d[LL:LL + LC].rearrange("(t p) h d -> p t h d", p=128)
    ov = o_d.rearrange("(r c) f -> c r f", c=64)
    ns = 0; no = 0
    for h in range(NH):
        b = h % 2
        kb.dma(qT[b].all, qT_d[h]); kb.dma(kT[b].all, kT_d[h])
        kb.dma(Vl[b][:, :, 0:128], vlat[:, :, h, :]); kb.dma(Vc[b][:, :, 0:128], vctx[:, :, h, :])
        kb.dma(bias[b].all, bias_d[h])
        done_ctx = set()
        for rp in range(ROWS):
            qr = qrows_for_keyrow(rp)
            lo, hi = qr[0], qr[-1]
            ptile = PT[rp % RING]
            for p0 in range(lo, hi + 1, 8):
                p1 = min(p0 + 8, hi + 1); n = p1 - p0
                S = pS[ns % 2]; t = tmp[ns % 2]; ns += 1
                kb.mm(S[:, :n*64], kT[b][:, rp*64:(rp+1)*64], qT[b][:, p0*64:p1*64])
                j0 = p0 - rp + 7
                kb.stt(t[:, :n*64], S[:, :n*64], scale, bias[b][:, j0*64:(j0+n)*64], ALU.mult, ALU.add)
                kb.act(ptile[:, j0*64:(j0+n)*64], t[:, :n*64], AF.Exp)
            for r in range(ROWS):
                st = start_of(r)
                if st + 7 != rp:
                    continue
                g = r // 8
                if g not in done_ctx:
                    done_ctx.add(g)
                    for ct in range(2):
                        C = pC[ct]
                        kb.mm(C.all, kT[b][:, LL+ct*128:LL+(ct+1)*128], qT[b][:, g*512:(g+1)*512])
                        kb.act(PcT[g % 2][ct].all, C.all, AF.Exp, scale=scale)
                O = pO[no % 2]; rc = rec[no % 2]; no += 1
                for i, rk in enumerate(range(st, st + 8)):
                    j = r - rk + 7
                    kb.mm(O.all, PT[rk % RING][:, j*64:(j+1)*64], Vl[b][:, rk, :], start=(i == 0), stop=False)
                for ct in range(2):
                    kb.mm(O.all, PcT[g % 2][ct][:, (r % 8)*64:(r % 8 + 1)*64], Vc[b][:, ct, :], start=False, stop=(ct == 1))
                kb.recip(rc.all, O[:, 128:129])
                ob = osb[g % 2]
                kb.ts(ob[:, r % 8, :], O[:, 0:128], rc[:, 0:1], ALU.mult)
                if r % 8 == 7:
                    kb.dma(ov[:, g*8:(g+1)*8, h*128:(h+1)*128], ob.all, is_output=True)
    kb.finish()
    return kb

def host_bias(rpb):
    H = rpb.shape[0]
    col = np.arange(64)
    cs = np.clip(col - 8, 0, 48)
    col_ok = (col[None, :] >= cs[:, None]) & (col[None, :] < cs[:, None] + 16)
    dc = np.clip(col[None, :] - col[:, None] + 15, 0, 30)
    out = np.empty((H, 64, 15, 64), np.float32)
    for j in range(15):
        dr = 14 - j
        bq = rpb[:, dr, :][:, dc]
        bq = np.where(col_ok[None], bq, np.float32(NEGM))
        out[:, :, j, :] = bq.transpose(0, 2, 1)
    return out.reshape(H, 64, 15 * 64)

def attn_ref(q, k, v, kc, vc, rpb):
    scale = 128 ** -0.5
    out = np.zeros((4096, 128))
    col = np.arange(64); cs = np.clip(col - 8, 0, 48)
    col_ok = (col[None, :] >= cs[:, None]) & (col[None, :] < cs[:, None] + 16)
    dc = np.clip(col[None, :] - col[:, None] + 15, 0, 30)
    for r in range(64):
        st = start_of(r)
        qr = q[r*64:(r+1)*64]
        kb_ = k[st*64:(st+8)*64]; vb = v[st*64:(st+8)*64]
        dr = st + np.arange(8) - r + 7
        bias = rpb[dr][:, dc]
        bias = np.where(col_ok[None], bias, -1e30).transpose(1, 0, 2).reshape(64, 512)
        s = np.concatenate([qr @ kb_.T * scale + bias, qr @ kc.T * scale], -1)
        p = np.exp(s - s.max(-1, keepdims=True)); p /= p.sum(-1, keepdims=True)
        out[r*64:(r+1)*64] = p[:, :512] @ vb + p[:, 512:] @ vc
    return out


def combine_y(kb, x_d, yg_d, out_tr, outv, vt, NT, segs_g2, ngroups=4, tag="cy"):
    xv = x_d.rearrange("(kt p) t -> p kt t", p=128)
    yv = yg_d.rearrange("g (kt p) t -> p g kt t", p=128)
    xt = [kb.sb([128, 512], name=f"{tag}_x{i}") for i in range(4)]
    yt = [kb.sb([128, ngroups, 512], BF16, name=f"{tag}_y{i}") for i in range(4)]
    ys = [kb.sb([128, 2, 512], name=f"{tag}_ys{i}") for i in range(4)]
    n = 0
    for (s0, sz, g2c) in segs_g2:
        for (c0, cs) in chunks(sz):
            a = s0 + c0
            for kt in range(16):
                x, y, s2 = xt[n % 4], yt[n % 4], ys[n % 4]; n += 1
                kb.dma(x[:, :cs], xv[:, kt, a:a+cs])
                kb.dma(y[:, :, :cs], yv[:, :, kt, a:a+cs])
                kb.tt(s2[:, 0, :cs], y[:, 0, :cs], y[:, 1, :cs], ALU.add)
                kb.tt(s2[:, 1, :cs], y[:, 2, :cs], y[:, 3, :cs], ALU.add, e='pool')
                kb.tt(s2[:, 0, :cs], s2[:, 0, :cs], s2[:, 1, :cs], ALU.add)
                kb.stt(x[:, :cs], s2[:, 0, :cs], vt[:, kt, g2c:g2c+1], x[:, :cs], ALU.mult, ALU.add)
                kb.dma(V(out_tr.tile, outv[:, kt, a:a+cs]), x[:, :cs], is_output=True)

def build_front1(NT=2176, NLAT=2048):
    kb = KB()
    x1T_d = kb.dram_in("x1T", [2048, NT]); yg_d = kb.dram_in("ygT", [4, 2048, NT], BF16)
    vecs_d = kb.dram_in("vecs", [2048, 7]); qkg_d = kb.dram_in("qkg", [128, 2]); W_d = kb.dram_in("wqkv", [2048, 6144])
    x2T_d = kb.dram_out("x2T", [2048, NT])
    outs = [kb.dram_out(n, [2048, NT], BF16) for n in ["qT", "kT", "vT"]]
    c = consts(kb)
    vt = kb.sb([128, 16, 7], name="vecs_sb"); kb.dma(vt.all, vecs_d.rearrange("(kt p) v -> p kt v", p=128))
    qkg = kb.sb([128, 2], name="qkg_sb"); kb.dma(qkg.all, qkg_d)
    x2tr = dtrack(kb, x2T_d, "x2T_dram"); x2v = x2T_d.rearrange("(kt p) t -> p kt t", p=128)
    hT = kb.sb([128, 16, NT], BF16, name="hT")
    mk = kb.mark()
    combine_y(kb, x1T_d, yg_d, x2tr, x2v, vt, NT, [(0, NLAT, 0), (NLAT, NT - NLAT, 1)])
    kb.release(mk)
    def x_src(kt, a, cs, dst):
        kb.dma(dst, V(x2tr.tile, x2v[:, kt, a:a+cs]))
    def sink(a, cs, h32):
        kb.copy(hT[:, :, a:a+cs], h32[:, :, :cs], e='pool')
    norm_mod2(kb, c, x_src, sink, vt, [(0, NLAT, 2, 3), (NLAT, NT - NLAT, 4, 5)], 6, tag="n1")
    ost = [kb.sb([128, NT], BF16, name=f"f1_ost{i}") for i in range(2)]
    sq = [kb.sb([128, 512], name=f"f1_sq{i}") for i in range(2)]
    rstd = [kb.sb([128, 512], name=f"f1_rstd{i}") for i in range(2)]
    pN = kb.ps([128, 512], name="f1_pN")
    st = {'n': 0}
    def evac(j, nsz, c0, cs, pv):
        o = ost[j % 2]; i = st['n'] % 2; st['n'] += 1
        which = j // 16
        if which < 2:
            kb.act(sq[i][:, :cs], pv, AF.Square)
            kb.mm(pN[:, :cs], c['ones'].all, sq[i][:, :cs])
            kb.act(rstd[i][:, :cs], pN[:, :cs], AF.Sqrt, bias=c['eps'].all, scale=1.0 / 128)
            kb.recip(rstd[i][:, :cs], rstd[i][:, :cs])
            kb.stt(o[:, c0:c0+cs], pv, qkg[:, which:which+1], rstd[i][:, :cs], ALU.mult, ALU.mult)
        else:
            kb.copy(o[:, c0:c0+cs], pv, e='act')
        if c0 + cs == NT:
            jj = j % 16
            kb.dma(outs[which][jj*128:(jj+1)*128, :], o.all, is_output=True)
    proj(kb, hT, W_d, NT, 6144, 16, evac, tag="qkv")
    kb.finish()
    return kb

def build_back1(NT=2048):
    kb = KB()
    oT_d = kb.dram_in("oT", [2048, NT], BF16); xT_d = kb.dram_in("xT", [2048, NT])
    vecs_d = kb.dram_in("vecs", [2048, 4])
    wout_d = kb.dram_in("wout", [2048, 2048]); wr_d = kb.dram_in("wr", [2048, 36])
    x1T_d = kb.dram_out("x1T", [2048, NT]); h2T_d = kb.dram_out("h2T", [2048, NT], BF16); cw_d = kb.dram_out("cw", [NT, 32])
    c = consts(kb)
    vt = kb.sb([128, 16, 4], name="vecs_sb"); kb.dma(vt.all, vecs_d.rearrange("(kt p) v -> p kt v", p=128))
    aT = kb.sb([128, 16, NT], BF16, name="aT")
    ov = oT_d.rearrange("(kt p) t -> p kt t", p=128)
    for kt in range(16):
        kb.dma(aT[:, kt, :], ov[:, kt, :])
    back_tail(kb, c, aT, 16, wout_d, xT_d, x1T_d, h2T_d, cw_d, vt, wr_d, NT, [(0, NT, 1, 2)], [0], 3)
    kb.finish()
    return kb

def build_final(NT=2048):
    kb = KB()
    xT_d = kb.dram_in("xT", [2048, NT]); yg_d = kb.dram_in("ygT", [4, 2048, NT], BF16); vecs_d = kb.dram_in("vecs", [128, 16])
    out_d = kb.dram_out("outT", [2048, NT])
    vt = kb.sb([128, 16, 1], name="vecs_sb"); kb.dma(vt[:, :, 0], vecs_d)
    otr = dtrack(kb, out_d, "out_dram"); ov = out_d.rearrange("(kt p) t -> p kt t", p=128)
    combine_y(kb, xT_d, yg_d, otr, ov, vt, NT, [(0, NT, 0)])
    kb.finish()
    return kb


def build_ada():
    kb = KB()
    NCOL = 1536
    cT = kb.dram_in("cT", [2048, 5])
    w = kb.dram_in("w", [2, 2048, NCOL])
    b = kb.dram_in("b", [2, 1, NCOL])
    out = kb.dram_out("mod", [2, 5, NCOL])
    ct = kb.sb([128, 16, 5]); st = kb.sb([128, 16, 5])
    kb.dma(ct.all, cT.rearrange("(kt p) m -> p kt m", p=128))
    kb.act(st.all, ct.all, AF.Silu)
    wpool = [kb.sb([128, NCOL], name=f"wp{i}") for i in range(4)]
    wi = 0
    for l in range(2):
        bt = kb.sb([5, NCOL])
        kb.dma(bt.all, b[l].broadcast_to([5, NCOL]))
        ot = kb.sb([5, NCOL])
        pss = [kb.ps([5, 512]) for _ in range(3)]
        for kt in range(16):
            wt = wpool[wi % 4]; wi += 1
            kb.dma(wt.all, w[l, kt*128:(kt+1)*128, :])
            for c in range(3):
                kb.mm(pss[c].all, st[:, kt, :], wt[:, c*512:(c+1)*512], start=(kt == 0), stop=(kt == 15))
        for c in range(3):
            kb.tt(ot[:, c*512:(c+1)*512], pss[c].all, bt[:, c*512:(c+1)*512], ALU.add)
        kb.dma(out[l], ot.all, is_output=True)
    kb.finish()
    return kb


import numpy as np

def build_s5gla(L=4352):
    kb = KB()
    uT_d = kb.dram_in("uT", [1024, L]); prm_d = kb.dram_in("prm", [128, 32, 3])
    bT_d = kb.dram_in("bT", [32, 32, 2, 128]); cT_d = kb.dram_in("cT", [128, 32, 2, 32])
    yT_d = kb.dram_out("yT", [1024, L])
    qT_d = kb.dram_in("qT", [512, L]); kT_d = kb.dram_in("kT", [512, L]); v_d = kb.dram_in("v", [L, 1024])
    lrT_d = kb.dram_in("lrT", [16, L]); wg_d = kb.dram_in("wg", [16, 512]); bg_d = kb.dram_in("bg", [128, 4])
    rc_d = kb.dram_in("rc", [128, L]); rs_d = kb.dram_in("rs", [128, L])
    cm_d = kb.dram_in("cmask", [64, 64]); id_d = kb.dram_in("identm", [128, 128])
    o_d = kb.dram_out("o", [L, 1024])
    mk = kb.mark()
    s5_part(kb, uT_d, prm_d, bT_d, cT_d, yT_d, L, 32)
    kb.release(mk)
    gla_part(kb, qT_d, kT_d, v_d, lrT_d, wg_d, bg_d, rc_d, rs_d, cm_d, id_d, o_d, L)
    kb.finish()
    return kb

def _c(a):
    return np.ascontiguousarray(a)

def kernel(x, c, ctx, c_ctx, ada_w, ada_b, norm1_g, norm2_g, ab_w_in, ab_w_out,
           s5_lam_re, s5_lam_im, s5_log_dt, s5_b_re, s5_b_im, s5_c_re, s5_c_im, s5_d, s5_w_glu,
           gla_w_gate2, gla_b_gate, gla_norm_g, na_w_qkv, na_w_out, na_q_norm, na_k_norm, na_rpb,
           moe_w_route_group, moe_w_route_expert, moe_w_gate, moe_w_up, moe_w_down):
    f32 = np.float32
    A = lambda a: np.asarray(a, dtype=f32) if np.asarray(a).dtype != f32 else np.asarray(a)
    x = A(x); c = A(c); ctx = A(ctx); c_ctx = A(c_ctx)
    B, SEQ, D = x.shape; CTX = ctx.shape[1]
    NC = 8; HL = SEQ // 2; HC = CTX // 2; NT = HL + HC
    cT = _c(np.concatenate([c.T, c_ctx[:, None]], axis=1))
    ada_w = A(ada_w); ada_b = A(ada_b)
    res = run(build_ada(), [{"cT": cT, "w": _c(ada_w[:, :, i*1536:(i+1)*1536]), "b": _c(ada_b[:, None, i*1536:(i+1)*1536])}
                            for i in range(NC)])
    mod = np.concatenate([r["mod"] for r in res], axis=2)
    def modv(l, k, row):
        return mod[l, row, k*D:(k+1)*D]
    cores = [(cc // 2, cc % 2) for cc in range(NC)]
    xT_core = []
    for (b, h) in cores:
        xT_core.append(_c(np.concatenate([x[b, h*HL:(h+1)*HL], ctx[b, h*HC:(h+1)*HC]], axis=0).T))
    w_in = A(ab_w_in)[0]
    ims = []
    for ci, (b, h) in enumerate(cores):
        vecs = np.stack([modv(0, 0, b), modv(0, 1, b), modv(0, 0, 4), modv(0, 1, 4), A(norm1_g)[0]], axis=1)
        ims.append({"xT": xT_core[ci], "vecs": _c(vecs), "W": w_in})
    res = run(build_front(NT, HL, 4128), ims)
    pT = [r["pT"] for r in res]
    px_lat = [np.concatenate([pT[2*b][:, :HL], pT[2*b+1][:, :HL]], axis=1) for b in range(B)]
    px_ctx = [np.concatenate([pT[2*b][:, HL:], pT[2*b+1][:, HL:]], axis=1) for b in range(B)]
    def seq_T(b, d, rows):
        cpart = px_ctx[b][rows]; lpart = px_lat[b][rows]
        if d == 1:
            cpart = cpart[:, ::-1]; lpart = lpart[:, ::-1]
        return np.concatenate([cpart, lpart], axis=1)
    L = CTX + SEQ
    ims = []
    for cc in range(NC):
        b, d = cc // 2, cc % 2
        prm, bT, cTt = s5_host_params(A(s5_lam_re)[0, d], A(s5_lam_im)[0, d], A(s5_log_dt)[0, d], A(s5_b_re)[0, d], A(s5_b_im)[0, d],
                                      A(s5_c_re)[0, d], A(s5_c_im)[0, d], NGP=32)
        ims.append({"uT": _c(seq_T(b, d, slice(0, 1024))), "prm": prm, "bT": bT, "cT": cTt})
    ims_s5 = ims
    o0 = 1024
    pos = np.arange(SEQ)
    cmask = np.triu(np.ones((64, 64), f32)); identm = np.eye(128, dtype=f32)
    ims = []
    for cc in range(NC):
        b, d = cc // 2, cc % 2
        p_lat = pos[::-1] if d == 1 else pos
        prow = np.concatenate([np.zeros(CTX, np.int64), p_lat // 64]); pcol = np.concatenate([np.zeros(CTX, np.int64), p_lat % 64])
        is_ctx = np.arange(L) < CTX
        rc, rs = rope_tables(prow, pcol, is_ctx)
        ims.append({**ims_s5[cc], "qT": _c(seq_T(b, d, slice(o0, o0 + 512))), "kT": _c(seq_T(b, d, slice(o0 + 512, o0 + 1024))),
                    "v": _c(seq_T(b, d, slice(o0 + 1024, o0 + 2048)).T),
                    "lrT": _c(seq_T(b, d, slice(o0 + 3072 + d*16, o0 + 3072 + (d+1)*16))),
                    "wg": _c(A(gla_w_gate2)[0, d]), "bg": _c(A(gla_b_gate)[0, d].reshape(4, 128).T),
                    "rc": rc, "rs": rs, "cmask": cmask, "identm": identm})
    res = run(build_s5gla(L), ims)
    def unflip(a, d):
        cpart, lpart = a[:, :CTX], a[:, CTX:]
        if d == 1:
            cpart = cpart[:, ::-1]; lpart = lpart[:, ::-1]
        return cpart, lpart
    yS = [[unflip(res[2*b+d]["yT"], d) for d in range(2)] for b in range(B)]
    oG = [[unflip(_c(res[2*b+d]["o"].T), d) for d in range(2)] for b in range(B)]
    def tokpart(pair, h):
        return np.concatenate([pair[1][:, h*HL:(h+1)*HL], pair[0][:, h*HC:(h+1)*HC]], axis=1)
    wr0 = _c(np.concatenate([A(moe_w_route_group)[0], A(moe_w_route_expert)[0]], axis=1))
    v8 = _c(np.stack([A(s5_d)[0], A(gla_norm_g)[0]], axis=1))
    ims = []
    for ci, (b, h) in enumerate(cores):
        vecs = np.stack([modv(0, 2, b), modv(0, 2, 4), modv(0, 3, b), modv(0, 4, b), modv(0, 3, 4), modv(0, 4, 4), A(norm2_g)[0]], axis=1)
        pxp = (px_ctx[b], px_lat[b])
        ims.append({"yfT": _c(tokpart(yS[b][0], h)), "ybT": _c(tokpart(yS[b][1], h)),
                    "uT": _c(tokpart((pxp[0][0:1024], pxp[1][0:1024]), h)),
                    "ofT": _c(tokpart(oG[b][0], h)), "obT": _c(tokpart(oG[b][1], h)),
                    "rT": _c(tokpart((pxp[0][o0+2048:o0+3072], pxp[1][o0+2048:o0+3072]), h)),
                    "xT": xT_core[ci], "vecs": _c(vecs), "v8": v8, "wglu": A(s5_w_glu)[0], "wout": A(ab_w_out)[0], "wr": wr0})
    res = run(build_back0(NT, HL), ims)
    x1T = [r["x1T"] for r in res]; h2T = [r["h2T"] for r in res]; cw = [r["cw"] for r in res]
    del pT, px_lat, px_ctx, yS, oG, ims
    def run_moe(layer, h2T, cw, ntok_core):
        NTOK = 4 * ntok_core
        ims = []
        for cc in range(NC):
            g, hh = cc // 2, cc % 2
            hT = _c(np.concatenate([h2T[4*hh + i] for i in range(4)], axis=1))
            cwT = _c(np.concatenate([cw[4*hh + i] for i in range(4)], axis=0)[:, g*8:(g+1)*8].T)
            ims.append({"hT": hT, "cwT": cwT, "wg": A(moe_w_gate)[layer, g*8:(g+1)*8], "wu": A(moe_w_up)[layer, g*8:(g+1)*8],
                        "wd": A(moe_w_down)[layer, g*8:(g+1)*8]})
        res = run(build_moe(NTOK, 8, 1024, 2048), ims)
        out = []
        for ci in range(NC):
            hh, i = ci // 4, ci % 4
            out.append(_c(np.stack([res[2*g + hh]["yT"][:, i*ntok_core:(i+1)*ntok_core] for g in range(4)], axis=0)))
        return out
    yg0 = run_moe(0, h2T, cw, NT)
    qkg = _c(np.stack([A(na_q_norm)[0], A(na_k_norm)[0]], axis=1))
    ims = []
    for ci, (b, h) in enumerate(cores):
        vecs = np.stack([modv(0, 5, b), modv(0, 5, 4), modv(1, 0, b), modv(1, 1, b), modv(1, 0, 4), modv(1, 1, 4), A(norm1_g)[1]], axis=1)
        ims.append({"x1T": x1T[ci], "ygT": yg0[ci], "vecs": _c(vecs), "qkg": qkg, "wqkv": A(na_w_qkv)[0]})
    res = run(build_front1(NT, HL), ims)
    x2T = [_c(r["x2T"][:, :HL]) for r in res]
    def full(name, b):
        return np.concatenate([res[2*b][name][:, :HL], res[2*b+1][name][:, :HL], res[2*b][name][:, HL:], res[2*b+1][name][:, HL:]], axis=1)
    del yg0, x1T, h2T, cw
    biasT = host_bias(A(na_rpb)[0])
    ims = []
    for cc in range(NC):
        b, hg = cc // 2, cc % 2
        qf = full("qT", b).reshape(16, 128, L)[hg*8:(hg+1)*8]
        kf = full("kT", b).reshape(16, 128, L)[hg*8:(hg+1)*8]
        vf = full("vT", b).reshape(16, 128, L)[hg*8:(hg+1)*8]
        ims.append({"qT": _c(qf[:, :, :SEQ]), "kT": _c(kf), "v": _c(vf.transpose(2, 0, 1)), "biasT": _c(biasT[hg*8:(hg+1)*8])})
    res = run(build_attn(8), ims)
    wr1 = _c(np.concatenate([A(moe_w_route_group)[1], A(moe_w_route_expert)[1]], axis=1))
    ims = []
    for ci, (b, h) in enumerate(cores):
        oT = _c(np.concatenate([res[2*b][ "o"][h*HL:(h+1)*HL], res[2*b+1]["o"][h*HL:(h+1)*HL]], axis=1).T)
        vecs = np.stack([modv(1, 2, b), modv(1, 3, b), modv(1, 4, b), A(norm2_g)[1]], axis=1)
        ims.append({"oT": oT, "xT": x2T[ci], "vecs": _c(vecs), "wout": A(na_w_out)[0], "wr": wr1})
    res = run(build_back1(HL), ims)
    x3T = [r["x1T"] for r in res]; h2T = [r["h2T"] for r in res]; cw = [r["cw"] for r in res]
    yg1 = run_moe(1, h2T, cw, HL)
    ims = []
    for ci, (b, h) in enumerate(cores):
        ims.append({"xT": x3T[ci], "ygT": yg1[ci], "vecs": _c(modv(1, 5, b).reshape(16, 128).T)})
    res = run(build_final(HL), ims)
    out = np.empty((B, SEQ, D), f32)
    for ci, (b, h) in enumerate(cores):
        out[b, h*HL:(h+1)*HL] = res[ci]["outT"].T
    return out
```
